# Optimizing a Trainium2 kernel written in Bass

```python
import jax, jax.numpy as jnp
from jax import lax
import numpy as np

D_MODEL = 1024
BATCH = 8
SEQ = 2048
DEPTH = 1

PLE_DIM = 256
ROPE_THETA = 500000.0
EPS = 1e-6
NEG_INF = -1e30

MIX_WIDTH = D_MODEL
MLA_V_DIM = 64
MLA_NOPE_DIM = 64
MLA_ROPE_DIM = 32
MLA_QK_DIM = MLA_NOPE_DIM + MLA_ROPE_DIM
MLA_WIDTH = MIX_WIDTH // 2
MLA_HEADS = MLA_WIDTH // MLA_V_DIM
MLA_Q_RANK = 3 * D_MODEL // 8
MLA_KV_RANK = 4 * MLA_V_DIM
Q_BLOCK = 128
DIL_HEAD_DIM = 64
DIL_WIDTH = MIX_WIDTH - MLA_WIDTH
DIL_HEADS = DIL_WIDTH // DIL_HEAD_DIM
DIL_ROT_DIM = DIL_HEAD_DIM // 4
DIL_PATTERNS = ((128, 1), (512, 4), (2048, 16))
IN_SPLITS = (MLA_Q_RANK, MLA_KV_RANK, MLA_ROPE_DIM, DIL_WIDTH, DIL_WIDTH, DIL_WIDTH)
IN_WIDTH = sum(IN_SPLITS)
PEER_KEYS = 128
PEER_EXPERTS = PEER_KEYS * PEER_KEYS
PEER_HEADS = 8
PEER_QDIM = 128
PEER_HALF = PEER_QDIM // 2
PEER_TOPK = 16
PEER_CHUNK = 128

kernel_name = "hybrid_mla_dilswa_peer_encoder"


def rms_norm(x, g):
    xf = x.astype(jnp.float32)
    y = xf * lax.rsqrt(jnp.mean(xf * xf, axis=-1, keepdims=True) + EPS)
    return (y * g.astype(jnp.float32)).astype(x.dtype)


def rope_tables(seq, rot_dim):
    inv = ROPE_THETA ** (-jnp.arange(0, rot_dim, 2, dtype=jnp.float32) / rot_dim)
    ang = jnp.arange(seq, dtype=jnp.float32)[:, None] * inv[None, :]
    return jnp.cos(ang), jnp.sin(ang)


def apply_rope(x, cos, sin, rot_dim):
    half = rot_dim // 2
    shape = (x.shape[1],) + (1,) * (x.ndim - 3) + (half,)
    c = cos.reshape(shape).astype(x.dtype)
    s = sin.reshape(shape).astype(x.dtype)
    x1 = x[..., :half]
    x2 = x[..., half:rot_dim]
    return jnp.concatenate([x1 * c - x2 * s, x2 * c + x1 * s, x[..., rot_dim:]], axis=-1)


def mla_attention(q_nope, q_rope, k_nope, k_rope, v):
    B, S, H, _ = q_nope.shape
    nq = S // Q_BLOCK
    scale = MLA_QK_DIM ** -0.5

    def blocks(t):
        return jnp.moveaxis(t.reshape((B, nq, Q_BLOCK) + t.shape[2:]), 1, 0)

    def one_block(args):
        qn, qr = args
        s = jnp.einsum('bqhd,bkhd->bhqk', qn, k_nope) + jnp.einsum('bqhr,bkr->bhqk', qr, k_rope)
        w = jax.nn.softmax(s.astype(jnp.float32) * scale, axis=-1).astype(v.dtype)
        return jnp.einsum('bhqk,bkhd->bqhd', w, v)

    out = lax.map(one_block, (blocks(q_nope), blocks(q_rope)))
    return jnp.moveaxis(out, 0, 1).reshape(B, S, H, v.shape[-1])


def banded_attention(q, k, v, radius):
    N, L, hd = q.shape
    blk = radius
    nb = -(-L // blk)
    lp = nb * blk
    qb = jnp.pad(q, ((0, 0), (0, lp - L), (0, 0))).reshape(N, nb, blk, hd)

    def windows(t):
        tp = jnp.pad(t, ((0, 0), (blk, lp - L + blk), (0, 0))).reshape(N, nb + 2, blk, hd)
        return jnp.concatenate([tp[:, :-2], tp[:, 1:-1], tp[:, 2:]], axis=2)

    kw, vw = windows(k), windows(v)
    qpos = jnp.arange(nb)[:, None] * blk + jnp.arange(blk)[None, :]
    kpos = jnp.arange(nb)[:, None] * blk - blk + jnp.arange(3 * blk)[None, :]
    kp = kpos[:, None, :]
    mask = (jnp.abs(qpos[:, :, None] - kp) <= radius) & (kp >= 0) & (kp < L)
    s = jnp.einsum('nbqd,nbkd->nbqk', qb, kw).astype(jnp.float32) * (hd ** -0.5)
    s = jnp.where(mask, s, NEG_INF)
    m = jnp.max(s, axis=-1, keepdims=True)
    e = jnp.exp(s - m)
    den = jnp.sum(e, axis=-1, keepdims=True)
    out = jnp.einsum('nbqk,nbkd->nbqd', (e / den).astype(v.dtype), vw)
    lse = (m + jnp.log(den))[..., 0]
    return out.reshape(N, lp, hd)[:, :L], lse.reshape(N, lp)[:, :L]


def dilated_pattern(q, k, v, window, dil):
    B, S, H, hd = q.shape
    L = S // dil

    def to_sub(t):
        return t.reshape(B, L, dil, H, hd).transpose(0, 2, 3, 1, 4).reshape(B * dil * H, L, hd)

    o, l = banded_attention(to_sub(q), to_sub(k), to_sub(v), window // (2 * dil))
    o = o.reshape(B, dil, H, L, hd).transpose(0, 3, 1, 2, 4).reshape(B, S, H, hd)
    l = l.reshape(B, dil, H, L).transpose(0, 3, 1, 2).reshape(B, S, H)
    return o, l


def dilated_attention(q, k, v):
    outs, lses = [], []
    for window, dil in DIL_PATTERNS:
        o, l = dilated_pattern(q, k, v, window, dil)
        outs.append(o)
        lses.append(l)
    wts = jax.nn.softmax(jnp.stack(lses, axis=0), axis=0).astype(q.dtype)
    return jnp.einsum('pbsh,pbshd->bshd', wts, jnp.stack(outs, axis=0))


def peer_ffn(xn, w_q, keys1, keys2, u_tab, v_tab):
    B, S, D = xn.shape
    T = B * S
    xt = xn.reshape(T, D)
    q = (xt @ w_q).reshape(T, PEER_HEADS, 2, PEER_HALF)
    s1 = jnp.einsum('thd,kd->thk', q[:, :, 0], keys1)
    s2 = jnp.einsum('thd,kd->thk', q[:, :, 1], keys2)
    v1, i1 = lax.top_k(s1, PEER_TOPK)
    v2, i2 = lax.top_k(s2, PEER_TOPK)
    cand = (v1[..., :, None] + v2[..., None, :]).reshape(T, PEER_HEADS, PEER_TOPK * PEER_TOPK)
    cidx = (i1[..., :, None] * PEER_KEYS + i2[..., None, :]).reshape(T, PEER_HEADS, PEER_TOPK * PEER_TOPK)
    top, pos = lax.top_k(cand, PEER_TOPK)
    eidx = jnp.take_along_axis(cidx, pos, axis=-1)
    g = jax.nn.softmax(top.astype(jnp.float32), axis=-1).astype(xn.dtype)
    nc = T // PEER_CHUNK

    def chunk(args):
        xc, ec, gc = args
        a = jax.nn.gelu(jnp.einsum('td,thkd->thk', xc, u_tab[ec]), approximate=False)
        return jnp.einsum('thk,thkd->td', gc * a, v_tab[ec])

    y = lax.map(chunk, (xt.reshape(nc, PEER_CHUNK, D),
                        eidx.reshape(nc, PEER_CHUNK, PEER_HEADS, PEER_TOPK),
                        g.reshape(nc, PEER_CHUNK, PEER_HEADS, PEER_TOPK)))
    return y.reshape(B, S, D)


def setup_inputs(seed: int = 0) -> dict:
    key = jax.random.key(seed)
    ks = jax.random.split(key, 24)
    f32 = jnp.float32

    def nrm(k, shape, scale):
        return jax.random.normal(k, shape, f32) * scale

    def gain(k, shape):
        return 1.0 + 0.01 * jax.random.normal(k, shape, f32)

    L = DEPTH
    return {
        "x": nrm(ks[0], (BATCH, SEQ, D_MODEL), 1.0),
        "p": nrm(ks[1], (DEPTH, BATCH, SEQ, PLE_DIM), 1.0),
        "g_mix": gain(ks[2], (L, D_MODEL)),
        "w_in": nrm(ks[3], (L, D_MODEL, IN_WIDTH), D_MODEL ** -0.5),
        "g_cq": gain(ks[4], (L, MLA_Q_RANK)),
        "w_uq": nrm(ks[5], (L, MLA_Q_RANK, MLA_HEADS * MLA_QK_DIM), MLA_Q_RANK ** -0.5),
        "g_ckv": gain(ks[6], (L, MLA_KV_RANK)),
        "w_ukv": nrm(ks[7], (L, MLA_KV_RANK, MLA_HEADS * (MLA_NOPE_DIM + MLA_V_DIM)), MLA_KV_RANK ** -0.5),
        "g_out_mla": gain(ks[8], (L, MLA_WIDTH)),
        "g_out_dil": gain(ks[9], (L, DIL_WIDTH)),
        "w_out": nrm(ks[10], (L, MIX_WIDTH, D_MODEL), MIX_WIDTH ** -0.5),
        "g_ffn": gain(ks[11], (L, D_MODEL)),
        "w_peer_q": nrm(ks[12], (L, D_MODEL, PEER_HEADS * PEER_QDIM), D_MODEL ** -0.5),
        "peer_keys1": nrm(ks[13], (L, PEER_KEYS, PEER_HALF), PEER_HALF ** -0.5),
        "peer_keys2": nrm(ks[14], (L, PEER_KEYS, PEER_HALF), PEER_HALF ** -0.5),
        "peer_u": nrm(ks[15], (L, PEER_EXPERTS, D_MODEL), D_MODEL ** -0.5),
        "peer_v": nrm(ks[16], (L, PEER_EXPERTS, D_MODEL), 0.5),
        "g_ple": gain(ks[17], (L, D_MODEL)),
        "w_ple_gate": nrm(ks[18], (L, D_MODEL, D_MODEL), D_MODEL ** -0.5),
        "w_ple_proj": nrm(ks[19], (L, PLE_DIM, D_MODEL), PLE_DIM ** -0.5),
        "g_final": gain(ks[20], (D_MODEL,)),
    }


def reference(x, p, g_mix, w_in, g_cq, w_uq, g_ckv, w_ukv, g_out_mla, g_out_dil, w_out,
              g_ffn, w_peer_q, peer_keys1, peer_keys2, peer_u, peer_v,
              g_ple, w_ple_gate, w_ple_proj, g_final):
    B, S, _ = x.shape
    cos_m, sin_m = rope_tables(S, MLA_ROPE_DIM)
    cos_d, sin_d = rope_tables(S, DIL_ROT_DIM)
    offs = np.cumsum(IN_SPLITS)[:-1].tolist()
    h = x
    for i in range(DEPTH):
        hn = rms_norm(h, g_mix[i])
        c_q, c_kv, k_r, q_d, k_d, v_d = jnp.split(hn @ w_in[i], offs, axis=-1)
        q = (rms_norm(c_q, g_cq[i]) @ w_uq[i]).reshape(B, S, MLA_HEADS, MLA_QK_DIM)
        q_nope = q[..., :MLA_NOPE_DIM]
        q_rope = apply_rope(q[..., MLA_NOPE_DIM:], cos_m, sin_m, MLA_ROPE_DIM)
        kv = (rms_norm(c_kv, g_ckv[i]) @ w_ukv[i]).reshape(B, S, MLA_HEADS, MLA_NOPE_DIM + MLA_V_DIM)
        k_nope = kv[..., :MLA_NOPE_DIM]
        v_m = kv[..., MLA_NOPE_DIM:]
        k_rope = apply_rope(k_r, cos_m, sin_m, MLA_ROPE_DIM)
        o_mla = mla_attention(q_nope, q_rope, k_nope, k_rope, v_m).reshape(B, S, MLA_WIDTH)
        qd = apply_rope(q_d.reshape(B, S, DIL_HEADS, DIL_HEAD_DIM), cos_d, sin_d, DIL_ROT_DIM)
        kd = apply_rope(k_d.reshape(B, S, DIL_HEADS, DIL_HEAD_DIM), cos_d, sin_d, DIL_ROT_DIM)
        vd = v_d.reshape(B, S, DIL_HEADS, DIL_HEAD_DIM)
        o_dil = dilated_attention(qd, kd, vd).reshape(B, S, DIL_WIDTH)
        mixed = jnp.concatenate([rms_norm(o_mla, g_out_mla[i]), rms_norm(o_dil, g_out_dil[i])], axis=-1)
        h = h + mixed @ w_out[i]
        h = h + peer_ffn(rms_norm(h, g_ffn[i]), w_peer_q[i], peer_keys1[i], peer_keys2[i],
                         peer_u[i], peer_v[i])
        gate = jax.nn.sigmoid(rms_norm(h, g_ple[i]) @ w_ple_gate[i])
        h = h + gate * (p[i] @ w_ple_proj[i])
    return rms_norm(h, g_final)
```

```python
import threading
from contextlib import ExitStack

import numpy as np
import ml_dtypes

import concourse.bass as bass
import concourse.mybir as mybir
from concourse.bass_utils import run_bass_kernel_spmd

F32 = mybir.dt.float32
BF16 = mybir.dt.bfloat16
I32 = mybir.dt.int32
U32 = mybir.dt.uint32
ALU = mybir.AluOpType
AF = mybir.ActivationFunctionType
AX = mybir.AxisListType

S = 2048
D = 1024
NT = 16
EPS = 1e-6
IN_W = 2208
NCORES = 8
DBG_ONLY = None
STOP = None
LVL = 99
TL = 99
P3L = 99
NT_RUN = None


class Buf:
    def __init__(self, name, dma=False):
        self.name = name
        self.w = None
        self.r = []
        self.dma = dma
        self.dsem = None
        self.dcount = 0


class Prog:
    ENGS = ("sync", "scalar", "vector", "gpsimd", "tensor")

    def __init__(self):
        self.ops = {e: [] for e in self.ENGS}
        self.waited = {e: {} for e in self.ENGS}
        self.dma_bufs = []
        self.final_tokens = []
        self.hook = None

    def _need(self, eng, tok, kind):
        if tok[0] == "eng" and tok[1] == eng:
            if eng == "tensor":
                return False
        return True

    def op(self, eng, fn, reads=(), writes=(), dma_buf=None, extra=()):
        deps = []
        for b in reads:
            if b.w is not None:
                deps.append((b.w, "raw"))
            if b.name.startswith("pf") or b.name.startswith("pb"):
                for t in b.r:
                    if not (t[0] == "eng" and t[1] == eng):
                        deps.append((t, "rar"))
        for b in writes:
            if b.w is not None:
                deps.append((b.w, "waw"))
            for t in b.r:
                deps.append((t, "war"))
        for t in extra:
            deps.append((t, "raw"))
        waits = []
        wd = self.waited[eng]
        for tok, kind in deps:
            if not self._need(eng, tok, kind):
                continue
            key = (tok[0], tok[1] if tok[0] == "eng" else id(tok[1]))
            if wd.get(key, -1) >= tok[2]:
                continue
            wd[key] = tok[2]
            waits.append(tok)
        seq = len(self.ops[eng])
        if dma_buf is not None:
            if dma_buf.dsem is None:
                self.dma_bufs.append(dma_buf)
                dma_buf.dsem = True
            dma_buf.dcount += 16
            tok = ("dma", dma_buf, dma_buf.dcount)
        else:
            tok = ("eng", eng, seq)
        self.ops[eng].append(dict(fn=fn, waits=waits, dma_buf=dma_buf, marked=False))
        for b in reads:
            b.r.append(tok)
        for b in writes:
            b.w = tok
            b.r = []
        if self.hook is not None:
            self.hook.tick()
        return tok

    def lag(self, n):
        if self.hook is not None:
            for _ in range(n):
                self.hook.tick()

    def finalize_marks(self):
        for e in self.ENGS:
            for o in self.ops[e]:
                for tok in o["waits"]:
                    if tok[0] == "eng":
                        self.ops[tok[1]][tok[2]]["marked"] = True
        self.count_at = {}
        for e in self.ENGS:
            c = 0
            arr = []
            for o in self.ops[e]:
                if o["marked"]:
                    c += 1
                arr.append(c)
            self.count_at[e] = arr

    def emit(self, eng, handle, esems):
        for o in self.ops[eng]:
            for tok in o["waits"]:
                if tok[0] == "eng":
                    handle.wait_ge(esems[tok[1]], self.count_at[tok[1]][tok[2]])
                else:
                    handle.wait_ge(tok[1].dsem, tok[2])
            if o["fn"] is None:
                continue
            ins = o["fn"](handle)
            if o["dma_buf"] is not None:
                ins.then_inc(o["dma_buf"].dsem, 16)
            elif o["marked"]:
                ins.then_inc(esems[eng], 1)


class Stepper:
    def __init__(self, prog, fn):
        self.prog = prog
        self.fn = fn
        self.go = threading.Semaphore(0)
        self.back = threading.Semaphore(0)
        self.finished = False
        self.budget = 0
        self.err = None
        self.th = threading.Thread(target=self._run, daemon=True)
        self.th.start()

    def _run(self):
        self.go.acquire()
        try:
            self.prog.hook = self
            self.fn()
        except BaseException as e:
            self.err = e
        self.prog.hook = None
        self.finished = True
        self.back.release()

    def tick(self):
        if threading.current_thread() is not self.th:
            return
        self.budget -= 1
        if self.budget <= 0:
            self.prog.hook = None
            self.back.release()
            self.go.acquire()
            self.prog.hook = self

    def advance(self, k):
        if self.finished:
            return
        self.budget = k
        self.go.release()
        self.back.acquire()
        if self.err is not None:
            raise self.err

    def finish(self):
        while not self.finished:
            self.advance(1 << 30)


STEP_K = 6
LAG = 8


class Ring:
    def __init__(self, items):
        self.items = items
        self.i = 0

    def next(self):
        it = self.items[self.i % len(self.items)]
        self.i += 1
        return it


def build_program(debug=False, phases=3):
    nc = bass.Bass("TRN2", target_bir_lowering=False)
    P = Prog()
    es = ExitStack()

    declared = []

    def din(name, shape, dt=F32, need=1):
        if phases < need:
            return None
        declared.append(name)
        return nc.dram_tensor(name, list(shape), dt, kind="ExternalInput").ap()

    def dout(name, shape, dt=F32):
        return nc.dram_tensor(name, list(shape), dt, kind="ExternalOutput").ap()

    x_d = din("x", [S, D])
    p_d = din("p", [S, 256], need=3)
    w_in_d = din("w_in", [D, IN_W])
    w_uq_d = din("w_uq", [384, 768])
    w_ukv_d = din("w_ukv", [256, 1024])
    w_out_d = din("w_out", [D, D], need=3)
    w_pq_d = din("w_peer_q", [D, D], need=3)
    keys_d = din("keys12", [128, 128], need=3)
    u_d = din("peer_u", [16384, D], need=3)
    v_d = din("peer_v", [16384, D], need=3)
    w_pg_d = din("w_ple_gate", [D, D], need=3)
    w_pp_d = din("w_ple_proj", [256, D], need=3)
    g_ffn_d = din("g_ffn", [D], need=3)
    g_fin_d = din("g_final", [D], need=3)
    gcols_d = din("gcols", [128, 37])
    ident_d = din("ident", [128, 128])
    ropem_d = din("ropem", [128, NT, 2, 16])
    roped_d = din("roped", [128, NT, 2, 8])
    mask_d = din("dilmask", [128, 2944])
    iota_d = din("iota16", [128, 16], need=3)
    out_d = dout("out", [S, D])
    if phases >= 3:
        uv16_d = nc.dram_tensor("uv16", [16384, 2 * D], BF16, kind="Internal").ap()
    dbg = {}

    def sb(name, shape, dt):
        return es.enter_context(nc.sbuf_tensor("s_" + name, list(shape), dt))

    def ps(name, shape, dt):
        return es.enter_context(nc.psum_tensor("p_" + name, list(shape), dt))

    regA = sb("regA", [128, 22016], BF16)
    regB = sb("regB", [128, 65792], BF16)
    w_in_sb = regA[:, 0:17664].rearrange("p (c n) -> p c n", n=IN_W)
    w_uq_sb = regA[:, 17664:19968].rearrange("p (c n) -> p c n", n=768)
    w_ukv_sb = regA[:, 19968:22016].rearrange("p (c n) -> p c n", n=1024)
    o_mla_sb = regA[:, 0:8192].rearrange("p (i n) -> p i n", n=512)
    o_dil_sb = regA[:, 8192:16384].rearrange("p (i n) -> p i n", n=512)
    QT = regB[:, 0:16384].rearrange("p (h t) -> p h t", t=S)
    KT = regB[:, 16384:32768].rearrange("p (h t) -> p h t", t=S)
    QdT = regB[:, 32768:40960].rearrange("p (h t) -> p h t", t=S)
    KdT = regB[:, 40960:49152].rearrange("p (h t) -> p h t", t=S)
    Vaug = regB[:, 49152:57472].rearrange("p (i h d) -> p i h d", h=8, d=65)
    Vdaug = regB[:, 57472:65792].rearrange("p (i h d) -> p i h d", h=8, d=65)
    w_out_sb = regB[:, 0:8192].rearrange("p (c n) -> p c n", n=D)
    w_pq_sb = regB[:, 8192:16384].rearrange("p (c n) -> p c n", n=D)
    w_pg_sb = regB[:, 16384:24576].rearrange("p (c n) -> p c n", n=D)
    w_pp_sb = regB[:, 24576:26624].rearrange("p (c n) -> p c n", n=D)
    NG = 8
    ubufs = [regB[:, 26624 + k * 2048: 26624 + (k + 1) * 2048].bitcast(F32) for k in range(NG)]
    vbufs = [regB[:, 43008 + k * 2048: 43008 + (k + 1) * 2048].bitcast(F32) for k in range(NG)]
    gffn_bc = regB[:, 59392:61440].bitcast(F32)
    gfin_bc = regB[:, 61440:63488].bitcast(F32)

    ident = sb("ident", [128, 128], BF16)
    identf = sb("identf", [128, 128], F32)
    gcols = sb("gcols", [128, 37], F32)
    p1w = sb("p1w", [128, 9408], BF16)
    ropem = p1w[:, 7872:8896].bitcast(F32).rearrange("p (i a k) -> p i a k", a=2, k=16)
    roped = p1w[:, 8896:9408].bitcast(F32).rearrange("p (i a k) -> p i a k", a=2, k=8)
    dmask = p1w[:, 4928:7872]
    iota16 = sb("iota16", [128, 16], F32)
    keysT = sb("keysT", [128, 128], BF16)
    xt = [sb(f"xt{k}", [128, D], F32) for k in range(2)]
    junk = sb("junk", [128, D], BF16)
    hn_bf = sb("hn_bf", [128, D], BF16)
    hnT = sb("hnT", [128, 8, 128], BF16)
    st = sb("st", [128, 16], F32)
    cn_bf = p1w[:, 0:640]
    cnT = p1w[:, 640:1280].rearrange("p (c t) -> p c t", t=128)
    q_bf = p1w[:, 1280:2048].rearrange("p (h d) -> p h d", d=96)
    k_bf = p1w[:, 2048:2816].rearrange("p (h d) -> p h d", d=96)
    krope = p1w[:, 2816:2880].bitcast(F32)
    qd_bf = p1w[:, 2880:3392]
    kd_bf = p1w[:, 3392:3904]
    rt = p1w[:, 3904:4928].bitcast(F32).rearrange("p (a h k) -> p a h k", a=4, h=8)

    pf = [ps(f"pf{k}", [128, 512], F32) for k in range(6)]
    pb = [ps(f"pb{k}", [128, 1024], BF16) for k in range(2)]

    B = {}

    def buf(name, dma=False):
        if name not in B:
            B[name] = Buf(name, dma)
        return B[name]

    pf_ring = Ring([(pf[k], buf(f"pf{k}")) for k in range(6)])
    pb_ring = Ring([(pb[k], buf(f"pb{k}")) for k in range(2)])

    def dma(eng, out, in_, sbuf_buf, is_load, extra_reads=(), extra_writes=()):
        sbuf_buf.dma = True
        if is_load:
            return P.op(eng, lambda h: h.dma_start(out=out, in_=in_), reads=extra_reads,
                        writes=(sbuf_buf,) + tuple(extra_writes), dma_buf=sbuf_buf)
        return P.op(eng, lambda h: h.dma_start(out=out, in_=in_), reads=(sbuf_buf,) + tuple(extra_reads),
                    writes=extra_writes, dma_buf=sbuf_buf)

    def V(fn, reads=(), writes=()):
        return P.op("vector", fn, reads, writes)

    def A(fn, reads=(), writes=()):
        return P.op("scalar", fn, reads, writes)

    def G(fn, reads=(), writes=()):
        return P.op("gpsimd", fn, reads, writes)

    def T(fn, reads=(), writes=()):
        return P.op("tensor", fn, reads, writes)

    def transposes(src_ap_fn, n, rows, src_buf, dst_ap, dst_buf, evac="vector"):
        pt, pbuf = pb_ring.next()
        ptv = pt[:].rearrange("p (c t) -> p c t", t=128)
        for c in range(n):
            T(lambda h, c=c: h.transpose(out=ptv[0:rows, c, :], in_=src_ap_fn(c), identity=ident[:]),
              reads=(src_buf, buf("ident")), writes=(pbuf,))
        if evac == "vector":
            V(lambda h: h.tensor_copy(out=dst_ap, in_=ptv[0:rows, 0:n, :]), reads=(pbuf,), writes=(dst_buf,))
        else:
            A(lambda h: h.activation(out=dst_ap, in_=ptv[0:rows, 0:n, :], func=AF.Identity), reads=(pbuf,), writes=(dst_buf,))

    def rstd_from_ss(ss_ap, out_ap, n, bname):
        A(lambda h: h.activation(out=out_ap, in_=ss_ap, func=AF.Sqrt, bias=EPS, scale=1.0 / n),
          reads=(buf(bname),), writes=(buf(bname),))
        P.lag(LAG)
        V(lambda h: h.reciprocal(out=out_ap, in_=out_ap), reads=(buf(bname),), writes=(buf(bname),))
        return buf(bname)

    dma("sync", identf[:], ident_d[:, :], buf("identf"), True)
    dma("sync", gcols[:], gcols_d[:, :], buf("gcols"), True)
    dma("sync", ropem[:], ropem_d[:], buf("ropem"), True)
    dma("sync", roped[:], roped_d[:], buf("roped"), True)
    if phases >= 3:
        dma("sync", iota16[:], iota_d[:, :], buf("iota16"), True)
    dma("gpsimd", ident[:], ident_d[:, :], buf("ident"), True)
    if LVL >= 1:
        dma("gpsimd", dmask[:, 0:1472], mask_d[:, 0:1472], buf("dmask"), True)
        dma("gpsimd", dmask[:, 1472:2944], mask_d[:, 1472:2944], buf("dmask"), True)
    for c in range(8):
        if LVL < 2:
            break
        for hlf in range(2):
            cs = slice(hlf * 1104, (hlf + 1) * 1104)
            dma("gpsimd", w_in_sb[:, c, cs], w_in_d[c * 128:(c + 1) * 128, cs], buf(f"w_in{c}"), True)
        if LVL < 3:
            continue
        V(lambda h, c=c: h.tensor_scalar(out=w_in_sb[:, c, :], in0=w_in_sb[:, c, :], scalar1=gcols[:, c:c + 1],
                                         scalar2=None, op0=ALU.mult),
          reads=(buf(f"w_in{c}"), buf("gcols")), writes=(buf(f"w_in{c}"),))
    for c in range(3):
        if LVL < 4:
            break
        dma("gpsimd", w_uq_sb[:, c, :], w_uq_d[c * 128:(c + 1) * 128, :], buf(f"w_uq{c}"), True)
        V(lambda h, c=c: h.tensor_scalar(out=w_uq_sb[:, c, :], in0=w_uq_sb[:, c, :], scalar1=gcols[:, 8 + c:9 + c],
                                         scalar2=None, op0=ALU.mult),
          reads=(buf(f"w_uq{c}"), buf("gcols")), writes=(buf(f"w_uq{c}"),))
    for c in range(2):
        if LVL < 4:
            break
        dma("gpsimd", w_ukv_sb[:, c, :], w_ukv_d[c * 128:(c + 1) * 128, :], buf(f"w_ukv{c}"), True)
        V(lambda h, c=c: h.tensor_scalar(out=w_ukv_sb[:, c, :], in0=w_ukv_sb[:, c, :], scalar1=gcols[:, 11 + c:12 + c],
                                         scalar2=None, op0=ALU.mult),
          reads=(buf(f"w_ukv{c}"), buf("gcols")), writes=(buf(f"w_ukv{c}"),))
    if debug and LVL >= 5:
        V(lambda h: h.memset(regB[:, 0:32896], 0.0), writes=(buf("dbgms"),))
        V(lambda h: h.memset(regB[:, 32896:65792], 0.0),
          writes=tuple(buf(f"{n}{i}") for n in ("QT", "KT", "QdT", "KdT", "Vm", "Vd") for i in range(NT)) + (buf("Vaug_ones"), buf("Vdaug_ones")))
    if LVL >= 6:
        V(lambda h: h.memset(Vaug[:, :, :, 64:65], 1.0), writes=(buf("Vaug_ones"),))
        V(lambda h: h.memset(Vdaug[:, :, :, 64:65], 1.0), writes=(buf("Vdaug_ones"),))

    if phases >= 3:
        for (src, c0) in ((u_d, 0), (v_d, D)):
            for k in range(8):
                rs = slice(k * 2048, (k + 1) * 2048)
                dma("gpsimd", uv16_d[rs, c0:c0 + D], src[rs, :], buf("uv16"), True)
    MLA_SCALE = 96.0 ** -0.5
    w_in_bufs = tuple(buf(f"w_in{c}") for c in range(8))

    def phase1_tile(i):
        tsl = slice(i * 128, (i + 1) * 128)
        xb = xt[i % 2]
        xbuf = buf(f"xt{i % 2}")
        dma("sync", xb[:], x_d[tsl, :], xbuf, True)
        A(lambda h, xb=xb: h.activation(out=junk[:], in_=xb[:], func=AF.Square, accum_out=st[:, 0:1]),
          reads=(xbuf,), writes=(buf("junk"), buf("ssx")))
        rb = rstd_from_ss(st[:, 0:1], st[:, 0:1], D, "ssx")
        V(lambda h, xb=xb: h.tensor_scalar(out=hn_bf[:], in0=xb[:], scalar1=st[:, 0:1], scalar2=None, op0=ALU.mult),
          reads=(xbuf, rb), writes=(buf("hn_bf"),))
        transposes(lambda c: hn_bf[:, c * 128:(c + 1) * 128], 8, 128, buf("hn_bf"), hnT[:], buf("hnT"))

        if TL < 1:
            return
        def proj_group(c0, c1):
            pt, pbuf = pf_ring.next()
            for c in range(8):
                T(lambda h, c=c, pt=pt: h.matmul(pt[:, 0:c1 - c0], lhsT=hnT[:, c, :], rhs=w_in_sb[:, c, c0:c1],
                                                 start=(c == 0), stop=(c == 7)),
                  reads=(buf("hnT"), w_in_bufs[c]), writes=(pbuf,))
            return pt, pbuf

        pt, pbuf = proj_group(0, 384)
        A(lambda h, pt=pt: h.activation(out=junk[:, 0:384], in_=pt[:, 0:384], func=AF.Square, accum_out=st[:, 1:2]),
          reads=(pbuf,), writes=(buf("junk"), buf("ssq")))
        rq = rstd_from_ss(st[:, 1:2], st[:, 1:2], 384, "ssq")
        V(lambda h, pt=pt: h.tensor_scalar(out=cn_bf[:, 0:384], in0=pt[:, 0:384], scalar1=st[:, 1:2], scalar2=None,
                                           op0=ALU.mult),
          reads=(pbuf, rq), writes=(buf("cn_q"),))
        if TL < 2:
            return
        pt, pbuf = proj_group(384, 672)
        A(lambda h, pt=pt: h.activation(out=junk[:, 0:256], in_=pt[:, 0:256], func=AF.Square, accum_out=st[:, 2:3]),
          reads=(pbuf,), writes=(buf("junk"), buf("sskv")))
        rkv = rstd_from_ss(st[:, 2:3], st[:, 2:3], 256, "sskv")
        V(lambda h, pt=pt: h.tensor_scalar(out=cn_bf[:, 384:640], in0=pt[:, 0:256], scalar1=st[:, 2:3], scalar2=None,
                                           op0=ALU.mult),
          reads=(pbuf, rkv), writes=(buf("cn_kv"),))
        cm = ropem[:, i, 0, :]
        sm = ropem[:, i, 1, :]
        V(lambda h, pt=pt: h.tensor_tensor(out=krope[:, 0:16], in0=pt[:, 256:272], in1=cm, op=ALU.mult),
          reads=(pbuf, buf("ropem")), writes=(buf("krope_a"),))
        V(lambda h, pt=pt: h.tensor_tensor(out=rt[:, 0, 0, :], in0=pt[:, 272:288], in1=sm, op=ALU.mult),
          reads=(pbuf, buf("ropem")), writes=(buf("rt0"),))
        V(lambda h: h.tensor_tensor(out=krope[:, 0:16], in0=krope[:, 0:16], in1=rt[:, 0, 0, :], op=ALU.subtract),
          reads=(buf("krope_a"), buf("rt0")), writes=(buf("krope_a"),))
        V(lambda h, pt=pt: h.tensor_tensor(out=krope[:, 16:32], in0=pt[:, 272:288], in1=cm, op=ALU.mult),
          reads=(pbuf, buf("ropem")), writes=(buf("krope_b"),))
        V(lambda h, pt=pt: h.tensor_tensor(out=rt[:, 1, 0, :], in0=pt[:, 256:272], in1=sm, op=ALU.mult),
          reads=(pbuf, buf("ropem")), writes=(buf("rt1"),))
        V(lambda h: h.tensor_tensor(out=krope[:, 16:32], in0=krope[:, 16:32], in1=rt[:, 1, 0, :], op=ALU.add),
          reads=(buf("krope_b"), buf("rt1")), writes=(buf("krope_b"),))
        V(lambda h: h.tensor_copy(out=k_bf[:, :, 64:96], in_=krope[:].unsqueeze(1).to_broadcast([128, 8, 32])),
          reads=(buf("krope_a"), buf("krope_b")), writes=(buf("k_bf_r"),))

        if TL < 3:
            return
        def dil_rope(pt, pbuf, dst, dname, scale):
            cd = roped[:, i, 0, :].unsqueeze(1).to_broadcast([128, 8, 8])
            sd = roped[:, i, 1, :].unsqueeze(1).to_broadcast([128, 8, 8])
            pv = pt[:].rearrange("p (h d) -> p h d", d=64)
            dv = dst[:].rearrange("p (h d) -> p h d", d=64)
            x1 = pv[:, :, 0:8]
            x2 = pv[:, :, 8:16]
            t0 = rt[:, 0, :, 0:8]
            t1 = rt[:, 1, :, 0:8]
            t2 = rt[:, 2, :, 0:8]
            t3 = rt[:, 3, :, 0:8]
            V(lambda h: h.tensor_tensor(out=t0, in0=x1, in1=cd, op=ALU.mult), reads=(pbuf, buf("roped")), writes=(buf("rt0"),))
            V(lambda h: h.tensor_tensor(out=t1, in0=x2, in1=sd, op=ALU.mult), reads=(pbuf, buf("roped")), writes=(buf("rt1"),))
            V(lambda h: h.tensor_tensor(out=t2, in0=x2, in1=cd, op=ALU.mult), reads=(pbuf, buf("roped")), writes=(buf("rt2"),))
            V(lambda h: h.tensor_tensor(out=t3, in0=x1, in1=sd, op=ALU.mult), reads=(pbuf, buf("roped")), writes=(buf("rt3"),))
            V(lambda h: h.tensor_tensor(out=t0, in0=t0, in1=t1, op=ALU.subtract),
              reads=(buf("rt0"), buf("rt1")), writes=(buf("rt0"),))
            V(lambda h: h.tensor_tensor(out=t2, in0=t2, in1=t3, op=ALU.add),
              reads=(buf("rt2"), buf("rt3")), writes=(buf("rt2"),))
            V(lambda h: h.tensor_scalar(out=dv[:, :, 0:8], in0=t0, scalar1=scale, scalar2=None, op0=ALU.mult),
              reads=(buf("rt0"),), writes=(buf(dname + "a"),))
            V(lambda h: h.tensor_scalar(out=dv[:, :, 8:16], in0=t2, scalar1=scale, scalar2=None, op0=ALU.mult),
              reads=(buf("rt2"),), writes=(buf(dname + "b"),))
            V(lambda h: h.tensor_scalar(out=dv[:, :, 16:64], in0=pv[:, :, 16:64], scalar1=scale, scalar2=None, op0=ALU.mult),
              reads=(pbuf,), writes=(buf(dname + "c"),))
            return (buf(dname + "a"), buf(dname + "b"), buf(dname + "c"))

        pt, pbuf = proj_group(672, 1184)
        qd_bufs = dil_rope(pt, pbuf, qd_bf, "qd_bf", 0.125)
        if TL < 3.3:
            return
        pt, pbuf = proj_group(1184, 1696)
        kd_bufs = dil_rope(pt, pbuf, kd_bf, "kd_bf", 1.0)
        if TL < 3.6:
            return
        pt, pbuf = proj_group(1696, 2208)
        A(lambda h, pt=pt: h.activation(out=Vdaug[:, i, :, 0:64], in_=pt[:].rearrange("p (h d) -> p h d", d=64), func=AF.Identity),
          reads=(pbuf,), writes=(buf(f"Vd{i}"),))

        if TL < 4:
            return
        pt_, pbuf_ = pb_ring.next()
        ptv = pt_[:].rearrange("p (c t) -> p c t", t=128)
        for c in range(4):
            T(lambda h, c=c, ptv=ptv: h.transpose(out=ptv[:, c, :], in_=qd_bf[:, c * 128:(c + 1) * 128], identity=ident[:]),
              reads=qd_bufs + (buf("ident"),), writes=(pbuf_,))
        for c in range(4):
            T(lambda h, c=c, ptv=ptv: h.transpose(out=ptv[:, 4 + c, :], in_=kd_bf[:, c * 128:(c + 1) * 128], identity=ident[:]),
              reads=kd_bufs + (buf("ident"),), writes=(pbuf_,))
        V(lambda h, ptv=ptv: h.tensor_copy(out=QdT[:, :, tsl], in_=ptv[:, 0:4, :]), reads=(pbuf_,), writes=(buf(f"QdT{i}"),))
        A(lambda h, ptv=ptv: h.activation(out=KdT[:, :, tsl], in_=ptv[:, 4:8, :], func=AF.Identity), reads=(pbuf_,), writes=(buf(f"KdT{i}"),))

        if TL < 5:
            return
        pt_, pbuf_ = pb_ring.next()
        ptv = pt_[:].rearrange("p (c t) -> p c t", t=128)
        for c in range(5):
            T(lambda h, c=c, ptv=ptv: h.transpose(out=ptv[:, c, :], in_=cn_bf[:, c * 128:(c + 1) * 128], identity=ident[:]),
              reads=(buf("cn_q") if c < 3 else buf("cn_kv"), buf("ident")), writes=(pbuf_,))
        V(lambda h, ptv=ptv: h.tensor_copy(out=cnT[:], in_=ptv[:, 0:5, :]), reads=(pbuf_,), writes=(buf("cnT"),))

        for (h0, h1) in ((0, 5), (5, 8)):
            c0, c1 = h0 * 96, h1 * 96
            nh = h1 - h0
            pt, pbuf = pf_ring.next()
            for c in range(3):
                T(lambda h, c=c, pt=pt, c0=c0, c1=c1: h.matmul(pt[:, 0:c1 - c0], lhsT=cnT[:, c, :], rhs=w_uq_sb[:, c, c0:c1],
                                                                start=(c == 0), stop=(c == 2)),
                  reads=(buf("cnT"), buf(f"w_uq{c}")), writes=(pbuf,))
            pv = pt[:, 0:nh * 96].rearrange("p (h d) -> p h d", d=96)
            qv = q_bf[:, h0:h1, :]
            cmb = ropem[:, i, 0, :].unsqueeze(1).to_broadcast([128, nh, 16])
            smb = ropem[:, i, 1, :].unsqueeze(1).to_broadcast([128, nh, 16])
            x1 = pv[:, :, 64:80]
            x2 = pv[:, :, 80:96]
            t0 = rt[:, 0, 0:nh, :]
            t1 = rt[:, 1, 0:nh, :]
            t2 = rt[:, 2, 0:nh, :]
            t3 = rt[:, 3, 0:nh, :]
            V(lambda h, pv=pv, qv=qv: h.tensor_scalar(out=qv[:, :, 0:64], in0=pv[:, :, 0:64], scalar1=MLA_SCALE, scalar2=None, op0=ALU.mult),
              reads=(pbuf,), writes=(buf(f"q_bf_n{h0}"),))
            V(lambda h, t0=t0, x1=x1, cmb=cmb: h.tensor_tensor(out=t0, in0=x1, in1=cmb, op=ALU.mult),
              reads=(pbuf, buf("ropem")), writes=(buf("rt0"),))
            V(lambda h, t1=t1, x2=x2, smb=smb: h.tensor_tensor(out=t1, in0=x2, in1=smb, op=ALU.mult),
              reads=(pbuf, buf("ropem")), writes=(buf("rt1"),))
            V(lambda h, t2=t2, x2=x2, cmb=cmb: h.tensor_tensor(out=t2, in0=x2, in1=cmb, op=ALU.mult),
              reads=(pbuf, buf("ropem")), writes=(buf("rt2"),))
            V(lambda h, t3=t3, x1=x1, smb=smb: h.tensor_tensor(out=t3, in0=x1, in1=smb, op=ALU.mult),
              reads=(pbuf, buf("ropem")), writes=(buf("rt3"),))
            V(lambda h, t0=t0, t1=t1: h.tensor_tensor(out=t0, in0=t0, in1=t1, op=ALU.subtract),
              reads=(buf("rt0"), buf("rt1")), writes=(buf("rt0"),))
            V(lambda h, t2=t2, t3=t3: h.tensor_tensor(out=t2, in0=t2, in1=t3, op=ALU.add),
              reads=(buf("rt2"), buf("rt3")), writes=(buf("rt2"),))
            V(lambda h, t0=t0, qv=qv: h.tensor_scalar(out=qv[:, :, 64:80], in0=t0, scalar1=MLA_SCALE, scalar2=None, op0=ALU.mult),
              reads=(buf("rt0"),), writes=(buf(f"q_bf_a{h0}"),))
            V(lambda h, t2=t2, qv=qv: h.tensor_scalar(out=qv[:, :, 80:96], in0=t2, scalar1=MLA_SCALE, scalar2=None, op0=ALU.mult),
              reads=(buf("rt2"),), writes=(buf(f"q_bf_b{h0}"),))
        q_bufs = tuple(buf(f"q_bf_{s}{h0}") for s in "nab" for h0 in (0, 5))
        if TL < 6:
            return
        for gidx in range(2):
            pt, pbuf = pf_ring.next()
            for c in range(2):
                T(lambda h, c=c, pt=pt, gidx=gidx: h.matmul(pt[:, :], lhsT=cnT[:, 3 + c, :],
                                                            rhs=w_ukv_sb[:, c, gidx * 512:(gidx + 1) * 512],
                                                            start=(c == 0), stop=(c == 1)),
                  reads=(buf("cnT"), buf(f"w_ukv{c}")), writes=(pbuf,))
            pv = pt[:].rearrange("p (h d) -> p h d", d=128)
            A(lambda h, pv=pv, gidx=gidx: h.activation(out=k_bf[:, gidx * 4:(gidx + 1) * 4, 0:64], in_=pv[:, :, 0:64], func=AF.Identity),
              reads=(pbuf,), writes=(buf(f"k_bf_n{gidx}"),))
            V(lambda h, pv=pv, gidx=gidx: h.tensor_copy(out=Vaug[:, i, gidx * 4:(gidx + 1) * 4, 0:64], in_=pv[:, :, 64:128]),
              reads=(pbuf,), writes=(buf(f"Vm{i}"),))
        k_bufs = (buf("k_bf_n0"), buf("k_bf_n1"), buf("k_bf_r"))
        if TL < 7:
            return
        for (src, sbufs, dst, dname, ev) in ((q_bf, q_bufs, QT, "QT", "vector"), (k_bf, k_bufs, KT, "KT", "scalar")):
            pt_, pbuf_ = pb_ring.next()
            ptv = pt_[:].rearrange("p (c t) -> p c t", t=128)
            for hh in range(8):
                T(lambda h, hh=hh, ptv=ptv, src=src: h.transpose(out=ptv[0:96, hh, :], in_=src[:, hh, :], identity=ident[:]),
                  reads=sbufs + (buf("ident"),), writes=(pbuf_,))
            if ev == "vector":
                V(lambda h, ptv=ptv, dst=dst: h.tensor_copy(out=dst[0:96, :, tsl], in_=ptv[0:96, :, :]),
                  reads=(pbuf_,), writes=(buf(f"{dname}{i}"),))
            else:
                A(lambda h, ptv=ptv, dst=dst: h.activation(out=dst[0:96, :, tsl], in_=ptv[0:96, :, :], func=AF.Identity),
                  reads=(pbuf_,), writes=(buf(f"{dname}{i}"),))

    if STOP == "weights":
        dbg["w"] = dout("dbg_w", [128, IN_W], BF16)
        if LVL >= 2:
            tok = P.op("sync", lambda h: h.dma_start(out=dbg["w"][:, :], in_=w_in_sb[:, 0, :]),
                       reads=(buf("w_in0"),), writes=(), dma_buf=buf("dbgw"))
        else:
            tok = P.op("sync", lambda h: h.dma_start(out=dbg["w"][:, 0:128], in_=ident[:, :]),
                       reads=(buf("ident"),), writes=(), dma_buf=buf("dbgw"))
        P.final_tokens.append(tok)
    else:
        for i in range(NT if NT_RUN is None else NT_RUN):
            phase1_tile(i)

    if debug and phases == 1 and STOP is None:
        allb = [buf(f"{n}{i}") for n in ("QT", "KT", "QdT", "KdT", "Vm", "Vd") for i in range(NT if NT_RUN is None else NT_RUN)]
        for nm, ap, shp in (("QT", QT[0:96], [96, 8, S]), ("KT", KT[0:96], [96, 8, S]), ("QdT", QdT[:], [128, 4, S]),
                            ("KdT", KdT[:], [128, 4, S]), ("Vaug", Vaug[:], [128, NT, 8, 65]), ("Vdaug", Vdaug[:], [128, NT, 8, 65])):
            if DBG_ONLY and nm not in DBG_ONLY:
                continue
            dbg[nm] = dout("dbg_" + nm, shp, BF16)
            tok = P.op("sync", lambda h, nm=nm, ap=ap: h.dma_start(out=dbg[nm][:], in_=ap),
                       reads=tuple(allb) + (buf("Vaug_ones"), buf("Vdaug_ones")), writes=(), dma_buf=buf("dbg" + nm))
            P.final_tokens.append(tok)

    def alias(new_names, old_bufs):
        toks = []
        for ob in old_bufs:
            if ob.w is not None:
                toks.append(ob.w)
            toks.extend(ob.r)
        for n in new_names:
            nb = buf(n)
            nb.r = list(nb.r) + toks

    def mm(out, lhsT, rhs, start, stop, reads, writes, skip=False):
        if skip:
            T(lambda h: h.matmul(out, lhsT=lhsT, rhs=rhs, start=start, stop=stop, skip_group_check=True), reads, writes)
        else:
            T(lambda h: h.matmul(out, lhsT=lhsT, rhs=rhs, start=start, stop=stop), reads, writes)

    def act(out, in_, func, reads, writes, bias=None, scale=None, accum=None):
        kw = {}
        if bias is not None:
            kw["bias"] = bias
        if scale is not None:
            kw["scale"] = scale
        if accum is not None:
            kw["accum_out"] = accum
        A(lambda h: h.activation(out=out, in_=in_, func=func, **kw), reads, writes)

    def vtt(out, in0, in1, op, reads, writes):
        V(lambda h: h.tensor_tensor(out=out, in0=in0, in1=in1, op=op), reads, writes)

    def vts(out, in0, s1, op0, reads, writes, s2=None, op1=None):
        if op1 is None:
            V(lambda h: h.tensor_scalar(out=out, in0=in0, scalar1=s1, scalar2=None, op0=op0), reads, writes)
        else:
            V(lambda h: h.tensor_scalar(out=out, in0=in0, scalar1=s1, scalar2=s2, op0=op0, op1=op1), reads, writes)

    def vcopy(out, in_, reads, writes):
        V(lambda h: h.tensor_copy(out=out, in_=in_), reads, writes)

    def vrecip(out, in_, reads, writes):
        V(lambda h: h.reciprocal(out=out, in_=in_), reads, writes)

    if phases >= 2:
        p1_work = [buf(n) for n in ("xt0", "xt1", "junk", "hn_bf", "hnT")]
        w_bufs = [buf(f"w_in{c}") for c in range(8)] + [buf(f"w_uq{c}") for c in range(3)] + [buf(f"w_ukv{c}") for c in range(2)]
        pTs = [xt[0][:, k * 256:(k + 1) * 256].bitcast(BF16) for k in range(4)]
        alias([f"pT{k}" for k in range(4)] + ["rc"], p1_work)
        alias([f"o_mla{i}" for i in range(NT)] + [f"o_dil{i}" for i in range(NT)], w_bufs)
        pT_ring = Ring([(pTs[k], buf(f"pT{k}")) for k in range(4)])
        s_ring = Ring([(pf[k], buf(f"pf{k}")) for k in range(3)])
        acc_ring = Ring([(pf[3 + k], buf(f"pf{3 + k}")) for k in range(2)])
        rc = xt[1][:, 0:8]

        def attention(kind):
            for hd in range(8):
                if kind == "mla":
                    Qop = QT[0:96, hd, :]
                    Kop = KT[0:96, hd, :]
                    Vt = Vaug
                    qbufs = [buf(f"QT{i}") for i in range(NT)]
                    kbufs = [buf(f"KT{i}") for i in range(NT)]
                    vbufs_ = [buf(f"Vm{i}") for i in range(NT)]
                    vones = buf("Vaug_ones")
                    o_sb, oname = o_mla_sb, "o_mla"
                else:
                    r0 = (hd % 2) * 64
                    Qop = QdT[r0:r0 + 64, hd // 2, :]
                    Kop = KdT[r0:r0 + 64, hd // 2, :]
                    Vt = Vdaug
                    qbufs = [buf(f"QdT{i}") for i in range(NT)]
                    kbufs = [buf(f"KdT{i}") for i in range(NT)]
                    vbufs_ = [buf(f"Vd{i}") for i in range(NT)]
                    vones = buf("Vdaug_ones")
                    o_sb, oname = o_dil_sb, "o_dil"
                for qb in range(4):
                    acc, accb = acc_ring.next()
                    accv = acc[:, 0:264].rearrange("p (q d) -> p q d", d=66)
                    kts = []
                    for kt in range(NT):
                        d0 = qb * 512 - kt * 128
                        if kind == "dil" and (d0 < -1408 or d0 > 1024):
                            continue
                        kts.append(kt)
                    state = {"first": True}

                    def pv(kt, pT, pTb, last):
                        for qt in range(4):
                            st_flag = state["first"]
                            mm(accv[:, qt, 0:65], pT[:, qt * 128:(qt + 1) * 128], Vt[:, kt, hd, 0:65],
                               st_flag, last and qt == 3, (pTb, vbufs_[kt], vones), (accb,))
                            state["first"] = False

                    prev = None
                    for kt in kts:
                        sp, spb = s_ring.next()
                        mm(sp[:, 0:512], Kop[:, kt * 128:(kt + 1) * 128], Qop[:, qb * 512:(qb + 1) * 512], True, True,
                           (kbufs[kt],) + tuple(qbufs[qb * 4:(qb + 1) * 4]), (spb,))
                        pT, pTb = pT_ring.next()
                        act(pT, sp[:, 0:512], AF.Exp, (spb,), (pTb,))
                        if kind == "dil":
                            base = qb * 512 - kt * 128 + 1408
                            vtt(pT, pT, dmask[:, base:base + 512], ALU.mult, (pTb, buf("dmask")), (pTb,))
                        if prev is not None:
                            pv(*prev, False)
                        prev = (kt, pT, pTb)
                    pv(*prev, True)
                    vrecip(rc[:, 0:4], accv[:, :, 64], (accb,), (buf("rc"),))
                    obufs = tuple(buf(f"{oname}{qb * 4 + qt}") for qt in range(4))
                    vtt(o_sb[:, qb * 4:(qb + 1) * 4, hd * 64:(hd + 1) * 64], accv[:, :, 0:64],
                        rc[:, 0:4].unsqueeze(2).to_broadcast([128, 4, 64]), ALU.mult, (accb, buf("rc")), obufs)

        attention("mla")
        attention("dil")

    if debug and phases == 2:
        for nm, ap in (("o_mla", o_mla_sb), ("o_dil", o_dil_sb)):
            dbg[nm] = dout("dbg_" + nm, [128, NT, 512], BF16)
            tok = P.op("sync", lambda h, nm=nm, ap=ap: h.dma_start(out=dbg[nm][:], in_=ap[:]),
                       reads=tuple(buf(f"{nm}{i}") for i in range(NT)), writes=(), dma_buf=buf("dbg" + nm))
            P.final_tokens.append(tok)

    if phases >= 3:
        qkv_bufs = [buf(f"{n}{i}") for n in ("QT", "KT", "QdT", "KdT", "Vm", "Vd") for i in range(NT)] + \
                   [buf("Vaug_ones"), buf("Vdaug_ones")]
        NGB = 13
        uvbufs = [regB[:, 26624 + k * 2048: 26624 + (k + 1) * 2048] for k in range(8)] + [regA[:, 16384:18432]] + \
                 [p1w[:, k * 2048:(k + 1) * 2048] for k in range(4)]
        diagt = sb("diagt", [128, 4, 128], BF16)
        diag_ring = Ring([(diagt[:, k, :], buf(f"diag{k}")) for k in range(4)])
        gffn_bc3 = regB[:, 43008:45056].bitcast(F32)
        gfin_bc3 = regB[:, 45056:47104].bitcast(F32)
        hbuf = regB[:, 47104:49152].bitcast(F32)
        xn2a = regB[:, 49152:51200].bitcast(F32)
        xn2b = regA[:, 18432:20480].bitcast(F32)
        gates2 = regA[:, 20480:20736].bitcast(F32).rearrange("p (h k) -> p h k", h=8)
        gelu_t = sb("gelu_t", [128, 128], F32)
        ybuf = regB[:, 51200:53248].bitcast(F32)
        Ssc = regB[:, 53248:57344].bitcast(F32)
        cand = regB[:, 57344:59392].bitcast(F32)
        oh = regB[:, 59392:61440].bitcast(F32)
        gate_sb = regB[:, 61440:62464].bitcast(F32)
        qTp = regB[:, 62464:63488].rearrange("p (h t) -> p h t", t=128)
        v12 = regB[:, 63488:64000].bitcast(F32).rearrange("p (a h k) -> p a h k", a=2, h=8)
        i12 = regB[:, 64000:64512].bitcast(U32).rearrange("p (a h k) -> p a h k", a=2, h=8)
        i12f = regB[:, 64512:65024].bitcast(F32).rearrange("p (a h k) -> p a h k", a=2, h=8)
        wk = regB[:, 65024:65536].bitcast(F32)
        top = regB[:, 65536:65792].bitcast(F32).rearrange("p (h k) -> p h k", h=8)
        x0 = xt[0]
        pos = x0[:, 0:128].bitcast(U32).rearrange("p (h k) -> p h k", h=8)
        ai = x0[:, 128:256].bitcast(U32).rearrange("p (h k) -> p h k", h=8)
        bi = x0[:, 256:384].bitcast(U32).rearrange("p (h k) -> p h k", h=8)
        af = x0[:, 384:512].rearrange("p (h k) -> p h k", h=8)
        bf = x0[:, 512:640].rearrange("p (h k) -> p h k", h=8)
        i1sel = x0[:, 640:768].rearrange("p (h k) -> p h k", h=8)
        i2sel = x0[:, 768:896].rearrange("p (h k) -> p h k", h=8)
        eidxf = x0[:, 896:1024]
        x1_ = xt[1]
        eidxi = x1_[:, 0:128].bitcast(I32)
        gates = x1_[:, 128:256].rearrange("p (h k) -> p h k", h=8)
        avals = x1_[:, 256:384]
        coef = x1_[:, 384:512]
        ptile = x1_[:, 512:768]
        p_bf = x1_[:, 768:896].bitcast(BF16)
        pTt = x1_[:, 896:1024].bitcast(BF16).rearrange("p (c t) -> p c t", t=128)
        keys_bf = p1w[:, 8192:8320]

        p3_names = ["w_out%d" % c for c in range(8)] + ["w_pq%d" % c for c in range(8)] + ["w_pg%d" % c for c in range(8)] + \
                   ["w_pp0", "w_pp1", "gffn", "gfin", "hbuf0", "xn20", "hbuf1", "Ssc", "cand", "oh", "gate_sb", "qTp",
                    "v12", "i12", "i12f", "wk", "top"] + [f"ohs{k}" for k in range(8)] + [f"v12lo{j}" for j in range(16)] + \
                   [f"v12hi{j}" for j in range(16)] + [f"i12_{j}" for j in range(16)] + [f"toplo{h}" for h in range(8)] + [f"tophi{h}" for h in range(8)] + [f"uvb{k}" for k in range(8)]
        alias(p3_names, qkv_bufs)
        alias(["uvb8", "xn21", "gates1"], w_bufs)
        x0_names = [f"pos{h}" for h in range(8)] + ["ai", "bi", "af", "bf", "i1sel", "i2sel", "eidxf"]
        x1_names = ["eidxi0", "gates0", "ptile", "p_bf", "pTt"] + [f"coef{j}" for j in range(128)] + [f"avals{j}" for j in range(128)]
        alias(x0_names, [buf(f"pT{k}") for k in range(4)] + [buf("xt0")])
        alias(x1_names, [buf("rc"), buf("xt1")])
        all_prev = [b_ for n_, b_ in list(B.items()) if not n_.startswith("pf") and not n_.startswith("pb")]
        alias(["keys_bf"] + [f"uvb{k}" for k in range(9, 13)], all_prev)

        for c in range(8):
            dma("gpsimd", w_out_sb[:, c, :], w_out_d[c * 128:(c + 1) * 128, :], buf(f"w_out{c}"), True)
            vts(w_out_sb[:, c, :], w_out_sb[:, c, :], gcols[:, 13 + c:14 + c], ALU.mult,
                (buf(f"w_out{c}"), buf("gcols")), (buf(f"w_out{c}"),))
        for c in range(8):
            dma("gpsimd", w_pq_sb[:, c, :], w_pq_d[c * 128:(c + 1) * 128, :], buf(f"w_pq{c}"), True)
        for c in range(8):
            dma("gpsimd", w_pg_sb[:, c, :], w_pg_d[c * 128:(c + 1) * 128, :], buf(f"w_pg{c}"), True)
            vts(w_pg_sb[:, c, :], w_pg_sb[:, c, :], gcols[:, 21 + c:22 + c], ALU.mult,
                (buf(f"w_pg{c}"), buf("gcols")), (buf(f"w_pg{c}"),))
        for c in range(2):
            dma("gpsimd", w_pp_sb[:, c, :], w_pp_d[c * 128:(c + 1) * 128, :], buf(f"w_pp{c}"), True)
        dma("sync", gffn_bc3, g_ffn_d.partition_broadcast(128), buf("gffn"), True)
        dma("sync", gfin_bc3, g_fin_d.partition_broadcast(128), buf("gfin"), True)
        dma("gpsimd", keys_bf, keys_d[:, :], buf("keys_bf"), True)
        pt_, pbuf_ = pb_ring.next()
        T(lambda h: h.transpose(out=pt_[:, 0:128], in_=keys_bf, identity=ident[:]), reads=(buf("keys_bf"), buf("ident")), writes=(pbuf_,))
        vcopy(keysT[:], pt_[:, 0:128], (pbuf_,), (buf("keysT"),))
        w_out_bufs = tuple(buf(f"w_out{c}") for c in range(8))
        w_pq_bufs = tuple(buf(f"w_pq{c}") for c in range(8))
        w_pg_bufs = tuple(buf(f"w_pg{c}") for c in range(8))
        pf3_ring = Ring([(pf[k], buf(f"pf{k}")) for k in range(2)])
        uv_ring = Ring([(uvbufs[k], buf(f"uvb{k}")) for k in range(NGB)])
        xn2s = [(xn2a, buf("xn20")), (xn2b, buf("xn21"))]
        gatess = [(gates, buf("gates0")), (gates2, buf("gates1"))]

        def vmax(out, in_, reads, writes):
            V(lambda h: h.max(out=out, in_=in_), reads, writes)

        def vmatch(out, rep, vals, reads, writes):
            V(lambda h: h.match_replace(out=out, in_to_replace=rep, in_values=vals, imm_value=-1e30), reads, writes)

        def vmaxidx(out, mx, vals, reads, writes):
            V(lambda h: h.max_index(out=out, in_max=mx, in_values=vals), reads, writes)

        def vstt(out, in0, scalar, in1, op0, op1, reads, writes, accum=None):
            if accum is None:
                V(lambda h: h.scalar_tensor_tensor(out=out, in0=in0, scalar=scalar, in1=in1, op0=op0, op1=op1), reads, writes)
            else:
                V(lambda h: h.scalar_tensor_tensor(out=out, in0=in0, scalar=scalar, in1=in1, op0=op0, op1=op1,
                                                   accum_out=accum), reads, writes)

        def vreduce(out, in_, op, reads, writes):
            V(lambda h: h.tensor_reduce(out=out, in_=in_, axis=AX.X, op=op), reads, writes)

        nhalf = sb("nhalf", [128, 2], F32)
        G(lambda h: h.memset(nhalf[:], -0.5), writes=(buf("nhalf"),))

        def norm_rstd(src_ap, col, n, src_bufs, bname):
            c = st[:, col:col + 1]
            act(junk[:, 0:n], src_ap, AF.Square, tuple(src_bufs), (buf("junk"), buf(bname)), accum=c)
            P.lag(3 * LAG)
            G(lambda h: h.tensor_scalar(out=c, in0=c, scalar1=1.0 / n, scalar2=EPS, op0=ALU.mult, op1=ALU.add),
              reads=(buf(bname),), writes=(buf(bname),))
            G(lambda h: h.tensor_tensor(out=c, in0=c, in1=nhalf[:, 0:1], op=ALU.pow),
              reads=(buf(bname), buf("nhalf")), writes=(buf(bname),))
            P.lag(LAG)
            return buf(bname)

        def gather(dst, dst_buf, table, col, tname, ei, eib):
            P.op("gpsimd", lambda h: h.indirect_dma_start(out=dst, out_offset=None, in_=table[:, :],
                                                          in_offset=bass.IndirectOffsetOnAxis(ap=ei[:, col:col + 1], axis=0)),
                 reads=(eib, buf(tname)), writes=(dst_buf,), dma_buf=dst_buf)

        hbufs = [(hbuf, buf("hbuf0")), (ybuf, buf("hbuf1"))]
        eidxi2 = sb("eidxi2", [128, 128], I32)
        eidxis = [(eidxi, buf("eidxi0")), (eidxi2[:], buf("eidxi1"))]

        def p3_pre(i):
            tsl = slice(i * 128, (i + 1) * 128)
            hb, hbb = hbufs[i % 2]
            ei, eib = eidxis[i % 2]
            xn2, xnb = xn2s[i % 2]
            gates, gtb = gatess[i % 2]
            rm = norm_rstd(o_mla_sb[:, i, :], 3, 512, (buf(f"o_mla{i}"),), "ssm")
            act(hn_bf[:, 0:512], o_mla_sb[:, i, :], AF.Identity, (buf(f"o_mla{i}"), rm), (buf("hn_bf"),), scale=st[:, 3:4])
            rd = norm_rstd(o_dil_sb[:, i, :], 4, 512, (buf(f"o_dil{i}"),), "ssd")
            act(hn_bf[:, 512:1024], o_dil_sb[:, i, :], AF.Identity, (buf(f"o_dil{i}"), rd), (buf("hn_bf"),), scale=st[:, 4:5])
            transposes(lambda c: hn_bf[:, c * 128:(c + 1) * 128], 8, 128, buf("hn_bf"), hnT[:], buf("hnT"), evac="scalar")
            dma("sync", hb, x_d[tsl, :], hbb, True)
            for cg in range(2):
                pt, pbuf = pf3_ring.next()
                for c in range(8):
                    mm(pt[:, :], hnT[:, c, :], w_out_sb[:, c, cg * 512:(cg + 1) * 512], c == 0, c == 7,
                       (buf("hnT"), w_out_bufs[c]), (pbuf,))
                P.lag(LAG)
                vtt(hb[:, cg * 512:(cg + 1) * 512], hb[:, cg * 512:(cg + 1) * 512], pt[:, :], ALU.add,
                    (hbb, pbuf), (hbb,))
            r2 = norm_rstd(hb, 5, D, (hbb,), "ss2")
            vstt(xn2, hb, st[:, 5:6], gffn_bc3, ALU.mult, ALU.mult, (hbb, r2, buf("gffn")), (xnb,))
            act(hn_bf[:], xn2, AF.Identity, (xnb,), (buf("hn_bf"),))
            transposes(lambda c: hn_bf[:, c * 128:(c + 1) * 128], 8, 128, buf("hn_bf"), hnT[:], buf("hnT"), evac="scalar")
            for g4 in range(2):
                pt, pbuf = pf3_ring.next()
                ptv = pt[:].rearrange("p (h t) -> p h t", t=128)
                for hl in range(4):
                    hd = g4 * 4 + hl
                    for c in range(8):
                        mm(ptv[:, hl, :], w_pq_sb[:, c, hd * 128:(hd + 1) * 128], hnT[:, c, :],
                           hl == 0 and c == 0, hl == 3 and c == 7, (buf("hnT"), w_pq_bufs[c]), (pbuf,))
                act(qTp[:, g4 * 4:(g4 + 1) * 4, :], ptv, AF.Identity, (pbuf,), (buf("qTp"),))
            for g4 in range(2):
                banks = [pf3_ring.next(), pf3_ring.next()]
                for hl in range(4):
                    hd = g4 * 4 + hl
                    for half in range(2):
                        pt, pbuf = banks[half]
                        r0 = half * 64
                        mm(pt[:, hl * 128:(hl + 1) * 128], qTp[r0:r0 + 64, hd, :], keysT[r0:r0 + 64, :], True, True,
                           (buf("qTp"), buf("keysT")), (pbuf,))
                for half in range(2):
                    pt, pbuf = banks[half]
                    j0 = half * 8 + g4 * 4
                    act(Ssc[:, j0 * 128:(j0 + 4) * 128], pt[:, :], AF.Identity, (pbuf,), (buf("Ssc"),))
            P.lag(2 * LAG)
            ohs = [buf(f"ohs{k}") for k in range(8)]
            v12lo = [buf(f"v12lo{j}") for j in range(16)]
            v12hi = [buf(f"v12hi{j}") for j in range(16)]
            i12b = [buf(f"i12_{j}") for j in range(16)]
            for grp in range(2):
                probs = [(grp * 8 + k, k) for k in range(8)]

                def views(j):
                    half, hd = j // 8, j % 8
                    return Ssc[:, j * 128:(j + 1) * 128], v12[:, half, hd, :], i12[:, half, hd, :]
                for (j, k) in probs:
                    Sv, vv, iv = views(j)
                    vmax(vv[:, 0:8], Sv, (buf("Ssc"),), (v12lo[j],))
                for (j, k) in probs:
                    Sv, vv, iv = views(j)
                    vmatch(oh[:, k * 128:(k + 1) * 128], vv[:, 0:8], Sv, (buf("Ssc"), v12lo[j]), (ohs[k],))
                for (j, k) in probs:
                    Sv, vv, iv = views(j)
                    vmax(vv[:, 8:16], oh[:, k * 128:(k + 1) * 128], (ohs[k],), (v12hi[j],))
                for (j, k) in probs:
                    Sv, vv, iv = views(j)
                    vmaxidx(iv[:, 0:8], vv[:, 0:8], Sv, (buf("Ssc"), v12lo[j]), (i12b[j],))
                for (j, k) in probs:
                    Sv, vv, iv = views(j)
                    vmaxidx(iv[:, 8:16], vv[:, 8:16], Sv, (buf("Ssc"), v12hi[j]), (i12b[j],))
            v12all = tuple(v12lo) + tuple(v12hi)
            vcopy(i12f[:], i12[:], tuple(i12b), (buf("i12f"),))
            vts(i12f[:, 0, :, :], i12f[:, 0, :, :], 128.0, ALU.mult, (buf("i12f"),), (buf("i12f"),))
            toplo = [buf(f"toplo{h}") for h in range(8)]
            tophi = [buf(f"tophi{h}") for h in range(8)]
            posb = [buf(f"pos{h}") for h in range(8)]
            for g4 in range(2):
                hs = slice(g4 * 4, (g4 + 1) * 4)
                candv = cand.rearrange("p (h a b) -> p h a b", h=4, a=16)
                vtt(candv, v12[:, 0, hs, :].unsqueeze(3).to_broadcast([128, 4, 16, 16]),
                    v12[:, 1, hs, :].unsqueeze(2).to_broadcast([128, 4, 16, 16]), ALU.add, v12all, (buf("cand"),))
                hds = [(g4 * 4 + hl, hl) for hl in range(4)]
                for (hd, hl) in hds:
                    vmax(top[:, hd, 0:8], cand[:, hl * 256:(hl + 1) * 256], (buf("cand"),), (toplo[hd],))
                for (hd, hl) in hds:
                    vmatch(oh[:, hl * 256:(hl + 1) * 256], top[:, hd, 0:8], cand[:, hl * 256:(hl + 1) * 256],
                           (buf("cand"), toplo[hd]), (ohs[2 * hl], ohs[2 * hl + 1]))
                for (hd, hl) in hds:
                    vmax(top[:, hd, 8:16], oh[:, hl * 256:(hl + 1) * 256], (ohs[2 * hl], ohs[2 * hl + 1]), (tophi[hd],))
                for (hd, hl) in hds:
                    vmaxidx(pos[:, hd, 0:8], top[:, hd, 0:8], cand[:, hl * 256:(hl + 1) * 256], (buf("cand"), toplo[hd]), (posb[hd],))
                for (hd, hl) in hds:
                    vmaxidx(pos[:, hd, 8:16], top[:, hd, 8:16], cand[:, hl * 256:(hl + 1) * 256], (buf("cand"), tophi[hd]), (posb[hd],))
            topall = tuple(toplo) + tuple(tophi)
            posall = tuple(posb)
            ohall = (buf("oh"),) + tuple(ohs)
            vts(ai[:], pos[:], 4, ALU.logical_shift_right, posall, (buf("ai"),))
            vts(bi[:], pos[:], 15, ALU.bitwise_and, posall, (buf("bi"),))
            vcopy(af[:], ai[:], (buf("ai"),), (buf("af"),))
            vcopy(bf[:], bi[:], (buf("bi"),), (buf("bf"),))
            iob = iota16[:].unsqueeze(1).unsqueeze(1).to_broadcast([128, 4, 16, 16])
            for (selv, srcv, half, sname, fname) in ((i1sel, af, 0, "i1sel", "af"), (i2sel, bf, 1, "i2sel", "bf")):
                for g4 in range(2):
                    hs = slice(g4 * 4, (g4 + 1) * 4)
                    ohv = oh.rearrange("p (h k a) -> p h k a", h=4, k=16)
                    vtt(ohv, iob, srcv[:, hs, :].unsqueeze(3).to_broadcast([128, 4, 16, 16]), ALU.is_equal,
                        (buf("iota16"), buf(fname)), ohall)
                    vtt(ohv, ohv, i12f[:, half, hs, :].unsqueeze(2).to_broadcast([128, 4, 16, 16]), ALU.mult,
                        ohall + (buf("i12f"),), ohall)
                    vreduce(selv[:, hs, :], ohv, ALU.add, ohall, (buf(sname),))
            vtt(eidxf, i1sel.rearrange("p h k -> p (h k)"), i2sel.rearrange("p h k -> p (h k)"), ALU.add,
                (buf("i1sel"), buf("i2sel")), (buf("eidxf"),))
            vcopy(ei, eidxf, (buf("eidxf"),), (eib,))
            vtt(gates[:], top[:], top[:, :, 0:1].to_broadcast([128, 8, 16]), ALU.subtract, topall, (gtb,))
            act(gates[:], gates[:], AF.Tanh, (gtb,), (gtb,), scale=0.5)
            P.lag(LAG)
            gden = cand[:, 0:128].rearrange("p (h k) -> p h k", h=8)
            vts(gden, gates[:], -1.0, ALU.mult, (gtb,), (buf("cand"),), s2=1.0, op1=ALU.add)
            vrecip(gden, gden, (buf("cand"),), (buf("cand"),))
            vstt(gates[:], gates[:], 1.0, gden, ALU.add, ALU.mult, (gtb, buf("cand")), (gtb,))
            P.lag(LAG)
            vreduce(st[:, 8:16], gates[:], ALU.add, (gtb,), (buf("gsum"),))
            vrecip(st[:, 8:16], st[:, 8:16], (buf("gsum"),), (buf("gsum"),))
            vtt(gates[:], gates[:], st[:, 8:16].unsqueeze(2).to_broadcast([128, 8, 16]), ALU.mult,
                (gtb, buf("gsum")), (gtb,))

        def p3_uv(i, stepper):
            ei, eib = eidxis[i % 2]
            xn2, xnb = xn2s[i % 2]
            gates, gtb = gatess[i % 2]
            gflat = gates.rearrange("p h k -> p (h k)")
            yps = [(pf[2 + 2 * (i % 2) + k], buf(f"pf{2 + 2 * (i % 2) + k}")) for k in range(2)]
            G = 1
            held = []

            def finish_group(g0):
                act(gelu_t[:, g0:g0 + G], avals[:, g0:g0 + G], AF.Gelu, tuple(buf(f"avals{j}") for j in range(g0, g0 + G)),
                    (buf(f"gelu{g0}"),))
                for (j, ub, ubb) in held:
                    act(coef[:, j:j + 1], gelu_t[:, j:j + 1], AF.Identity, (buf(f"gelu{g0}"), gtb), (buf(f"coef{j}"),),
                        scale=gflat[:, j:j + 1])
                    dg, dgb = diag_ring.next()
                    act(dg, ident[:], AF.Identity, (buf("ident"), buf(f"coef{j}")), (dgb,), scale=coef[:, j:j + 1])
                    for cg in range(2):
                        mm(yps[cg][0][:, :], dg, ub[:, D + cg * 512:D + (cg + 1) * 512], j == 0, j == 127,
                           (dgb, ubb), (yps[cg][1],))
                del held[:]

            for j in range(128):
                ub, ubb = uv_ring.next()
                gather(ub, ubb, uv16_d, j, "uv16", ei, eib)
                vstt(ub[:, 0:D], ub[:, 0:D], 1.0, xn2, ALU.mult, ALU.mult, (ubb, xnb), (ubb, buf(f"avals{j}")),
                     accum=avals[:, j:j + 1])
                held.append((j, ub, ubb))
                if j % G == G - 1:
                    finish_group(j - G + 1)
                if stepper is not None:
                    stepper.advance(STEP_K)
            return yps

        def p3_post(i, yps):
            tsl = slice(i * 128, (i + 1) * 128)
            hb, hbb = hbufs[i % 2]
            for cg in range(2):
                cs = slice(cg * 512, (cg + 1) * 512)
                vtt(hb[:, cs], hb[:, cs], yps[cg][0][:, :], ALU.add, (hbb, yps[cg][1]), (hbb,))
            r3 = norm_rstd(hb, 6, D, (hbb,), "ss3")
            vts(hn_bf[:], hb, st[:, 6:7], ALU.mult, (hbb, r3), (buf("hn_bf"),))
            transposes(lambda c: hn_bf[:, c * 128:(c + 1) * 128], 8, 128, buf("hn_bf"), hnT[:], buf("hnT"))
            dma("sync", ptile, p_d[tsl, :], buf("ptile"), True)
            vcopy(p_bf, ptile, (buf("ptile"),), (buf("p_bf"),))
            transposes(lambda c: p_bf[:, c * 128:(c + 1) * 128], 2, 128, buf("p_bf"), pTt, buf("pTt"))
            for cg in range(2):
                cs = slice(cg * 512, (cg + 1) * 512)
                pg, pgb = pf3_ring.next()
                for c in range(8):
                    mm(pg[:, :], hnT[:, c, :], w_pg_sb[:, c, cs], c == 0, c == 7, (buf("hnT"), w_pg_bufs[c]), (pgb,))
                pp, ppb = pf3_ring.next()
                for c in range(2):
                    mm(pp[:, :], pTt[:, c, :], w_pp_sb[:, c, cs], c == 0, c == 1, (buf("pTt"), buf(f"w_pp{c}")), (ppb,))
                act(gate_sb, pg[:, :], AF.Tanh, (pgb,), (buf("gate_sb"),), scale=0.5)
                P.lag(LAG)
                vstt(gate_sb, gate_sb, 1.0, pp[:, :], ALU.add, ALU.mult, (buf("gate_sb"), ppb), (buf("gate_sb"),))
                vstt(hb[:, cs], gate_sb, 0.5, hb[:, cs], ALU.mult, ALU.add, (hbb, buf("gate_sb")), (hbb,))
            r4 = norm_rstd(hb, 7, D, (hbb,), "ss4")
            vstt(hb, hb, st[:, 7:8], gfin_bc3, ALU.mult, ALU.mult, (hbb, r4, buf("gfin")), (hbb,))
            tok = dma("sync", out_d[tsl, :], hb, hbb, False)
            P.final_tokens.append(tok)


        ntr = NT if NT_RUN is None else NT_RUN
        p3_pre(0)
        prev = None
        for i in range(ntr):
            def woven(i=i, prev=prev):
                if prev is not None:
                    p3_post(*prev)
                if i + 1 < ntr:
                    p3_pre(i + 1)
            stepper = Stepper(P, woven)
            yps = p3_uv(i, stepper)
            stepper.finish()
            prev = (i, yps)
        p3_post(*prev)

    P.op("sync", None, extra=tuple(P.final_tokens))

    P.finalize_marks()
    esems = {e: es.enter_context(nc.semaphore(f"sem_{e}")) for e in Prog.ENGS}
    for b in P.dma_bufs:
        b.dsem = es.enter_context(nc.semaphore(f"ds_{b.name}"))
    block = es.enter_context(nc.Block())

    @block.sync
    def _(h):
        P.emit("sync", h, esems)

    @block.scalar
    def _(h):
        P.emit("scalar", h, esems)

    @block.vector
    def _(h):
        P.emit("vector", h, esems)

    @block.gpsimd
    def _(h):
        P.emit("gpsimd", h, esems)

    @block.tensor
    def _(h):
        P.emit("tensor", h, esems)

    es.close()
    return nc, dbg, declared


def _constants():
    ident = np.eye(128, dtype=np.float32)
    t = np.arange(S, dtype=np.float32)

    def tables(rot):
        inv = (np.float32(500000.0) ** (-np.arange(0, rot, 2, dtype=np.float32) / np.float32(rot))).astype(np.float32)
        ang = (t[:, None] * inv[None, :]).astype(np.float32)
        return np.cos(ang).astype(np.float32), np.sin(ang).astype(np.float32)

    cm, sm = tables(32)
    cd, sd = tables(16)
    ropem = np.stack([cm, sm], axis=1).reshape(NT, 128, 2, 16).transpose(1, 0, 2, 3).copy()
    roped = np.stack([cd, sd], axis=1).reshape(NT, 128, 2, 8).transpose(1, 0, 2, 3).copy()
    u = np.arange(2944)[None, :]
    p = np.arange(128)[:, None]
    d = u - p - 1408
    ad = np.abs(d)
    c = (ad <= 64).astype(np.float32) + ((d % 4 == 0) & (ad <= 256)) + ((d % 16 == 0) & (ad <= 1024))
    iota16 = np.tile(np.arange(16, dtype=np.float32)[None, :], (128, 1))
    return ident, ropem, roped, c.astype(np.float32), iota16


_CACHE = {}


def kernel(**inputs):
    return _run(inputs)


def _prep_maps(inputs):
    ident, ropem, roped, mask, iota16 = _constants()
    f = lambda a: np.ascontiguousarray(np.asarray(a, dtype=np.float32))

    def col(g, n):
        return f(g).reshape(n, 128).T

    gcols = np.concatenate([
        col(inputs["g_mix"][0], 8), col(inputs["g_cq"][0], 3), col(inputs["g_ckv"][0], 2),
        col(inputs["g_out_mla"][0], 4), col(inputs["g_out_dil"][0], 4), col(inputs["g_ple"][0], 8),
        col(inputs["g_ffn"][0], 8)], axis=1)
    gcols = np.ascontiguousarray(gcols)
    keys12 = np.ascontiguousarray(np.concatenate([f(inputs["peer_keys1"][0]), f(inputs["peer_keys2"][0])], axis=1))
    shared = {
        "w_in": f(inputs["w_in"][0]), "w_uq": f(inputs["w_uq"][0]), "w_ukv": f(inputs["w_ukv"][0]),
        "w_out": f(inputs["w_out"][0]), "w_peer_q": f(inputs["w_peer_q"][0]), "keys12": keys12,
        "peer_u": f(inputs["peer_u"][0]), "peer_v": f(inputs["peer_v"][0]),
        "w_ple_gate": f(inputs["w_ple_gate"][0]), "w_ple_proj": f(inputs["w_ple_proj"][0]),
        "g_ffn": f(inputs["g_ffn"][0]), "g_final": f(inputs["g_final"]), "gcols": gcols,
        "ident": ident, "ropem": ropem, "roped": roped, "dilmask": mask, "iota16": iota16,
    }
    x = f(inputs["x"])
    p = f(inputs["p"][0])
    return [dict(shared, x=x[b], p=p[b]) for b in range(NCORES)]


def _run(inputs, debug=False, phases=3, cores=NCORES):
    nc, dbg, declared = build_program(debug=debug, phases=phases)
    maps = [{k: m[k] for k in declared} for m in _prep_maps(inputs)[:cores]]
    res = run_bass_kernel_spmd(nc, maps, core_ids=list(range(cores)))
    if debug:
        return res
    return np.stack([r["out"] for r in res.results], axis=0).astype(np.float32)
```

```python
import threading
from contextlib import ExitStack

import numpy as np
import ml_dtypes

import concourse.bass as bass
import concourse.mybir as mybir
from concourse.bass_utils import run_bass_kernel_spmd

F32 = mybir.dt.float32
BF16 = mybir.dt.bfloat16
I32 = mybir.dt.int32
U32 = mybir.dt.uint32
ALU = mybir.AluOpType
AF = mybir.ActivationFunctionType
AX = mybir.AxisListType

S = 2048
D = 1024
NT = 16
EPS = 1e-6
IN_W = 2208
NCORES = 8
DBG_ONLY = None
STOP = None
LVL = 99
TL = 99
P3L = 99
NT_RUN = None


class Buf:
    def __init__(self, name, dma=False):
        self.name = name
        self.w = None
        self.r = []
        self.dma = dma
        self.dsem = None
        self.dcount = 0


class Prog:
    ENGS = ("sync", "scalar", "vector", "gpsimd", "tensor")

    def __init__(self):
        self.ops = {e: [] for e in self.ENGS}
        self.waited = {e: {} for e in self.ENGS}
        self.dma_bufs = []
        self.final_tokens = []
        self.hook = None

    def _need(self, eng, tok, kind):
        if tok[0] == "eng" and tok[1] == eng:
            if eng == "tensor":
                return False
        return True

    def op(self, eng, fn, reads=(), writes=(), dma_buf=None, extra=()):
        deps = []
        for b in reads:
            if b.w is not None:
                deps.append((b.w, "raw"))
            if b.name.startswith("pf") or b.name.startswith("pb"):
                for t in b.r:
                    if not (t[0] == "eng" and t[1] == eng):
                        deps.append((t, "rar"))
        for b in writes:
            if b.w is not None:
                deps.append((b.w, "waw"))
            for t in b.r:
                deps.append((t, "war"))
        for t in extra:
            deps.append((t, "raw"))
        waits = []
        wd = self.waited[eng]
        for tok, kind in deps:
            if not self._need(eng, tok, kind):
                continue
            key = (tok[0], tok[1] if tok[0] == "eng" else id(tok[1]))
            if wd.get(key, -1) >= tok[2]:
                continue
            wd[key] = tok[2]
            waits.append(tok)
        seq = len(self.ops[eng])
        if dma_buf is not None:
            if dma_buf.dsem is None:
                self.dma_bufs.append(dma_buf)
                dma_buf.dsem = True
            dma_buf.dcount += 16
            tok = ("dma", dma_buf, dma_buf.dcount)
        else:
            tok = ("eng", eng, seq)
        self.ops[eng].append(dict(fn=fn, waits=waits, dma_buf=dma_buf, marked=False))
        for b in reads:
            b.r.append(tok)
        for b in writes:
            b.w = tok
            b.r = []
        if self.hook is not None:
            self.hook.tick()
        return tok

    def lag(self, n):
        if self.hook is not None:
            for _ in range(n):
                self.hook.tick()

    def finalize_marks(self):
        for e in self.ENGS:
            for o in self.ops[e]:
                for tok in o["waits"]:
                    if tok[0] == "eng":
                        self.ops[tok[1]][tok[2]]["marked"] = True
        self.count_at = {}
        for e in self.ENGS:
            c = 0
            arr = []
            for o in self.ops[e]:
                if o["marked"]:
                    c += 1
                arr.append(c)
            self.count_at[e] = arr

    def emit(self, eng, handle, esems):
        for o in self.ops[eng]:
            for tok in o["waits"]:
                if tok[0] == "eng":
                    handle.wait_ge(esems[tok[1]], self.count_at[tok[1]][tok[2]])
                else:
                    handle.wait_ge(tok[1].dsem, tok[2])
            if o["fn"] is None:
                continue
            ins = o["fn"](handle)
            if o["dma_buf"] is not None:
                ins.then_inc(o["dma_buf"].dsem, 16)
            elif o["marked"]:
                ins.then_inc(esems[eng], 1)


class Stepper:
    def __init__(self, prog, fn):
        self.prog = prog
        self.fn = fn
        self.go = threading.Semaphore(0)
        self.back = threading.Semaphore(0)
        self.finished = False
        self.budget = 0
        self.err = None
        self.th = threading.Thread(target=self._run, daemon=True)
        self.th.start()

    def _run(self):
        self.go.acquire()
        try:
            self.prog.hook = self
            self.fn()
        except BaseException as e:
            self.err = e
        self.prog.hook = None
        self.finished = True
        self.back.release()

    def tick(self):
        if threading.current_thread() is not self.th:
            return
        self.budget -= 1
        if self.budget <= 0:
            self.prog.hook = None
            self.back.release()
            self.go.acquire()
            self.prog.hook = self

    def advance(self, k):
        if self.finished:
            return
        self.budget = k
        self.go.release()
        self.back.acquire()
        if self.err is not None:
            raise self.err

    def finish(self):
        while not self.finished:
            self.advance(1 << 30)


STEP_K = 6
LAG = 8


class Ring:
    def __init__(self, items):
        self.items = items
        self.i = 0

    def next(self):
        it = self.items[self.i % len(self.items)]
        self.i += 1
        return it


def build_program(debug=False, phases=3):
    nc = bass.Bass("TRN2", target_bir_lowering=False)
    P = Prog()
    es = ExitStack()

    declared = []

    def din(name, shape, dt=F32, need=1):
        if phases < need:
            return None
        declared.append(name)
        return nc.dram_tensor(name, list(shape), dt, kind="ExternalInput").ap()

    def dout(name, shape, dt=F32):
        return nc.dram_tensor(name, list(shape), dt, kind="ExternalOutput").ap()

    x_d = din("x", [S, D])
    p_d = din("p", [S, 256], need=3)
    w_in_d = din("w_in", [D, IN_W])
    w_uq_d = din("w_uq", [384, 768])
    w_ukv_d = din("w_ukv", [256, 1024])
    w_out_d = din("w_out", [D, D], need=3)
    w_pq_d = din("w_peer_q", [D, D], need=3)
    keys_d = din("keys12", [128, 128], need=3)
    u_d = din("peer_u", [16384, D], need=3)
    v_d = din("peer_v", [16384, D], need=3)
    w_pg_d = din("w_ple_gate", [D, D], need=3)
    w_pp_d = din("w_ple_proj", [256, D], need=3)
    g_ffn_d = din("g_ffn", [D], need=3)
    g_fin_d = din("g_final", [D], need=3)
    gcols_d = din("gcols", [128, 37])
    ident_d = din("ident", [128, 128])
    ropem_d = din("ropem", [128, NT, 2, 16])
    roped_d = din("roped", [128, NT, 2, 8])
    mask_d = din("dilmask", [128, 2944])
    iota_d = din("iota16", [128, 16], need=3)
    out_d = dout("out", [S, D])
    if phases >= 3:
        uv16_d = nc.dram_tensor("uv16", [16384, 2 * D], BF16, kind="Internal").ap()
    dbg = {}

    def sb(name, shape, dt):
        return es.enter_context(nc.sbuf_tensor("s_" + name, list(shape), dt))

    def ps(name, shape, dt):
        return es.enter_context(nc.psum_tensor("p_" + name, list(shape), dt))

    regA = sb("regA", [128, 22016], BF16)
    regB = sb("regB", [128, 65792], BF16)
    w_in_sb = regA[:, 0:17664].rearrange("p (c n) -> p c n", n=IN_W)
    w_uq_sb = regA[:, 17664:19968].rearrange("p (c n) -> p c n", n=768)
    w_ukv_sb = regA[:, 19968:22016].rearrange("p (c n) -> p c n", n=1024)
    o_mla_sb = regA[:, 0:8192].rearrange("p (i n) -> p i n", n=512)
    o_dil_sb = regA[:, 8192:16384].rearrange("p (i n) -> p i n", n=512)
    QT = regB[:, 0:16384].rearrange("p (h t) -> p h t", t=S)
    KT = regB[:, 16384:32768].rearrange("p (h t) -> p h t", t=S)
    QdT = regB[:, 32768:40960].rearrange("p (h t) -> p h t", t=S)
    KdT = regB[:, 40960:49152].rearrange("p (h t) -> p h t", t=S)
    Vaug = regB[:, 49152:57472].rearrange("p (i h d) -> p i h d", h=8, d=65)
    Vdaug = regB[:, 57472:65792].rearrange("p (i h d) -> p i h d", h=8, d=65)
    w_out_sb = regB[:, 0:8192].rearrange("p (c n) -> p c n", n=D)
    w_pq_sb = regB[:, 8192:16384].rearrange("p (c n) -> p c n", n=D)
    w_pg_sb = regB[:, 16384:24576].rearrange("p (c n) -> p c n", n=D)
    w_pp_sb = regB[:, 24576:26624].rearrange("p (c n) -> p c n", n=D)
    NG = 8
    ubufs = [regB[:, 26624 + k * 2048: 26624 + (k + 1) * 2048].bitcast(F32) for k in range(NG)]
    vbufs = [regB[:, 43008 + k * 2048: 43008 + (k + 1) * 2048].bitcast(F32) for k in range(NG)]
    gffn_bc = regB[:, 59392:61440].bitcast(F32)
    gfin_bc = regB[:, 61440:63488].bitcast(F32)

    ident = sb("ident", [128, 128], BF16)
    identf = sb("identf", [128, 128], F32)
    gcols = sb("gcols", [128, 37], F32)
    p1w = sb("p1w", [128, 9408], BF16)
    ropem = p1w[:, 7872:8896].bitcast(F32).rearrange("p (i a k) -> p i a k", a=2, k=16)
    roped = p1w[:, 8896:9408].bitcast(F32).rearrange("p (i a k) -> p i a k", a=2, k=8)
    dmask = p1w[:, 4928:7872]
    iota16 = sb("iota16", [128, 16], F32)
    keysT = sb("keysT", [128, 128], BF16)
    xt = [sb(f"xt{k}", [128, D], F32) for k in range(2)]
    junk = sb("junk", [128, D], BF16)
    hn_bf = sb("hn_bf", [128, D], BF16)
    hnT = sb("hnT", [128, 8, 128], BF16)
    st = sb("st", [128, 16], F32)
    cn_bf = p1w[:, 0:640]
    cnT = p1w[:, 640:1280].rearrange("p (c t) -> p c t", t=128)
    q_bf = p1w[:, 1280:2048].rearrange("p (h d) -> p h d", d=96)
    k_bf = p1w[:, 2048:2816].rearrange("p (h d) -> p h d", d=96)
    krope = p1w[:, 2816:2880].bitcast(F32)
    qd_bf = p1w[:, 2880:3392]
    kd_bf = p1w[:, 3392:3904]
    rt = p1w[:, 3904:4928].bitcast(F32).rearrange("p (a h k) -> p a h k", a=4, h=8)

    pf = [ps(f"pf{k}", [128, 512], F32) for k in range(6)]
    pb = [ps(f"pb{k}", [128, 1024], BF16) for k in range(2)]

    B = {}

    def buf(name, dma=False):
        if name not in B:
            B[name] = Buf(name, dma)
        return B[name]

    pf_ring = Ring([(pf[k], buf(f"pf{k}")) for k in range(6)])
    pb_ring = Ring([(pb[k], buf(f"pb{k}")) for k in range(2)])

    def dma(eng, out, in_, sbuf_buf, is_load, extra_reads=(), extra_writes=()):
        sbuf_buf.dma = True
        if is_load:
            return P.op(eng, lambda h: h.dma_start(out=out, in_=in_), reads=extra_reads,
                        writes=(sbuf_buf,) + tuple(extra_writes), dma_buf=sbuf_buf)
        return P.op(eng, lambda h: h.dma_start(out=out, in_=in_), reads=(sbuf_buf,) + tuple(extra_reads),
                    writes=extra_writes, dma_buf=sbuf_buf)

    def V(fn, reads=(), writes=()):
        return P.op("vector", fn, reads, writes)

    def A(fn, reads=(), writes=()):
        return P.op("scalar", fn, reads, writes)

    def G(fn, reads=(), writes=()):
        return P.op("gpsimd", fn, reads, writes)

    def T(fn, reads=(), writes=()):
        return P.op("tensor", fn, reads, writes)

    def transposes(src_ap_fn, n, rows, src_buf, dst_ap, dst_buf, evac="vector"):
        pt, pbuf = pb_ring.next()
        ptv = pt[:].rearrange("p (c t) -> p c t", t=128)
        for c in range(n):
            T(lambda h, c=c: h.transpose(out=ptv[0:rows, c, :], in_=src_ap_fn(c), identity=ident[:]),
              reads=(src_buf, buf("ident")), writes=(pbuf,))
        if evac == "vector":
            V(lambda h: h.tensor_copy(out=dst_ap, in_=ptv[0:rows, 0:n, :]), reads=(pbuf,), writes=(dst_buf,))
        else:
            A(lambda h: h.activation(out=dst_ap, in_=ptv[0:rows, 0:n, :], func=AF.Identity), reads=(pbuf,), writes=(dst_buf,))

    def rstd_from_ss(ss_ap, out_ap, n, bname):
        A(lambda h: h.activation(out=out_ap, in_=ss_ap, func=AF.Sqrt, bias=EPS, scale=1.0 / n),
          reads=(buf(bname),), writes=(buf(bname),))
        P.lag(LAG)
        V(lambda h: h.reciprocal(out=out_ap, in_=out_ap), reads=(buf(bname),), writes=(buf(bname),))
        return buf(bname)

    dma("sync", identf[:], ident_d[:, :], buf("identf"), True)
    dma("sync", gcols[:], gcols_d[:, :], buf("gcols"), True)
    dma("sync", ropem[:], ropem_d[:], buf("ropem"), True)
    dma("sync", roped[:], roped_d[:], buf("roped"), True)
    if phases >= 3:
        dma("sync", iota16[:], iota_d[:, :], buf("iota16"), True)
    dma("gpsimd", ident[:], ident_d[:, :], buf("ident"), True)
    if LVL >= 1:
        dma("gpsimd", dmask[:, 0:1472], mask_d[:, 0:1472], buf("dmask"), True)
        dma("gpsimd", dmask[:, 1472:2944], mask_d[:, 1472:2944], buf("dmask"), True)
    for c in range(8):
        if LVL < 2:
            break
        for hlf in range(2):
            cs = slice(hlf * 1104, (hlf + 1) * 1104)
            dma("gpsimd", w_in_sb[:, c, cs], w_in_d[c * 128:(c + 1) * 128, cs], buf(f"w_in{c}"), True)
        if LVL < 3:
            continue
        V(lambda h, c=c: h.tensor_scalar(out=w_in_sb[:, c, :], in0=w_in_sb[:, c, :], scalar1=gcols[:, c:c + 1],
                                         scalar2=None, op0=ALU.mult),
          reads=(buf(f"w_in{c}"), buf("gcols")), writes=(buf(f"w_in{c}"),))
    for c in range(3):
        if LVL < 4:
            break
        dma("gpsimd", w_uq_sb[:, c, :], w_uq_d[c * 128:(c + 1) * 128, :], buf(f"w_uq{c}"), True)
        V(lambda h, c=c: h.tensor_scalar(out=w_uq_sb[:, c, :], in0=w_uq_sb[:, c, :], scalar1=gcols[:, 8 + c:9 + c],
                                         scalar2=None, op0=ALU.mult),
          reads=(buf(f"w_uq{c}"), buf("gcols")), writes=(buf(f"w_uq{c}"),))
    for c in range(2):
        if LVL < 4:
            break
        dma("gpsimd", w_ukv_sb[:, c, :], w_ukv_d[c * 128:(c + 1) * 128, :], buf(f"w_ukv{c}"), True)
        V(lambda h, c=c: h.tensor_scalar(out=w_ukv_sb[:, c, :], in0=w_ukv_sb[:, c, :], scalar1=gcols[:, 11 + c:12 + c],
                                         scalar2=None, op0=ALU.mult),
          reads=(buf(f"w_ukv{c}"), buf("gcols")), writes=(buf(f"w_ukv{c}"),))
    if debug and LVL >= 5:
        V(lambda h: h.memset(regB[:, 0:32896], 0.0), writes=(buf("dbgms"),))
        V(lambda h: h.memset(regB[:, 32896:65792], 0.0),
          writes=tuple(buf(f"{n}{i}") for n in ("QT", "KT", "QdT", "KdT", "Vm", "Vd") for i in range(NT)) + (buf("Vaug_ones"), buf("Vdaug_ones")))
    if LVL >= 6:
        V(lambda h: h.memset(Vaug[:, :, :, 64:65], 1.0), writes=(buf("Vaug_ones"),))
        V(lambda h: h.memset(Vdaug[:, :, :, 64:65], 1.0), writes=(buf("Vdaug_ones"),))

    if phases >= 3:
        for (src, c0) in ((u_d, 0), (v_d, D)):
            for k in range(8):
                rs = slice(k * 2048, (k + 1) * 2048)
                dma("gpsimd", uv16_d[rs, c0:c0 + D], src[rs, :], buf("uv16"), True)
    MLA_SCALE = 96.0 ** -0.5
    w_in_bufs = tuple(buf(f"w_in{c}") for c in range(8))

    def phase1_tile(i):
        tsl = slice(i * 128, (i + 1) * 128)
        xb = xt[i % 2]
        xbuf = buf(f"xt{i % 2}")
        dma("sync", xb[:], x_d[tsl, :], xbuf, True)
        A(lambda h, xb=xb: h.activation(out=junk[:], in_=xb[:], func=AF.Square, accum_out=st[:, 0:1]),
          reads=(xbuf,), writes=(buf("junk"), buf("ssx")))
        rb = rstd_from_ss(st[:, 0:1], st[:, 0:1], D, "ssx")
        V(lambda h, xb=xb: h.tensor_scalar(out=hn_bf[:], in0=xb[:], scalar1=st[:, 0:1], scalar2=None, op0=ALU.mult),
          reads=(xbuf, rb), writes=(buf("hn_bf"),))
        transposes(lambda c: hn_bf[:, c * 128:(c + 1) * 128], 8, 128, buf("hn_bf"), hnT[:], buf("hnT"))

        if TL < 1:
            return
        def proj_group(c0, c1):
            pt, pbuf = pf_ring.next()
            for c in range(8):
                T(lambda h, c=c, pt=pt: h.matmul(pt[:, 0:c1 - c0], lhsT=hnT[:, c, :], rhs=w_in_sb[:, c, c0:c1],
                                                 start=(c == 0), stop=(c == 7)),
                  reads=(buf("hnT"), w_in_bufs[c]), writes=(pbuf,))
            return pt, pbuf

        pt, pbuf = proj_group(0, 384)
        A(lambda h, pt=pt: h.activation(out=junk[:, 0:384], in_=pt[:, 0:384], func=AF.Square, accum_out=st[:, 1:2]),
          reads=(pbuf,), writes=(buf("junk"), buf("ssq")))
        rq = rstd_from_ss(st[:, 1:2], st[:, 1:2], 384, "ssq")
        V(lambda h, pt=pt: h.tensor_scalar(out=cn_bf[:, 0:384], in0=pt[:, 0:384], scalar1=st[:, 1:2], scalar2=None,
                                           op0=ALU.mult),
          reads=(pbuf, rq), writes=(buf("cn_q"),))
        if TL < 2:
            return
        pt, pbuf = proj_group(384, 672)
        A(lambda h, pt=pt: h.activation(out=junk[:, 0:256], in_=pt[:, 0:256], func=AF.Square, accum_out=st[:, 2:3]),
          reads=(pbuf,), writes=(buf("junk"), buf("sskv")))
        rkv = rstd_from_ss(st[:, 2:3], st[:, 2:3], 256, "sskv")
        V(lambda h, pt=pt: h.tensor_scalar(out=cn_bf[:, 384:640], in0=pt[:, 0:256], scalar1=st[:, 2:3], scalar2=None,
                                           op0=ALU.mult),
          reads=(pbuf, rkv), writes=(buf("cn_kv"),))
        cm = ropem[:, i, 0, :]
        sm = ropem[:, i, 1, :]
        V(lambda h, pt=pt: h.tensor_tensor(out=krope[:, 0:16], in0=pt[:, 256:272], in1=cm, op=ALU.mult),
          reads=(pbuf, buf("ropem")), writes=(buf("krope_a"),))
        V(lambda h, pt=pt: h.tensor_tensor(out=rt[:, 0, 0, :], in0=pt[:, 272:288], in1=sm, op=ALU.mult),
          reads=(pbuf, buf("ropem")), writes=(buf("rt0"),))
        V(lambda h: h.tensor_tensor(out=krope[:, 0:16], in0=krope[:, 0:16], in1=rt[:, 0, 0, :], op=ALU.subtract),
          reads=(buf("krope_a"), buf("rt0")), writes=(buf("krope_a"),))
        V(lambda h, pt=pt: h.tensor_tensor(out=krope[:, 16:32], in0=pt[:, 272:288], in1=cm, op=ALU.mult),
          reads=(pbuf, buf("ropem")), writes=(buf("krope_b"),))
        V(lambda h, pt=pt: h.tensor_tensor(out=rt[:, 1, 0, :], in0=pt[:, 256:272], in1=sm, op=ALU.mult),
          reads=(pbuf, buf("ropem")), writes=(buf("rt1"),))
        V(lambda h: h.tensor_tensor(out=krope[:, 16:32], in0=krope[:, 16:32], in1=rt[:, 1, 0, :], op=ALU.add),
          reads=(buf("krope_b"), buf("rt1")), writes=(buf("krope_b"),))
        V(lambda h: h.tensor_copy(out=k_bf[:, :, 64:96], in_=krope[:].unsqueeze(1).to_broadcast([128, 8, 32])),
          reads=(buf("krope_a"), buf("krope_b")), writes=(buf("k_bf_r"),))

        if TL < 3:
            return
        def dil_rope(pt, pbuf, dst, dname, scale):
            cd = roped[:, i, 0, :].unsqueeze(1).to_broadcast([128, 8, 8])
            sd = roped[:, i, 1, :].unsqueeze(1).to_broadcast([128, 8, 8])
            pv = pt[:].rearrange("p (h d) -> p h d", d=64)
            dv = dst[:].rearrange("p (h d) -> p h d", d=64)
            x1 = pv[:, :, 0:8]
            x2 = pv[:, :, 8:16]
            t0 = rt[:, 0, :, 0:8]
            t1 = rt[:, 1, :, 0:8]
            t2 = rt[:, 2, :, 0:8]
            t3 = rt[:, 3, :, 0:8]
            V(lambda h: h.tensor_tensor(out=t0, in0=x1, in1=cd, op=ALU.mult), reads=(pbuf, buf("roped")), writes=(buf("rt0"),))
            V(lambda h: h.tensor_tensor(out=t1, in0=x2, in1=sd, op=ALU.mult), reads=(pbuf, buf("roped")), writes=(buf("rt1"),))
            V(lambda h: h.tensor_tensor(out=t2, in0=x2, in1=cd, op=ALU.mult), reads=(pbuf, buf("roped")), writes=(buf("rt2"),))
            V(lambda h: h.tensor_tensor(out=t3, in0=x1, in1=sd, op=ALU.mult), reads=(pbuf, buf("roped")), writes=(buf("rt3"),))
            V(lambda h: h.tensor_tensor(out=t0, in0=t0, in1=t1, op=ALU.subtract),
              reads=(buf("rt0"), buf("rt1")), writes=(buf("rt0"),))
            V(lambda h: h.tensor_tensor(out=t2, in0=t2, in1=t3, op=ALU.add),
              reads=(buf("rt2"), buf("rt3")), writes=(buf("rt2"),))
            V(lambda h: h.tensor_scalar(out=dv[:, :, 0:8], in0=t0, scalar1=scale, scalar2=None, op0=ALU.mult),
              reads=(buf("rt0"),), writes=(buf(dname + "a"),))
            V(lambda h: h.tensor_scalar(out=dv[:, :, 8:16], in0=t2, scalar1=scale, scalar2=None, op0=ALU.mult),
              reads=(buf("rt2"),), writes=(buf(dname + "b"),))
            V(lambda h: h.tensor_scalar(out=dv[:, :, 16:64], in0=pv[:, :, 16:64], scalar1=scale, scalar2=None, op0=ALU.mult),
              reads=(pbuf,), writes=(buf(dname + "c"),))
            return (buf(dname + "a"), buf(dname + "b"), buf(dname + "c"))

        pt, pbuf = proj_group(672, 1184)
        qd_bufs = dil_rope(pt, pbuf, qd_bf, "qd_bf", 0.125)
        if TL < 3.3:
            return
        pt, pbuf = proj_group(1184, 1696)
        kd_bufs = dil_rope(pt, pbuf, kd_bf, "kd_bf", 1.0)
        if TL < 3.6:
            return
        pt, pbuf = proj_group(1696, 2208)
        A(lambda h, pt=pt: h.activation(out=Vdaug[:, i, :, 0:64], in_=pt[:].rearrange("p (h d) -> p h d", d=64), func=AF.Identity),
          reads=(pbuf,), writes=(buf(f"Vd{i}"),))

        if TL < 4:
            return
        pt_, pbuf_ = pb_ring.next()
        ptv = pt_[:].rearrange("p (c t) -> p c t", t=128)
        for c in range(4):
            T(lambda h, c=c, ptv=ptv: h.transpose(out=ptv[:, c, :], in_=qd_bf[:, c * 128:(c + 1) * 128], identity=ident[:]),
              reads=qd_bufs + (buf("ident"),), writes=(pbuf_,))
        for c in range(4):
            T(lambda h, c=c, ptv=ptv: h.transpose(out=ptv[:, 4 + c, :], in_=kd_bf[:, c * 128:(c + 1) * 128], identity=ident[:]),
              reads=kd_bufs + (buf("ident"),), writes=(pbuf_,))
        V(lambda h, ptv=ptv: h.tensor_copy(out=QdT[:, :, tsl], in_=ptv[:, 0:4, :]), reads=(pbuf_,), writes=(buf(f"QdT{i}"),))
        A(lambda h, ptv=ptv: h.activation(out=KdT[:, :, tsl], in_=ptv[:, 4:8, :], func=AF.Identity), reads=(pbuf_,), writes=(buf(f"KdT{i}"),))

        if TL < 5:
            return
        pt_, pbuf_ = pb_ring.next()
        ptv = pt_[:].rearrange("p (c t) -> p c t", t=128)
        for c in range(5):
            T(lambda h, c=c, ptv=ptv: h.transpose(out=ptv[:, c, :], in_=cn_bf[:, c * 128:(c + 1) * 128], identity=ident[:]),
              reads=(buf("cn_q") if c < 3 else buf("cn_kv"), buf("ident")), writes=(pbuf_,))
        V(lambda h, ptv=ptv: h.tensor_copy(out=cnT[:], in_=ptv[:, 0:5, :]), reads=(pbuf_,), writes=(buf("cnT"),))

        for (h0, h1) in ((0, 5), (5, 8)):
            c0, c1 = h0 * 96, h1 * 96
            nh = h1 - h0
            pt, pbuf = pf_ring.next()
            for c in range(3):
                T(lambda h, c=c, pt=pt, c0=c0, c1=c1: h.matmul(pt[:, 0:c1 - c0], lhsT=cnT[:, c, :], rhs=w_uq_sb[:, c, c0:c1],
                                                                start=(c == 0), stop=(c == 2)),
                  reads=(buf("cnT"), buf(f"w_uq{c}")), writes=(pbuf,))
            pv = pt[:, 0:nh * 96].rearrange("p (h d) -> p h d", d=96)
            qv = q_bf[:, h0:h1, :]
            cmb = ropem[:, i, 0, :].unsqueeze(1).to_broadcast([128, nh, 16])
            smb = ropem[:, i, 1, :].unsqueeze(1).to_broadcast([128, nh, 16])
            x1 = pv[:, :, 64:80]
            x2 = pv[:, :, 80:96]
            t0 = rt[:, 0, 0:nh, :]
            t1 = rt[:, 1, 0:nh, :]
            t2 = rt[:, 2, 0:nh, :]
            t3 = rt[:, 3, 0:nh, :]
            V(lambda h, pv=pv, qv=qv: h.tensor_scalar(out=qv[:, :, 0:64], in0=pv[:, :, 0:64], scalar1=MLA_SCALE, scalar2=None, op0=ALU.mult),
              reads=(pbuf,), writes=(buf(f"q_bf_n{h0}"),))
            V(lambda h, t0=t0, x1=x1, cmb=cmb: h.tensor_tensor(out=t0, in0=x1, in1=cmb, op=ALU.mult),
              reads=(pbuf, buf("ropem")), writes=(buf("rt0"),))
            V(lambda h, t1=t1, x2=x2, smb=smb: h.tensor_tensor(out=t1, in0=x2, in1=smb, op=ALU.mult),
              reads=(pbuf, buf("ropem")), writes=(buf("rt1"),))
            V(lambda h, t2=t2, x2=x2, cmb=cmb: h.tensor_tensor(out=t2, in0=x2, in1=cmb, op=ALU.mult),
              reads=(pbuf, buf("ropem")), writes=(buf("rt2"),))
            V(lambda h, t3=t3, x1=x1, smb=smb: h.tensor_tensor(out=t3, in0=x1, in1=smb, op=ALU.mult),
              reads=(pbuf, buf("ropem")), writes=(buf("rt3"),))
            V(lambda h, t0=t0, t1=t1: h.tensor_tensor(out=t0, in0=t0, in1=t1, op=ALU.subtract),
              reads=(buf("rt0"), buf("rt1")), writes=(buf("rt0"),))
            V(lambda h, t2=t2, t3=t3: h.tensor_tensor(out=t2, in0=t2, in1=t3, op=ALU.add),
              reads=(buf("rt2"), buf("rt3")), writes=(buf("rt2"),))
            V(lambda h, t0=t0, qv=qv: h.tensor_scalar(out=qv[:, :, 64:80], in0=t0, scalar1=MLA_SCALE, scalar2=None, op0=ALU.mult),
              reads=(buf("rt0"),), writes=(buf(f"q_bf_a{h0}"),))
            V(lambda h, t2=t2, qv=qv: h.tensor_scalar(out=qv[:, :, 80:96], in0=t2, scalar1=MLA_SCALE, scalar2=None, op0=ALU.mult),
              reads=(buf("rt2"),), writes=(buf(f"q_bf_b{h0}"),))
        q_bufs = tuple(buf(f"q_bf_{s}{h0}") for s in "nab" for h0 in (0, 5))
        if TL < 6:
            return
        for gidx in range(2):
            pt, pbuf = pf_ring.next()
            for c in range(2):
                T(lambda h, c=c, pt=pt, gidx=gidx: h.matmul(pt[:, :], lhsT=cnT[:, 3 + c, :],
                                                            rhs=w_ukv_sb[:, c, gidx * 512:(gidx + 1) * 512],
                                                            start=(c == 0), stop=(c == 1)),
                  reads=(buf("cnT"), buf(f"w_ukv{c}")), writes=(pbuf,))
            pv = pt[:].rearrange("p (h d) -> p h d", d=128)
            A(lambda h, pv=pv, gidx=gidx: h.activation(out=k_bf[:, gidx * 4:(gidx + 1) * 4, 0:64], in_=pv[:, :, 0:64], func=AF.Identity),
              reads=(pbuf,), writes=(buf(f"k_bf_n{gidx}"),))
            V(lambda h, pv=pv, gidx=gidx: h.tensor_copy(out=Vaug[:, i, gidx * 4:(gidx + 1) * 4, 0:64], in_=pv[:, :, 64:128]),
              reads=(pbuf,), writes=(buf(f"Vm{i}"),))
        k_bufs = (buf("k_bf_n0"), buf("k_bf_n1"), buf("k_bf_r"))
        if TL < 7:
            return
        for (src, sbufs, dst, dname, ev) in ((q_bf, q_bufs, QT, "QT", "vector"), (k_bf, k_bufs, KT, "KT", "scalar")):
            pt_, pbuf_ = pb_ring.next()
            ptv = pt_[:].rearrange("p (c t) -> p c t", t=128)
            for hh in range(8):
                T(lambda h, hh=hh, ptv=ptv, src=src: h.transpose(out=ptv[0:96, hh, :], in_=src[:, hh, :], identity=ident[:]),
                  reads=sbufs + (buf("ident"),), writes=(pbuf_,))
            if ev == "vector":
                V(lambda h, ptv=ptv, dst=dst: h.tensor_copy(out=dst[0:96, :, tsl], in_=ptv[0:96, :, :]),
                  reads=(pbuf_,), writes=(buf(f"{dname}{i}"),))
            else:
                A(lambda h, ptv=ptv, dst=dst: h.activation(out=dst[0:96, :, tsl], in_=ptv[0:96, :, :], func=AF.Identity),
                  reads=(pbuf_,), writes=(buf(f"{dname}{i}"),))

    if STOP == "weights":
        dbg["w"] = dout("dbg_w", [128, IN_W], BF16)
        if LVL >= 2:
            tok = P.op("sync", lambda h: h.dma_start(out=dbg["w"][:, :], in_=w_in_sb[:, 0, :]),
                       reads=(buf("w_in0"),), writes=(), dma_buf=buf("dbgw"))
        else:
            tok = P.op("sync", lambda h: h.dma_start(out=dbg["w"][:, 0:128], in_=ident[:, :]),
                       reads=(buf("ident"),), writes=(), dma_buf=buf("dbgw"))
        P.final_tokens.append(tok)
    else:
        for i in range(NT if NT_RUN is None else NT_RUN):
            phase1_tile(i)

    if debug and phases == 1 and STOP is None:
        allb = [buf(f"{n}{i}") for n in ("QT", "KT", "QdT", "KdT", "Vm", "Vd") for i in range(NT if NT_RUN is None else NT_RUN)]
        for nm, ap, shp in (("QT", QT[0:96], [96, 8, S]), ("KT", KT[0:96], [96, 8, S]), ("QdT", QdT[:], [128, 4, S]),
                            ("KdT", KdT[:], [128, 4, S]), ("Vaug", Vaug[:], [128, NT, 8, 65]), ("Vdaug", Vdaug[:], [128, NT, 8, 65])):
            if DBG_ONLY and nm not in DBG_ONLY:
                continue
            dbg[nm] = dout("dbg_" + nm, shp, BF16)
            tok = P.op("sync", lambda h, nm=nm, ap=ap: h.dma_start(out=dbg[nm][:], in_=ap),
                       reads=tuple(allb) + (buf("Vaug_ones"), buf("Vdaug_ones")), writes=(), dma_buf=buf("dbg" + nm))
            P.final_tokens.append(tok)

    def alias(new_names, old_bufs):
        toks = []
        for ob in old_bufs:
            if ob.w is not None:
                toks.append(ob.w)
            toks.extend(ob.r)
        for n in new_names:
            nb = buf(n)
            nb.r = list(nb.r) + toks

    def mm(out, lhsT, rhs, start, stop, reads, writes, skip=False):
        if skip:
            T(lambda h: h.matmul(out, lhsT=lhsT, rhs=rhs, start=start, stop=stop, skip_group_check=True), reads, writes)
        else:
            T(lambda h: h.matmul(out, lhsT=lhsT, rhs=rhs, start=start, stop=stop), reads, writes)

    def act(out, in_, func, reads, writes, bias=None, scale=None, accum=None):
        kw = {}
        if bias is not None:
            kw["bias"] = bias
        if scale is not None:
            kw["scale"] = scale
        if accum is not None:
            kw["accum_out"] = accum
        A(lambda h: h.activation(out=out, in_=in_, func=func, **kw), reads, writes)

    def vtt(out, in0, in1, op, reads, writes):
        V(lambda h: h.tensor_tensor(out=out, in0=in0, in1=in1, op=op), reads, writes)

    def vts(out, in0, s1, op0, reads, writes, s2=None, op1=None):
        if op1 is None:
            V(lambda h: h.tensor_scalar(out=out, in0=in0, scalar1=s1, scalar2=None, op0=op0), reads, writes)
        else:
            V(lambda h: h.tensor_scalar(out=out, in0=in0, scalar1=s1, scalar2=s2, op0=op0, op1=op1), reads, writes)

    def vcopy(out, in_, reads, writes):
        V(lambda h: h.tensor_copy(out=out, in_=in_), reads, writes)

    def vrecip(out, in_, reads, writes):
        V(lambda h: h.reciprocal(out=out, in_=in_), reads, writes)

    if phases >= 2:
        p1_work = [buf(n) for n in ("xt0", "xt1", "junk", "hn_bf", "hnT")]
        w_bufs = [buf(f"w_in{c}") for c in range(8)] + [buf(f"w_uq{c}") for c in range(3)] + [buf(f"w_ukv{c}") for c in range(2)]
        pTs = [xt[0][:, k * 256:(k + 1) * 256].bitcast(BF16) for k in range(4)]
        alias([f"pT{k}" for k in range(4)] + ["rc"], p1_work)
        alias([f"o_mla{i}" for i in range(NT)] + [f"o_dil{i}" for i in range(NT)], w_bufs)
        pT_ring = Ring([(pTs[k], buf(f"pT{k}")) for k in range(4)])
        s_ring = Ring([(pf[k], buf(f"pf{k}")) for k in range(3)])
        acc_ring = Ring([(pf[3 + k], buf(f"pf{3 + k}")) for k in range(2)])
        rc = xt[1][:, 0:8]

        def attention(kind):
            for hd in range(8):
                if kind == "mla":
                    Qop = QT[0:96, hd, :]
                    Kop = KT[0:96, hd, :]
                    Vt = Vaug
                    qbufs = [buf(f"QT{i}") for i in range(NT)]
                    kbufs = [buf(f"KT{i}") for i in range(NT)]
                    vbufs_ = [buf(f"Vm{i}") for i in range(NT)]
                    vones = buf("Vaug_ones")
                    o_sb, oname = o_mla_sb, "o_mla"
                else:
                    r0 = (hd % 2) * 64
                    Qop = QdT[r0:r0 + 64, hd // 2, :]
                    Kop = KdT[r0:r0 + 64, hd // 2, :]
                    Vt = Vdaug
                    qbufs = [buf(f"QdT{i}") for i in range(NT)]
                    kbufs = [buf(f"KdT{i}") for i in range(NT)]
                    vbufs_ = [buf(f"Vd{i}") for i in range(NT)]
                    vones = buf("Vdaug_ones")
                    o_sb, oname = o_dil_sb, "o_dil"
                for qb in range(4):
                    acc, accb = acc_ring.next()
                    accv = acc[:, 0:264].rearrange("p (q d) -> p q d", d=66)
                    kts = []
                    for kt in range(NT):
                        d0 = qb * 512 - kt * 128
                        if kind == "dil" and (d0 < -1408 or d0 > 1024):
                            continue
                        kts.append(kt)
                    state = {"first": True}

                    def pv(kt, pT, pTb, last):
                        for qt in range(4):
                            st_flag = state["first"]
                            mm(accv[:, qt, 0:65], pT[:, qt * 128:(qt + 1) * 128], Vt[:, kt, hd, 0:65],
                               st_flag, last and qt == 3, (pTb, vbufs_[kt], vones), (accb,))
                            state["first"] = False

                    prev = None
                    for kt in kts:
                        sp, spb = s_ring.next()
                        mm(sp[:, 0:512], Kop[:, kt * 128:(kt + 1) * 128], Qop[:, qb * 512:(qb + 1) * 512], True, True,
                           (kbufs[kt],) + tuple(qbufs[qb * 4:(qb + 1) * 4]), (spb,))
                        pT, pTb = pT_ring.next()
                        act(pT, sp[:, 0:512], AF.Exp, (spb,), (pTb,))
                        if kind == "dil":
                            base = qb * 512 - kt * 128 + 1408
                            vtt(pT, pT, dmask[:, base:base + 512], ALU.mult, (pTb, buf("dmask")), (pTb,))
                        if prev is not None:
                            pv(*prev, False)
                        prev = (kt, pT, pTb)
                    pv(*prev, True)
                    vrecip(rc[:, 0:4], accv[:, :, 64], (accb,), (buf("rc"),))
                    obufs = tuple(buf(f"{oname}{qb * 4 + qt}") for qt in range(4))
                    vtt(o_sb[:, qb * 4:(qb + 1) * 4, hd * 64:(hd + 1) * 64], accv[:, :, 0:64],
                        rc[:, 0:4].unsqueeze(2).to_broadcast([128, 4, 64]), ALU.mult, (accb, buf("rc")), obufs)

        attention("mla")
        attention("dil")

    if debug and phases == 2:
        for nm, ap in (("o_mla", o_mla_sb), ("o_dil", o_dil_sb)):
            dbg[nm] = dout("dbg_" + nm, [128, NT, 512], BF16)
            tok = P.op("sync", lambda h, nm=nm, ap=ap: h.dma_start(out=dbg[nm][:], in_=ap[:]),
                       reads=tuple(buf(f"{nm}{i}") for i in range(NT)), writes=(), dma_buf=buf("dbg" + nm))
            P.final_tokens.append(tok)

    if phases >= 3:
        qkv_bufs = [buf(f"{n}{i}") for n in ("QT", "KT", "QdT", "KdT", "Vm", "Vd") for i in range(NT)] + \
                   [buf("Vaug_ones"), buf("Vdaug_ones")]
        NGB = 13
        uvbufs = [regB[:, 26624 + k * 2048: 26624 + (k + 1) * 2048] for k in range(8)] + [regA[:, 16384:18432]] + \
                 [p1w[:, k * 2048:(k + 1) * 2048] for k in range(4)]
        diagt = sb("diagt", [128, 4, 128], BF16)
        diag_ring = Ring([(diagt[:, k, :], buf(f"diag{k}")) for k in range(4)])
        gffn_bc3 = regB[:, 43008:45056].bitcast(F32)
        gfin_bc3 = regB[:, 45056:47104].bitcast(F32)
        hbuf = regB[:, 47104:49152].bitcast(F32)
        xn2a = regB[:, 49152:51200].bitcast(F32)
        xn2b = regA[:, 18432:20480].bitcast(F32)
        gates2 = regA[:, 20480:20736].bitcast(F32).rearrange("p (h k) -> p h k", h=8)
        gelu_t = sb("gelu_t", [128, 128], F32)
        ybuf = regB[:, 51200:53248].bitcast(F32)
        Ssc = regB[:, 53248:57344].bitcast(F32)
        cand = regB[:, 57344:59392].bitcast(F32)
        oh = regB[:, 59392:61440].bitcast(F32)
        gate_sb = regB[:, 61440:62464].bitcast(F32)
        qTp = regB[:, 62464:63488].rearrange("p (h t) -> p h t", t=128)
        v12 = regB[:, 63488:64000].bitcast(F32).rearrange("p (a h k) -> p a h k", a=2, h=8)
        i12 = regB[:, 64000:64512].bitcast(U32).rearrange("p (a h k) -> p a h k", a=2, h=8)
        i12f = regB[:, 64512:65024].bitcast(F32).rearrange("p (a h k) -> p a h k", a=2, h=8)
        wk = regB[:, 65024:65536].bitcast(F32)
        top = regB[:, 65536:65792].bitcast(F32).rearrange("p (h k) -> p h k", h=8)
        x0 = xt[0]
        pos = x0[:, 0:128].bitcast(U32).rearrange("p (h k) -> p h k", h=8)
        ai = x0[:, 128:256].bitcast(U32).rearrange("p (h k) -> p h k", h=8)
        bi = x0[:, 256:384].bitcast(U32).rearrange("p (h k) -> p h k", h=8)
        af = x0[:, 384:512].rearrange("p (h k) -> p h k", h=8)
        bf = x0[:, 512:640].rearrange("p (h k) -> p h k", h=8)
        i1sel = x0[:, 640:768].rearrange("p (h k) -> p h k", h=8)
        i2sel = x0[:, 768:896].rearrange("p (h k) -> p h k", h=8)
        eidxf = x0[:, 896:1024]
        x1_ = xt[1]
        eidxi = x1_[:, 0:128].bitcast(I32)
        gates = x1_[:, 128:256].rearrange("p (h k) -> p h k", h=8)
        avals = x1_[:, 256:384]
        coef = x1_[:, 384:512]
        ptile = x1_[:, 512:768]
        p_bf = x1_[:, 768:896].bitcast(BF16)
        pTt = x1_[:, 896:1024].bitcast(BF16).rearrange("p (c t) -> p c t", t=128)
        keys_bf = p1w[:, 8192:8320]

        p3_names = ["w_out%d" % c for c in range(8)] + ["w_pq%d" % c for c in range(8)] + ["w_pg%d" % c for c in range(8)] + \
                   ["w_pp0", "w_pp1", "gffn", "gfin", "hbuf0", "xn20", "hbuf1", "Ssc", "cand", "oh", "gate_sb", "qTp",
                    "v12", "i12", "i12f", "wk", "top"] + [f"ohs{k}" for k in range(8)] + [f"v12lo{j}" for j in range(16)] + \
                   [f"v12hi{j}" for j in range(16)] + [f"i12_{j}" for j in range(16)] + [f"toplo{h}" for h in range(8)] + [f"tophi{h}" for h in range(8)] + [f"uvb{k}" for k in range(8)]
        alias(p3_names, qkv_bufs)
        alias(["uvb8", "xn21", "gates1"], w_bufs)
        x0_names = [f"pos{h}" for h in range(8)] + ["ai", "bi", "af", "bf", "i1sel", "i2sel", "eidxf"]
        x1_names = ["eidxi0", "gates0", "ptile", "p_bf", "pTt"] + [f"coef{j}" for j in range(128)] + [f"avals{j}" for j in range(128)]
        alias(x0_names, [buf(f"pT{k}") for k in range(4)] + [buf("xt0")])
        alias(x1_names, [buf("rc"), buf("xt1")])
        all_prev = [b_ for n_, b_ in list(B.items()) if not n_.startswith("pf") and not n_.startswith("pb")]
        alias(["keys_bf"] + [f"uvb{k}" for k in range(9, 13)], all_prev)

        for c in range(8):
            dma("gpsimd", w_out_sb[:, c, :], w_out_d[c * 128:(c + 1) * 128, :], buf(f"w_out{c}"), True)
            vts(w_out_sb[:, c, :], w_out_sb[:, c, :], gcols[:, 13 + c:14 + c], ALU.mult,
                (buf(f"w_out{c}"), buf("gcols")), (buf(f"w_out{c}"),))
        for c in range(8):
            dma("gpsimd", w_pq_sb[:, c, :], w_pq_d[c * 128:(c + 1) * 128, :], buf(f"w_pq{c}"), True)
        for c in range(8):
            dma("gpsimd", w_pg_sb[:, c, :], w_pg_d[c * 128:(c + 1) * 128, :], buf(f"w_pg{c}"), True)
            vts(w_pg_sb[:, c, :], w_pg_sb[:, c, :], gcols[:, 21 + c:22 + c], ALU.mult,
                (buf(f"w_pg{c}"), buf("gcols")), (buf(f"w_pg{c}"),))
        for c in range(2):
            dma("gpsimd", w_pp_sb[:, c, :], w_pp_d[c * 128:(c + 1) * 128, :], buf(f"w_pp{c}"), True)
        dma("sync", gffn_bc3, g_ffn_d.partition_broadcast(128), buf("gffn"), True)
        dma("sync", gfin_bc3, g_fin_d.partition_broadcast(128), buf("gfin"), True)
        dma("gpsimd", keys_bf, keys_d[:, :], buf("keys_bf"), True)
        pt_, pbuf_ = pb_ring.next()
        T(lambda h: h.transpose(out=pt_[:, 0:128], in_=keys_bf, identity=ident[:]), reads=(buf("keys_bf"), buf("ident")), writes=(pbuf_,))
        vcopy(keysT[:], pt_[:, 0:128], (pbuf_,), (buf("keysT"),))
        w_out_bufs = tuple(buf(f"w_out{c}") for c in range(8))
        w_pq_bufs = tuple(buf(f"w_pq{c}") for c in range(8))
        w_pg_bufs = tuple(buf(f"w_pg{c}") for c in range(8))
        pf3_ring = Ring([(pf[k], buf(f"pf{k}")) for k in range(2)])
        uv_ring = Ring([(uvbufs[k], buf(f"uvb{k}")) for k in range(NGB)])
        xn2s = [(xn2a, buf("xn20")), (xn2b, buf("xn21"))]
        gatess = [(gates, buf("gates0")), (gates2, buf("gates1"))]

        def vmax(out, in_, reads, writes):
            V(lambda h: h.max(out=out, in_=in_), reads, writes)

        def vmatch(out, rep, vals, reads, writes):
            V(lambda h: h.match_replace(out=out, in_to_replace=rep, in_values=vals, imm_value=-1e30), reads, writes)

        def vmaxidx(out, mx, vals, reads, writes):
            V(lambda h: h.max_index(out=out, in_max=mx, in_values=vals), reads, writes)

        def vstt(out, in0, scalar, in1, op0, op1, reads, writes, accum=None):
            if accum is None:
                V(lambda h: h.scalar_tensor_tensor(out=out, in0=in0, scalar=scalar, in1=in1, op0=op0, op1=op1), reads, writes)
            else:
                V(lambda h: h.scalar_tensor_tensor(out=out, in0=in0, scalar=scalar, in1=in1, op0=op0, op1=op1,
                                                   accum_out=accum), reads, writes)

        def vreduce(out, in_, op, reads, writes):
            V(lambda h: h.tensor_reduce(out=out, in_=in_, axis=AX.X, op=op), reads, writes)

        nhalf = sb("nhalf", [128, 2], F32)
        G(lambda h: h.memset(nhalf[:], -0.5), writes=(buf("nhalf"),))

        def norm_rstd(src_ap, col, n, src_bufs, bname):
            act(junk[:, 0:n], src_ap, AF.Square, tuple(src_bufs), (buf("junk"), buf(bname)), accum=st[:, col:col + 1])
            return rstd_from_ss(st[:, col:col + 1], st[:, col:col + 1], n, bname)

        def gather(dst, dst_buf, table, col, tname, ei, eib):
            P.op("gpsimd", lambda h: h.indirect_dma_start(out=dst, out_offset=None, in_=table[:, :],
                                                          in_offset=bass.IndirectOffsetOnAxis(ap=ei[:, col:col + 1], axis=0)),
                 reads=(eib, buf(tname)), writes=(dst_buf,), dma_buf=dst_buf)

        hbufs = [(hbuf, buf("hbuf0")), (ybuf, buf("hbuf1"))]
        eidxi2 = sb("eidxi2", [128, 128], I32)
        eidxis = [(eidxi, buf("eidxi0")), (eidxi2[:], buf("eidxi1"))]

        def p3_pre(i):
            tsl = slice(i * 128, (i + 1) * 128)
            hb, hbb = hbufs[i % 2]
            ei, eib = eidxis[i % 2]
            xn2, xnb = xn2s[i % 2]
            gates, gtb = gatess[i % 2]
            rm = norm_rstd(o_mla_sb[:, i, :], 3, 512, (buf(f"o_mla{i}"),), "ssm")
            act(hn_bf[:, 0:512], o_mla_sb[:, i, :], AF.Identity, (buf(f"o_mla{i}"), rm), (buf("hn_bf"),), scale=st[:, 3:4])
            rd = norm_rstd(o_dil_sb[:, i, :], 4, 512, (buf(f"o_dil{i}"),), "ssd")
            act(hn_bf[:, 512:1024], o_dil_sb[:, i, :], AF.Identity, (buf(f"o_dil{i}"), rd), (buf("hn_bf"),), scale=st[:, 4:5])
            transposes(lambda c: hn_bf[:, c * 128:(c + 1) * 128], 8, 128, buf("hn_bf"), hnT[:], buf("hnT"), evac="scalar")
            dma("sync", hb, x_d[tsl, :], hbb, True)
            for cg in range(2):
                pt, pbuf = pf3_ring.next()
                for c in range(8):
                    mm(pt[:, :], hnT[:, c, :], w_out_sb[:, c, cg * 512:(cg + 1) * 512], c == 0, c == 7,
                       (buf("hnT"), w_out_bufs[c]), (pbuf,))
                P.lag(LAG)
                vtt(hb[:, cg * 512:(cg + 1) * 512], hb[:, cg * 512:(cg + 1) * 512], pt[:, :], ALU.add,
                    (hbb, pbuf), (hbb,))
            r2 = norm_rstd(hb, 5, D, (hbb,), "ss2")
            vstt(xn2, hb, st[:, 5:6], gffn_bc3, ALU.mult, ALU.mult, (hbb, r2, buf("gffn")), (xnb,))
            act(hn_bf[:], xn2, AF.Identity, (xnb,), (buf("hn_bf"),))
            transposes(lambda c: hn_bf[:, c * 128:(c + 1) * 128], 8, 128, buf("hn_bf"), hnT[:], buf("hnT"), evac="scalar")
            for g4 in range(2):
                pt, pbuf = pf3_ring.next()
                ptv = pt[:].rearrange("p (h t) -> p h t", t=128)
                for hl in range(4):
                    hd = g4 * 4 + hl
                    for c in range(8):
                        mm(ptv[:, hl, :], w_pq_sb[:, c, hd * 128:(hd + 1) * 128], hnT[:, c, :],
                           hl == 0 and c == 0, hl == 3 and c == 7, (buf("hnT"), w_pq_bufs[c]), (pbuf,))
                act(qTp[:, g4 * 4:(g4 + 1) * 4, :], ptv, AF.Identity, (pbuf,), (buf("qTp"),))
            for g4 in range(2):
                banks = [pf3_ring.next(), pf3_ring.next()]
                for hl in range(4):
                    hd = g4 * 4 + hl
                    for half in range(2):
                        pt, pbuf = banks[half]
                        r0 = half * 64
                        mm(pt[:, hl * 128:(hl + 1) * 128], qTp[r0:r0 + 64, hd, :], keysT[r0:r0 + 64, :], True, True,
                           (buf("qTp"), buf("keysT")), (pbuf,))
                for half in range(2):
                    pt, pbuf = banks[half]
                    j0 = half * 8 + g4 * 4
                    act(Ssc[:, j0 * 128:(j0 + 4) * 128], pt[:, :], AF.Identity, (pbuf,), (buf("Ssc"),))
            P.lag(2 * LAG)
            ohs = [buf(f"ohs{k}") for k in range(8)]
            v12lo = [buf(f"v12lo{j}") for j in range(16)]
            v12hi = [buf(f"v12hi{j}") for j in range(16)]
            i12b = [buf(f"i12_{j}") for j in range(16)]
            for grp in range(2):
                probs = [(grp * 8 + k, k) for k in range(8)]

                def views(j):
                    half, hd = j // 8, j % 8
                    return Ssc[:, j * 128:(j + 1) * 128], v12[:, half, hd, :], i12[:, half, hd, :]
                for (j, k) in probs:
                    Sv, vv, iv = views(j)
                    vmax(vv[:, 0:8], Sv, (buf("Ssc"),), (v12lo[j],))
                for (j, k) in probs:
                    Sv, vv, iv = views(j)
                    vmatch(oh[:, k * 128:(k + 1) * 128], vv[:, 0:8], Sv, (buf("Ssc"), v12lo[j]), (ohs[k],))
                for (j, k) in probs:
                    Sv, vv, iv = views(j)
                    vmax(vv[:, 8:16], oh[:, k * 128:(k + 1) * 128], (ohs[k],), (v12hi[j],))
                for (j, k) in probs:
                    Sv, vv, iv = views(j)
                    vmaxidx(iv[:, 0:8], vv[:, 0:8], Sv, (buf("Ssc"), v12lo[j]), (i12b[j],))
                for (j, k) in probs:
                    Sv, vv, iv = views(j)
                    vmaxidx(iv[:, 8:16], vv[:, 8:16], Sv, (buf("Ssc"), v12hi[j]), (i12b[j],))
            v12all = tuple(v12lo) + tuple(v12hi)
            vcopy(i12f[:], i12[:], tuple(i12b), (buf("i12f"),))
            vts(i12f[:, 0, :, :], i12f[:, 0, :, :], 128.0, ALU.mult, (buf("i12f"),), (buf("i12f"),))
            toplo = [buf(f"toplo{h}") for h in range(8)]
            tophi = [buf(f"tophi{h}") for h in range(8)]
            posb = [buf(f"pos{h}") for h in range(8)]
            for g4 in range(2):
                hs = slice(g4 * 4, (g4 + 1) * 4)
                candv = cand.rearrange("p (h a b) -> p h a b", h=4, a=16)
                vtt(candv, v12[:, 0, hs, :].unsqueeze(3).to_broadcast([128, 4, 16, 16]),
                    v12[:, 1, hs, :].unsqueeze(2).to_broadcast([128, 4, 16, 16]), ALU.add, v12all, (buf("cand"),))
                hds = [(g4 * 4 + hl, hl) for hl in range(4)]
                for (hd, hl) in hds:
                    vmax(top[:, hd, 0:8], cand[:, hl * 256:(hl + 1) * 256], (buf("cand"),), (toplo[hd],))
                for (hd, hl) in hds:
                    vmatch(oh[:, hl * 256:(hl + 1) * 256], top[:, hd, 0:8], cand[:, hl * 256:(hl + 1) * 256],
                           (buf("cand"), toplo[hd]), (ohs[2 * hl], ohs[2 * hl + 1]))
                for (hd, hl) in hds:
                    vmax(top[:, hd, 8:16], oh[:, hl * 256:(hl + 1) * 256], (ohs[2 * hl], ohs[2 * hl + 1]), (tophi[hd],))
                for (hd, hl) in hds:
                    vmaxidx(pos[:, hd, 0:8], top[:, hd, 0:8], cand[:, hl * 256:(hl + 1) * 256], (buf("cand"), toplo[hd]), (posb[hd],))
                for (hd, hl) in hds:
                    vmaxidx(pos[:, hd, 8:16], top[:, hd, 8:16], cand[:, hl * 256:(hl + 1) * 256], (buf("cand"), tophi[hd]), (posb[hd],))
            topall = tuple(toplo) + tuple(tophi)
            posall = tuple(posb)
            ohall = (buf("oh"),) + tuple(ohs)
            vts(ai[:], pos[:], 4, ALU.logical_shift_right, posall, (buf("ai"),))
            vts(bi[:], pos[:], 15, ALU.bitwise_and, posall, (buf("bi"),))
            vcopy(af[:], ai[:], (buf("ai"),), (buf("af"),))
            vcopy(bf[:], bi[:], (buf("bi"),), (buf("bf"),))
            iob = iota16[:].unsqueeze(1).unsqueeze(1).to_broadcast([128, 4, 16, 16])
            for (selv, srcv, half, sname, fname) in ((i1sel, af, 0, "i1sel", "af"), (i2sel, bf, 1, "i2sel", "bf")):
                for g4 in range(2):
                    hs = slice(g4 * 4, (g4 + 1) * 4)
                    ohv = oh.rearrange("p (h k a) -> p h k a", h=4, k=16)
                    vtt(ohv, iob, srcv[:, hs, :].unsqueeze(3).to_broadcast([128, 4, 16, 16]), ALU.is_equal,
                        (buf("iota16"), buf(fname)), ohall)
                    vtt(ohv, ohv, i12f[:, half, hs, :].unsqueeze(2).to_broadcast([128, 4, 16, 16]), ALU.mult,
                        ohall + (buf("i12f"),), ohall)
                    vreduce(selv[:, hs, :], ohv, ALU.add, ohall, (buf(sname),))
            vtt(eidxf, i1sel.rearrange("p h k -> p (h k)"), i2sel.rearrange("p h k -> p (h k)"), ALU.add,
                (buf("i1sel"), buf("i2sel")), (buf("eidxf"),))
            vcopy(ei, eidxf, (buf("eidxf"),), (eib,))
            vtt(gates[:], top[:], top[:, :, 0:1].to_broadcast([128, 8, 16]), ALU.subtract, topall, (gtb,))
            act(gates[:], gates[:], AF.Tanh, (gtb,), (gtb,), scale=0.5)
            P.lag(LAG)
            gden = cand[:, 0:128].rearrange("p (h k) -> p h k", h=8)
            vts(gden, gates[:], -1.0, ALU.mult, (gtb,), (buf("cand"),), s2=1.0, op1=ALU.add)
            vrecip(gden, gden, (buf("cand"),), (buf("cand"),))
            vstt(gates[:], gates[:], 1.0, gden, ALU.add, ALU.mult, (gtb, buf("cand")), (gtb,))
            P.lag(LAG)
            vreduce(st[:, 8:16], gates[:], ALU.add, (gtb,), (buf("gsum"),))
            vrecip(st[:, 8:16], st[:, 8:16], (buf("gsum"),), (buf("gsum"),))
            vtt(gates[:], gates[:], st[:, 8:16].unsqueeze(2).to_broadcast([128, 8, 16]), ALU.mult,
                (gtb, buf("gsum")), (gtb,))

        def p3_uv(i, stepper):
            ei, eib = eidxis[i % 2]
            xn2, xnb = xn2s[i % 2]
            gates, gtb = gatess[i % 2]
            gflat = gates.rearrange("p h k -> p (h k)")
            yps = [(pf[2 + 2 * (i % 2) + k], buf(f"pf{2 + 2 * (i % 2) + k}")) for k in range(2)]
            G = 1
            held = []

            def finish_group(g0):
                act(gelu_t[:, g0:g0 + G], avals[:, g0:g0 + G], AF.Gelu, tuple(buf(f"avals{j}") for j in range(g0, g0 + G)),
                    (buf(f"gelu{g0}"),))
                for (j, ub, ubb) in held:
                    act(coef[:, j:j + 1], gelu_t[:, j:j + 1], AF.Identity, (buf(f"gelu{g0}"), gtb), (buf(f"coef{j}"),),
                        scale=gflat[:, j:j + 1])
                    dg, dgb = diag_ring.next()
                    act(dg, ident[:], AF.Identity, (buf("ident"), buf(f"coef{j}")), (dgb,), scale=coef[:, j:j + 1])
                    for cg in range(2):
                        mm(yps[cg][0][:, :], dg, ub[:, D + cg * 512:D + (cg + 1) * 512], j == 0, j == 127,
                           (dgb, ubb), (yps[cg][1],))
                del held[:]

            for j in range(128):
                ub, ubb = uv_ring.next()
                gather(ub, ubb, uv16_d, j, "uv16", ei, eib)
                vstt(ub[:, 0:D], ub[:, 0:D], 1.0, xn2, ALU.mult, ALU.mult, (ubb, xnb), (ubb, buf(f"avals{j}")),
                     accum=avals[:, j:j + 1])
                held.append((j, ub, ubb))
                if j % G == G - 1:
                    finish_group(j - G + 1)
                if stepper is not None:
                    stepper.advance(STEP_K)
            return yps

        def p3_post(i, yps):
            tsl = slice(i * 128, (i + 1) * 128)
            hb, hbb = hbufs[i % 2]
            for cg in range(2):
                cs = slice(cg * 512, (cg + 1) * 512)
                vtt(hb[:, cs], hb[:, cs], yps[cg][0][:, :], ALU.add, (hbb, yps[cg][1]), (hbb,))
            r3 = norm_rstd(hb, 6, D, (hbb,), "ss3")
            vts(hn_bf[:], hb, st[:, 6:7], ALU.mult, (hbb, r3), (buf("hn_bf"),))
            transposes(lambda c: hn_bf[:, c * 128:(c + 1) * 128], 8, 128, buf("hn_bf"), hnT[:], buf("hnT"))
            dma("sync", ptile, p_d[tsl, :], buf("ptile"), True)
            vcopy(p_bf, ptile, (buf("ptile"),), (buf("p_bf"),))
            transposes(lambda c: p_bf[:, c * 128:(c + 1) * 128], 2, 128, buf("p_bf"), pTt, buf("pTt"))
            for cg in range(2):
                cs = slice(cg * 512, (cg + 1) * 512)
                pg, pgb = pf3_ring.next()
                for c in range(8):
                    mm(pg[:, :], hnT[:, c, :], w_pg_sb[:, c, cs], c == 0, c == 7, (buf("hnT"), w_pg_bufs[c]), (pgb,))
                pp, ppb = pf3_ring.next()
                for c in range(2):
                    mm(pp[:, :], pTt[:, c, :], w_pp_sb[:, c, cs], c == 0, c == 1, (buf("pTt"), buf(f"w_pp{c}")), (ppb,))
                act(gate_sb, pg[:, :], AF.Tanh, (pgb,), (buf("gate_sb"),), scale=0.5)
                P.lag(LAG)
                vstt(gate_sb, gate_sb, 1.0, pp[:, :], ALU.add, ALU.mult, (buf("gate_sb"), ppb), (buf("gate_sb"),))
                vstt(hb[:, cs], gate_sb, 0.5, hb[:, cs], ALU.mult, ALU.add, (hbb, buf("gate_sb")), (hbb,))
            r4 = norm_rstd(hb, 7, D, (hbb,), "ss4")
            vstt(hb, hb, st[:, 7:8], gfin_bc3, ALU.mult, ALU.mult, (hbb, r4, buf("gfin")), (hbb,))
            tok = dma("sync", out_d[tsl, :], hb, hbb, False)
            P.final_tokens.append(tok)


        ntr = NT if NT_RUN is None else NT_RUN
        p3_pre(0)
        prev = None
        for i in range(ntr):
            def woven(i=i, prev=prev):
                if prev is not None:
                    p3_post(*prev)
                if i + 1 < ntr:
                    p3_pre(i + 1)
            stepper = Stepper(P, woven)
            yps = p3_uv(i, stepper)
            stepper.finish()
            prev = (i, yps)
        p3_post(*prev)

    P.op("sync", None, extra=tuple(P.final_tokens))

    P.finalize_marks()
    esems = {e: es.enter_context(nc.semaphore(f"sem_{e}")) for e in Prog.ENGS}
    for b in P.dma_bufs:
        b.dsem = es.enter_context(nc.semaphore(f"ds_{b.name}"))
    block = es.enter_context(nc.Block())

    @block.sync
    def _(h):
        P.emit("sync", h, esems)

    @block.scalar
    def _(h):
        P.emit("scalar", h, esems)

    @block.vector
    def _(h):
        P.emit("vector", h, esems)

    @block.gpsimd
    def _(h):
        P.emit("gpsimd", h, esems)

    @block.tensor
    def _(h):
        P.emit("tensor", h, esems)

    es.close()
    return nc, dbg, declared


def _constants():
    ident = np.eye(128, dtype=np.float32)
    t = np.arange(S, dtype=np.float32)

    def tables(rot):
        inv = (np.float32(500000.0) ** (-np.arange(0, rot, 2, dtype=np.float32) / np.float32(rot))).astype(np.float32)
        ang = (t[:, None] * inv[None, :]).astype(np.float32)
        return np.cos(ang).astype(np.float32), np.sin(ang).astype(np.float32)

    cm, sm = tables(32)
    cd, sd = tables(16)
    ropem = np.stack([cm, sm], axis=1).reshape(NT, 128, 2, 16).transpose(1, 0, 2, 3).copy()
    roped = np.stack([cd, sd], axis=1).reshape(NT, 128, 2, 8).transpose(1, 0, 2, 3).copy()
    u = np.arange(2944)[None, :]
    p = np.arange(128)[:, None]
    d = u - p - 1408
    ad = np.abs(d)
    c = (ad <= 64).astype(np.float32) + ((d % 4 == 0) & (ad <= 256)) + ((d % 16 == 0) & (ad <= 1024))
    iota16 = np.tile(np.arange(16, dtype=np.float32)[None, :], (128, 1))
    return ident, ropem, roped, c.astype(np.float32), iota16


_CACHE = {}


def kernel(**inputs):
    return _run(inputs)


def _prep_maps(inputs):
    ident, ropem, roped, mask, iota16 = _constants()
    f = lambda a: np.ascontiguousarray(np.asarray(a, dtype=np.float32))

    def col(g, n):
        return f(g).reshape(n, 128).T

    gcols = np.concatenate([
        col(inputs["g_mix"][0], 8), col(inputs["g_cq"][0], 3), col(inputs["g_ckv"][0], 2),
        col(inputs["g_out_mla"][0], 4), col(inputs["g_out_dil"][0], 4), col(inputs["g_ple"][0], 8),
        col(inputs["g_ffn"][0], 8)], axis=1)
    gcols = np.ascontiguousarray(gcols)
    keys12 = np.ascontiguousarray(np.concatenate([f(inputs["peer_keys1"][0]), f(inputs["peer_keys2"][0])], axis=1))
    shared = {
        "w_in": f(inputs["w_in"][0]), "w_uq": f(inputs["w_uq"][0]), "w_ukv": f(inputs["w_ukv"][0]),
        "w_out": f(inputs["w_out"][0]), "w_peer_q": f(inputs["w_peer_q"][0]), "keys12": keys12,
        "peer_u": f(inputs["peer_u"][0]), "peer_v": f(inputs["peer_v"][0]),
        "w_ple_gate": f(inputs["w_ple_gate"][0]), "w_ple_proj": f(inputs["w_ple_proj"][0]),
        "g_ffn": f(inputs["g_ffn"][0]), "g_final": f(inputs["g_final"]), "gcols": gcols,
        "ident": ident, "ropem": ropem, "roped": roped, "dilmask": mask, "iota16": iota16,
    }
    x = f(inputs["x"])
    p = f(inputs["p"][0])
    return [dict(shared, x=x[b], p=p[b]) for b in range(NCORES)]


def _run(inputs, debug=False, phases=3, cores=NCORES):
    nc, dbg, declared = build_program(debug=debug, phases=phases)
    maps = [{k: m[k] for k in declared} for m in _prep_maps(inputs)[:cores]]
    res = run_bass_kernel_spmd(nc, maps, core_ids=list(range(cores)))
    if debug:
        return res
    return np.stack([r["out"] for r in res.results], axis=0).astype(np.float32)
```

```python
import threading
from contextlib import ExitStack

import numpy as np
import ml_dtypes

import concourse.bass as bass
import concourse.mybir as mybir
from concourse.bass_utils import run_bass_kernel_spmd

F32 = mybir.dt.float32
BF16 = mybir.dt.bfloat16
I32 = mybir.dt.int32
U32 = mybir.dt.uint32
ALU = mybir.AluOpType
AF = mybir.ActivationFunctionType
AX = mybir.AxisListType

S = 2048
D = 1024
NT = 16
EPS = 1e-6
IN_W = 2208
NCORES = 8
DBG_ONLY = None
STOP = None
LVL = 99
TL = 99
P3L = 99
NT_RUN = None


class Buf:
    def __init__(self, name, dma=False):
        self.name = name
        self.w = None
        self.r = []
        self.dma = dma
        self.dsem = None
        self.dcount = 0


class Prog:
    ENGS = ("sync", "scalar", "vector", "gpsimd", "tensor")

    def __init__(self):
        self.ops = {e: [] for e in self.ENGS}
        self.waited = {e: {} for e in self.ENGS}
        self.dma_bufs = []
        self.final_tokens = []
        self.hook = None

    def _need(self, eng, tok, kind):
        if tok[0] == "eng" and tok[1] == eng:
            if eng == "tensor":
                return False
        return True

    def op(self, eng, fn, reads=(), writes=(), dma_buf=None, extra=()):
        deps = []
        for b in reads:
            if b.w is not None:
                deps.append((b.w, "raw"))
            if b.name.startswith("pf") or b.name.startswith("pb"):
                for t in b.r:
                    if not (t[0] == "eng" and t[1] == eng):
                        deps.append((t, "rar"))
        for b in writes:
            if b.w is not None:
                deps.append((b.w, "waw"))
            for t in b.r:
                deps.append((t, "war"))
        for t in extra:
            deps.append((t, "raw"))
        waits = []
        wd = self.waited[eng]
        for tok, kind in deps:
            if not self._need(eng, tok, kind):
                continue
            key = (tok[0], tok[1] if tok[0] == "eng" else id(tok[1]))
            if wd.get(key, -1) >= tok[2]:
                continue
            wd[key] = tok[2]
            waits.append(tok)
        seq = len(self.ops[eng])
        if dma_buf is not None:
            if dma_buf.dsem is None:
                self.dma_bufs.append(dma_buf)
                dma_buf.dsem = True
            dma_buf.dcount += 16
            tok = ("dma", dma_buf, dma_buf.dcount)
        else:
            tok = ("eng", eng, seq)
        self.ops[eng].append(dict(fn=fn, waits=waits, dma_buf=dma_buf, marked=False))
        for b in reads:
            b.r.append(tok)
        for b in writes:
            b.w = tok
            b.r = []
        if self.hook is not None:
            self.hook.tick()
        return tok

    def lag(self, n):
        if self.hook is not None:
            for _ in range(n):
                self.hook.tick()

    def finalize_marks(self):
        for e in self.ENGS:
            for o in self.ops[e]:
                for tok in o["waits"]:
                    if tok[0] == "eng":
                        self.ops[tok[1]][tok[2]]["marked"] = True
        self.count_at = {}
        for e in self.ENGS:
            c = 0
            arr = []
            for o in self.ops[e]:
                if o["marked"]:
                    c += 1
                arr.append(c)
            self.count_at[e] = arr

    def emit(self, eng, handle, esems):
        for o in self.ops[eng]:
            for tok in o["waits"]:
                if tok[0] == "eng":
                    handle.wait_ge(esems[tok[1]], self.count_at[tok[1]][tok[2]])
                else:
                    handle.wait_ge(tok[1].dsem, tok[2])
            if o["fn"] is None:
                continue
            ins = o["fn"](handle)
            if o["dma_buf"] is not None:
                ins.then_inc(o["dma_buf"].dsem, 16)
            elif o["marked"]:
                ins.then_inc(esems[eng], 1)


class Stepper:
    def __init__(self, prog, fn):
        self.prog = prog
        self.fn = fn
        self.go = threading.Semaphore(0)
        self.back = threading.Semaphore(0)
        self.finished = False
        self.budget = 0
        self.err = None
        self.th = threading.Thread(target=self._run, daemon=True)
        self.th.start()

    def _run(self):
        self.go.acquire()
        try:
            self.prog.hook = self
            self.fn()
        except BaseException as e:
            self.err = e
        self.prog.hook = None
        self.finished = True
        self.back.release()

    def tick(self):
        if threading.current_thread() is not self.th:
            return
        self.budget -= 1
        if self.budget <= 0:
            self.prog.hook = None
            self.back.release()
            self.go.acquire()
            self.prog.hook = self

    def advance(self, k):
        if self.finished:
            return
        self.budget = k
        self.go.release()
        self.back.acquire()
        if self.err is not None:
            raise self.err

    def finish(self):
        while not self.finished:
            self.advance(1 << 30)


STEP_K = 4
LAG = 8


class Ring:
    def __init__(self, items):
        self.items = items
        self.i = 0

    def next(self):
        it = self.items[self.i % len(self.items)]
        self.i += 1
        return it


def build_program(debug=False, phases=3):
    nc = bass.Bass("TRN2", target_bir_lowering=False)
    P = Prog()
    es = ExitStack()

    declared = []

    def din(name, shape, dt=F32, need=1):
        if phases < need:
            return None
        declared.append(name)
        return nc.dram_tensor(name, list(shape), dt, kind="ExternalInput").ap()

    def dout(name, shape, dt=F32):
        return nc.dram_tensor(name, list(shape), dt, kind="ExternalOutput").ap()

    x_d = din("x", [S, D])
    p_d = din("p", [S, 256], need=3)
    w_in_d = din("w_in", [D, IN_W])
    w_uq_d = din("w_uq", [384, 768])
    w_ukv_d = din("w_ukv", [256, 1024])
    w_out_d = din("w_out", [D, D], need=3)
    w_pq_d = din("w_peer_q", [D, D], need=3)
    keys_d = din("keys12", [128, 128], need=3)
    u_d = din("peer_u", [16384, D], need=3)
    v_d = din("peer_v", [16384, D], need=3)
    w_pg_d = din("w_ple_gate", [D, D], need=3)
    w_pp_d = din("w_ple_proj", [256, D], need=3)
    g_ffn_d = din("g_ffn", [D], need=3)
    g_fin_d = din("g_final", [D], need=3)
    gcols_d = din("gcols", [128, 37])
    ident_d = din("ident", [128, 128])
    ropem_d = din("ropem", [128, NT, 2, 16])
    roped_d = din("roped", [128, NT, 2, 8])
    mask_d = din("dilmask", [128, 2944])
    iota_d = din("iota16", [128, 16], need=3)
    out_d = dout("out", [S, D])
    if phases >= 3:
        uv16_d = nc.dram_tensor("uv16", [16384, 2 * D], BF16, kind="Internal").ap()
    dbg = {}

    def sb(name, shape, dt):
        return es.enter_context(nc.sbuf_tensor("s_" + name, list(shape), dt))

    def ps(name, shape, dt):
        return es.enter_context(nc.psum_tensor("p_" + name, list(shape), dt))

    regA = sb("regA", [128, 22016], BF16)
    regB = sb("regB", [128, 65792], BF16)
    w_in_sb = regA[:, 0:17664].rearrange("p (c n) -> p c n", n=IN_W)
    w_uq_sb = regA[:, 17664:19968].rearrange("p (c n) -> p c n", n=768)
    w_ukv_sb = regA[:, 19968:22016].rearrange("p (c n) -> p c n", n=1024)
    o_mla_sb = regA[:, 0:8192].rearrange("p (i n) -> p i n", n=512)
    o_dil_sb = regA[:, 8192:16384].rearrange("p (i n) -> p i n", n=512)
    QT = regB[:, 0:16384].rearrange("p (h t) -> p h t", t=S)
    KT = regB[:, 16384:32768].rearrange("p (h t) -> p h t", t=S)
    QdT = regB[:, 32768:40960].rearrange("p (h t) -> p h t", t=S)
    KdT = regB[:, 40960:49152].rearrange("p (h t) -> p h t", t=S)
    Vaug = regB[:, 49152:57472].rearrange("p (i h d) -> p i h d", h=8, d=65)
    Vdaug = regB[:, 57472:65792].rearrange("p (i h d) -> p i h d", h=8, d=65)
    w_out_sb = regB[:, 0:8192].rearrange("p (c n) -> p c n", n=D)
    w_pq_sb = regB[:, 8192:16384].rearrange("p (c n) -> p c n", n=D)
    w_pg_sb = regB[:, 16384:24576].rearrange("p (c n) -> p c n", n=D)
    w_pp_sb = regB[:, 24576:26624].rearrange("p (c n) -> p c n", n=D)
    NG = 8
    ubufs = [regB[:, 26624 + k * 2048: 26624 + (k + 1) * 2048].bitcast(F32) for k in range(NG)]
    vbufs = [regB[:, 43008 + k * 2048: 43008 + (k + 1) * 2048].bitcast(F32) for k in range(NG)]
    gffn_bc = regB[:, 59392:61440].bitcast(F32)
    gfin_bc = regB[:, 61440:63488].bitcast(F32)

    ident = sb("ident", [128, 128], BF16)
    identf = sb("identf", [128, 128], F32)
    gcols = sb("gcols", [128, 37], F32)
    p1w = sb("p1w", [128, 9408], BF16)
    ropem = p1w[:, 7872:8896].bitcast(F32).rearrange("p (i a k) -> p i a k", a=2, k=16)
    roped = p1w[:, 8896:9408].bitcast(F32).rearrange("p (i a k) -> p i a k", a=2, k=8)
    dmask = p1w[:, 4928:7872]
    iota16 = sb("iota16", [128, 16], F32)
    keysT = sb("keysT", [128, 128], BF16)
    xt = [sb(f"xt{k}", [128, D], F32) for k in range(2)]
    junk = sb("junk", [128, D], BF16)
    hn_bf = sb("hn_bf", [128, D], BF16)
    hnT = sb("hnT", [128, 8, 128], BF16)
    st = sb("st", [128, 16], F32)
    cn_bf = p1w[:, 0:640]
    cnT = p1w[:, 640:1280].rearrange("p (c t) -> p c t", t=128)
    q_bf = p1w[:, 1280:2048].rearrange("p (h d) -> p h d", d=96)
    k_bf = p1w[:, 2048:2816].rearrange("p (h d) -> p h d", d=96)
    krope = p1w[:, 2816:2880].bitcast(F32)
    qd_bf = p1w[:, 2880:3392]
    kd_bf = p1w[:, 3392:3904]
    rt = p1w[:, 3904:4928].bitcast(F32).rearrange("p (a h k) -> p a h k", a=4, h=8)

    pf = [ps(f"pf{k}", [128, 512], F32) for k in range(6)]
    pb = [ps(f"pb{k}", [128, 1024], BF16) for k in range(2)]

    B = {}

    def buf(name, dma=False):
        if name not in B:
            B[name] = Buf(name, dma)
        return B[name]

    pf_ring = Ring([(pf[k], buf(f"pf{k}")) for k in range(6)])
    pb_ring = Ring([(pb[k], buf(f"pb{k}")) for k in range(2)])

    def dma(eng, out, in_, sbuf_buf, is_load, extra_reads=(), extra_writes=()):
        sbuf_buf.dma = True
        if is_load:
            return P.op(eng, lambda h: h.dma_start(out=out, in_=in_), reads=extra_reads,
                        writes=(sbuf_buf,) + tuple(extra_writes), dma_buf=sbuf_buf)
        return P.op(eng, lambda h: h.dma_start(out=out, in_=in_), reads=(sbuf_buf,) + tuple(extra_reads),
                    writes=extra_writes, dma_buf=sbuf_buf)

    def V(fn, reads=(), writes=()):
        return P.op("vector", fn, reads, writes)

    def A(fn, reads=(), writes=()):
        return P.op("scalar", fn, reads, writes)

    def G(fn, reads=(), writes=()):
        return P.op("gpsimd", fn, reads, writes)

    def T(fn, reads=(), writes=()):
        return P.op("tensor", fn, reads, writes)

    def transposes(src_ap_fn, n, rows, src_buf, dst_ap, dst_buf, evac="vector"):
        pt, pbuf = pb_ring.next()
        ptv = pt[:].rearrange("p (c t) -> p c t", t=128)
        for c in range(n):
            T(lambda h, c=c: h.transpose(out=ptv[0:rows, c, :], in_=src_ap_fn(c), identity=ident[:]),
              reads=(src_buf, buf("ident")), writes=(pbuf,))
        if evac == "vector":
            V(lambda h: h.tensor_copy(out=dst_ap, in_=ptv[0:rows, 0:n, :]), reads=(pbuf,), writes=(dst_buf,))
        else:
            A(lambda h: h.activation(out=dst_ap, in_=ptv[0:rows, 0:n, :], func=AF.Identity), reads=(pbuf,), writes=(dst_buf,))

    def rstd_from_ss(ss_ap, out_ap, n, bname):
        A(lambda h: h.activation(out=out_ap, in_=ss_ap, func=AF.Sqrt, bias=EPS, scale=1.0 / n),
          reads=(buf(bname),), writes=(buf(bname),))
        P.lag(LAG)
        V(lambda h: h.reciprocal(out=out_ap, in_=out_ap), reads=(buf(bname),), writes=(buf(bname),))
        return buf(bname)

    dma("sync", identf[:], ident_d[:, :], buf("identf"), True)
    dma("sync", gcols[:], gcols_d[:, :], buf("gcols"), True)
    dma("sync", ropem[:], ropem_d[:], buf("ropem"), True)
    dma("sync", roped[:], roped_d[:], buf("roped"), True)
    if phases >= 3:
        dma("sync", iota16[:], iota_d[:, :], buf("iota16"), True)
    dma("gpsimd", ident[:], ident_d[:, :], buf("ident"), True)
    if LVL >= 1:
        dma("gpsimd", dmask[:, 0:1472], mask_d[:, 0:1472], buf("dmask"), True)
        dma("gpsimd", dmask[:, 1472:2944], mask_d[:, 1472:2944], buf("dmask"), True)
    for c in range(8):
        if LVL < 2:
            break
        for hlf in range(2):
            cs = slice(hlf * 1104, (hlf + 1) * 1104)
            dma("gpsimd", w_in_sb[:, c, cs], w_in_d[c * 128:(c + 1) * 128, cs], buf(f"w_in{c}"), True)
        if LVL < 3:
            continue
        V(lambda h, c=c: h.tensor_scalar(out=w_in_sb[:, c, :], in0=w_in_sb[:, c, :], scalar1=gcols[:, c:c + 1],
                                         scalar2=None, op0=ALU.mult),
          reads=(buf(f"w_in{c}"), buf("gcols")), writes=(buf(f"w_in{c}"),))
    for c in range(3):
        if LVL < 4:
            break
        dma("gpsimd", w_uq_sb[:, c, :], w_uq_d[c * 128:(c + 1) * 128, :], buf(f"w_uq{c}"), True)
        V(lambda h, c=c: h.tensor_scalar(out=w_uq_sb[:, c, :], in0=w_uq_sb[:, c, :], scalar1=gcols[:, 8 + c:9 + c],
                                         scalar2=None, op0=ALU.mult),
          reads=(buf(f"w_uq{c}"), buf("gcols")), writes=(buf(f"w_uq{c}"),))
    for c in range(2):
        if LVL < 4:
            break
        dma("gpsimd", w_ukv_sb[:, c, :], w_ukv_d[c * 128:(c + 1) * 128, :], buf(f"w_ukv{c}"), True)
        V(lambda h, c=c: h.tensor_scalar(out=w_ukv_sb[:, c, :], in0=w_ukv_sb[:, c, :], scalar1=gcols[:, 11 + c:12 + c],
                                         scalar2=None, op0=ALU.mult),
          reads=(buf(f"w_ukv{c}"), buf("gcols")), writes=(buf(f"w_ukv{c}"),))
    if debug and LVL >= 5:
        V(lambda h: h.memset(regB[:, 0:32896], 0.0), writes=(buf("dbgms"),))
        V(lambda h: h.memset(regB[:, 32896:65792], 0.0),
          writes=tuple(buf(f"{n}{i}") for n in ("QT", "KT", "QdT", "KdT", "Vm", "Vd") for i in range(NT)) + (buf("Vaug_ones"), buf("Vdaug_ones")))
    if LVL >= 6:
        V(lambda h: h.memset(Vaug[:, :, :, 64:65], 1.0), writes=(buf("Vaug_ones"),))
        V(lambda h: h.memset(Vdaug[:, :, :, 64:65], 1.0), writes=(buf("Vdaug_ones"),))

    if phases >= 3:
        for (src, c0) in ((u_d, 0), (v_d, D)):
            for k in range(8):
                rs = slice(k * 2048, (k + 1) * 2048)
                dma("gpsimd", uv16_d[rs, c0:c0 + D], src[rs, :], buf("uv16"), True)
    MLA_SCALE = 96.0 ** -0.5
    w_in_bufs = tuple(buf(f"w_in{c}") for c in range(8))

    def phase1_tile(i):
        tsl = slice(i * 128, (i + 1) * 128)
        xb = xt[i % 2]
        xbuf = buf(f"xt{i % 2}")
        dma("sync", xb[:], x_d[tsl, :], xbuf, True)
        A(lambda h, xb=xb: h.activation(out=junk[:], in_=xb[:], func=AF.Square, accum_out=st[:, 0:1]),
          reads=(xbuf,), writes=(buf("junk"), buf("ssx")))
        rb = rstd_from_ss(st[:, 0:1], st[:, 0:1], D, "ssx")
        V(lambda h, xb=xb: h.tensor_scalar(out=hn_bf[:], in0=xb[:], scalar1=st[:, 0:1], scalar2=None, op0=ALU.mult),
          reads=(xbuf, rb), writes=(buf("hn_bf"),))
        transposes(lambda c: hn_bf[:, c * 128:(c + 1) * 128], 8, 128, buf("hn_bf"), hnT[:], buf("hnT"))

        if TL < 1:
            return
        def proj_group(c0, c1):
            pt, pbuf = pf_ring.next()
            for c in range(8):
                T(lambda h, c=c, pt=pt: h.matmul(pt[:, 0:c1 - c0], lhsT=hnT[:, c, :], rhs=w_in_sb[:, c, c0:c1],
                                                 start=(c == 0), stop=(c == 7)),
                  reads=(buf("hnT"), w_in_bufs[c]), writes=(pbuf,))
            return pt, pbuf

        pt, pbuf = proj_group(0, 384)
        A(lambda h, pt=pt: h.activation(out=junk[:, 0:384], in_=pt[:, 0:384], func=AF.Square, accum_out=st[:, 1:2]),
          reads=(pbuf,), writes=(buf("junk"), buf("ssq")))
        rq = rstd_from_ss(st[:, 1:2], st[:, 1:2], 384, "ssq")
        V(lambda h, pt=pt: h.tensor_scalar(out=cn_bf[:, 0:384], in0=pt[:, 0:384], scalar1=st[:, 1:2], scalar2=None,
                                           op0=ALU.mult),
          reads=(pbuf, rq), writes=(buf("cn_q"),))
        if TL < 2:
            return
        pt, pbuf = proj_group(384, 672)
        A(lambda h, pt=pt: h.activation(out=junk[:, 0:256], in_=pt[:, 0:256], func=AF.Square, accum_out=st[:, 2:3]),
          reads=(pbuf,), writes=(buf("junk"), buf("sskv")))
        rkv = rstd_from_ss(st[:, 2:3], st[:, 2:3], 256, "sskv")
        V(lambda h, pt=pt: h.tensor_scalar(out=cn_bf[:, 384:640], in0=pt[:, 0:256], scalar1=st[:, 2:3], scalar2=None,
                                           op0=ALU.mult),
          reads=(pbuf, rkv), writes=(buf("cn_kv"),))
        cm = ropem[:, i, 0, :]
        sm = ropem[:, i, 1, :]
        V(lambda h, pt=pt: h.tensor_tensor(out=krope[:, 0:16], in0=pt[:, 256:272], in1=cm, op=ALU.mult),
          reads=(pbuf, buf("ropem")), writes=(buf("krope_a"),))
        V(lambda h, pt=pt: h.tensor_tensor(out=rt[:, 0, 0, :], in0=pt[:, 272:288], in1=sm, op=ALU.mult),
          reads=(pbuf, buf("ropem")), writes=(buf("rt0"),))
        V(lambda h: h.tensor_tensor(out=krope[:, 0:16], in0=krope[:, 0:16], in1=rt[:, 0, 0, :], op=ALU.subtract),
          reads=(buf("krope_a"), buf("rt0")), writes=(buf("krope_a"),))
        V(lambda h, pt=pt: h.tensor_tensor(out=krope[:, 16:32], in0=pt[:, 272:288], in1=cm, op=ALU.mult),
          reads=(pbuf, buf("ropem")), writes=(buf("krope_b"),))
        V(lambda h, pt=pt: h.tensor_tensor(out=rt[:, 1, 0, :], in0=pt[:, 256:272], in1=sm, op=ALU.mult),
          reads=(pbuf, buf("ropem")), writes=(buf("rt1"),))
        V(lambda h: h.tensor_tensor(out=krope[:, 16:32], in0=krope[:, 16:32], in1=rt[:, 1, 0, :], op=ALU.add),
          reads=(buf("krope_b"), buf("rt1")), writes=(buf("krope_b"),))
        V(lambda h: h.tensor_copy(out=k_bf[:, :, 64:96], in_=krope[:].unsqueeze(1).to_broadcast([128, 8, 32])),
          reads=(buf("krope_a"), buf("krope_b")), writes=(buf("k_bf_r"),))

        if TL < 3:
            return
        def dil_rope(pt, pbuf, dst, dname, scale):
            cd = roped[:, i, 0, :].unsqueeze(1).to_broadcast([128, 8, 8])
            sd = roped[:, i, 1, :].unsqueeze(1).to_broadcast([128, 8, 8])
            pv = pt[:].rearrange("p (h d) -> p h d", d=64)
            dv = dst[:].rearrange("p (h d) -> p h d", d=64)
            x1 = pv[:, :, 0:8]
            x2 = pv[:, :, 8:16]
            t0 = rt[:, 0, :, 0:8]
            t1 = rt[:, 1, :, 0:8]
            t2 = rt[:, 2, :, 0:8]
            t3 = rt[:, 3, :, 0:8]
            V(lambda h: h.tensor_tensor(out=t0, in0=x1, in1=cd, op=ALU.mult), reads=(pbuf, buf("roped")), writes=(buf("rt0"),))
            V(lambda h: h.tensor_tensor(out=t1, in0=x2, in1=sd, op=ALU.mult), reads=(pbuf, buf("roped")), writes=(buf("rt1"),))
            V(lambda h: h.tensor_tensor(out=t2, in0=x2, in1=cd, op=ALU.mult), reads=(pbuf, buf("roped")), writes=(buf("rt2"),))
            V(lambda h: h.tensor_tensor(out=t3, in0=x1, in1=sd, op=ALU.mult), reads=(pbuf, buf("roped")), writes=(buf("rt3"),))
            V(lambda h: h.tensor_tensor(out=t0, in0=t0, in1=t1, op=ALU.subtract),
              reads=(buf("rt0"), buf("rt1")), writes=(buf("rt0"),))
            V(lambda h: h.tensor_tensor(out=t2, in0=t2, in1=t3, op=ALU.add),
              reads=(buf("rt2"), buf("rt3")), writes=(buf("rt2"),))
            V(lambda h: h.tensor_scalar(out=dv[:, :, 0:8], in0=t0, scalar1=scale, scalar2=None, op0=ALU.mult),
              reads=(buf("rt0"),), writes=(buf(dname + "a"),))
            V(lambda h: h.tensor_scalar(out=dv[:, :, 8:16], in0=t2, scalar1=scale, scalar2=None, op0=ALU.mult),
              reads=(buf("rt2"),), writes=(buf(dname + "b"),))
            V(lambda h: h.tensor_scalar(out=dv[:, :, 16:64], in0=pv[:, :, 16:64], scalar1=scale, scalar2=None, op0=ALU.mult),
              reads=(pbuf,), writes=(buf(dname + "c"),))
            return (buf(dname + "a"), buf(dname + "b"), buf(dname + "c"))

        pt, pbuf = proj_group(672, 1184)
        qd_bufs = dil_rope(pt, pbuf, qd_bf, "qd_bf", 0.125)
        if TL < 3.3:
            return
        pt, pbuf = proj_group(1184, 1696)
        kd_bufs = dil_rope(pt, pbuf, kd_bf, "kd_bf", 1.0)
        if TL < 3.6:
            return
        pt, pbuf = proj_group(1696, 2208)
        A(lambda h, pt=pt: h.activation(out=Vdaug[:, i, :, 0:64], in_=pt[:].rearrange("p (h d) -> p h d", d=64), func=AF.Identity),
          reads=(pbuf,), writes=(buf(f"Vd{i}"),))

        if TL < 4:
            return
        pt_, pbuf_ = pb_ring.next()
        ptv = pt_[:].rearrange("p (c t) -> p c t", t=128)
        for c in range(4):
            T(lambda h, c=c, ptv=ptv: h.transpose(out=ptv[:, c, :], in_=qd_bf[:, c * 128:(c + 1) * 128], identity=ident[:]),
              reads=qd_bufs + (buf("ident"),), writes=(pbuf_,))
        for c in range(4):
            T(lambda h, c=c, ptv=ptv: h.transpose(out=ptv[:, 4 + c, :], in_=kd_bf[:, c * 128:(c + 1) * 128], identity=ident[:]),
              reads=kd_bufs + (buf("ident"),), writes=(pbuf_,))
        V(lambda h, ptv=ptv: h.tensor_copy(out=QdT[:, :, tsl], in_=ptv[:, 0:4, :]), reads=(pbuf_,), writes=(buf(f"QdT{i}"),))
        A(lambda h, ptv=ptv: h.activation(out=KdT[:, :, tsl], in_=ptv[:, 4:8, :], func=AF.Identity), reads=(pbuf_,), writes=(buf(f"KdT{i}"),))

        if TL < 5:
            return
        pt_, pbuf_ = pb_ring.next()
        ptv = pt_[:].rearrange("p (c t) -> p c t", t=128)
        for c in range(5):
            T(lambda h, c=c, ptv=ptv: h.transpose(out=ptv[:, c, :], in_=cn_bf[:, c * 128:(c + 1) * 128], identity=ident[:]),
              reads=(buf("cn_q") if c < 3 else buf("cn_kv"), buf("ident")), writes=(pbuf_,))
        V(lambda h, ptv=ptv: h.tensor_copy(out=cnT[:], in_=ptv[:, 0:5, :]), reads=(pbuf_,), writes=(buf("cnT"),))

        for (h0, h1) in ((0, 5), (5, 8)):
            c0, c1 = h0 * 96, h1 * 96
            nh = h1 - h0
            pt, pbuf = pf_ring.next()
            for c in range(3):
                T(lambda h, c=c, pt=pt, c0=c0, c1=c1: h.matmul(pt[:, 0:c1 - c0], lhsT=cnT[:, c, :], rhs=w_uq_sb[:, c, c0:c1],
                                                                start=(c == 0), stop=(c == 2)),
                  reads=(buf("cnT"), buf(f"w_uq{c}")), writes=(pbuf,))
            pv = pt[:, 0:nh * 96].rearrange("p (h d) -> p h d", d=96)
            qv = q_bf[:, h0:h1, :]
            cmb = ropem[:, i, 0, :].unsqueeze(1).to_broadcast([128, nh, 16])
            smb = ropem[:, i, 1, :].unsqueeze(1).to_broadcast([128, nh, 16])
            x1 = pv[:, :, 64:80]
            x2 = pv[:, :, 80:96]
            t0 = rt[:, 0, 0:nh, :]
            t1 = rt[:, 1, 0:nh, :]
            t2 = rt[:, 2, 0:nh, :]
            t3 = rt[:, 3, 0:nh, :]
            V(lambda h, pv=pv, qv=qv: h.tensor_scalar(out=qv[:, :, 0:64], in0=pv[:, :, 0:64], scalar1=MLA_SCALE, scalar2=None, op0=ALU.mult),
              reads=(pbuf,), writes=(buf(f"q_bf_n{h0}"),))
            V(lambda h, t0=t0, x1=x1, cmb=cmb: h.tensor_tensor(out=t0, in0=x1, in1=cmb, op=ALU.mult),
              reads=(pbuf, buf("ropem")), writes=(buf("rt0"),))
            V(lambda h, t1=t1, x2=x2, smb=smb: h.tensor_tensor(out=t1, in0=x2, in1=smb, op=ALU.mult),
              reads=(pbuf, buf("ropem")), writes=(buf("rt1"),))
            V(lambda h, t2=t2, x2=x2, cmb=cmb: h.tensor_tensor(out=t2, in0=x2, in1=cmb, op=ALU.mult),
              reads=(pbuf, buf("ropem")), writes=(buf("rt2"),))
            V(lambda h, t3=t3, x1=x1, smb=smb: h.tensor_tensor(out=t3, in0=x1, in1=smb, op=ALU.mult),
              reads=(pbuf, buf("ropem")), writes=(buf("rt3"),))
            V(lambda h, t0=t0, t1=t1: h.tensor_tensor(out=t0, in0=t0, in1=t1, op=ALU.subtract),
              reads=(buf("rt0"), buf("rt1")), writes=(buf("rt0"),))
            V(lambda h, t2=t2, t3=t3: h.tensor_tensor(out=t2, in0=t2, in1=t3, op=ALU.add),
              reads=(buf("rt2"), buf("rt3")), writes=(buf("rt2"),))
            V(lambda h, t0=t0, qv=qv: h.tensor_scalar(out=qv[:, :, 64:80], in0=t0, scalar1=MLA_SCALE, scalar2=None, op0=ALU.mult),
              reads=(buf("rt0"),), writes=(buf(f"q_bf_a{h0}"),))
            V(lambda h, t2=t2, qv=qv: h.tensor_scalar(out=qv[:, :, 80:96], in0=t2, scalar1=MLA_SCALE, scalar2=None, op0=ALU.mult),
              reads=(buf("rt2"),), writes=(buf(f"q_bf_b{h0}"),))
        q_bufs = tuple(buf(f"q_bf_{s}{h0}") for s in "nab" for h0 in (0, 5))
        if TL < 6:
            return
        for gidx in range(2):
            pt, pbuf = pf_ring.next()
            for c in range(2):
                T(lambda h, c=c, pt=pt, gidx=gidx: h.matmul(pt[:, :], lhsT=cnT[:, 3 + c, :],
                                                            rhs=w_ukv_sb[:, c, gidx * 512:(gidx + 1) * 512],
                                                            start=(c == 0), stop=(c == 1)),
                  reads=(buf("cnT"), buf(f"w_ukv{c}")), writes=(pbuf,))
            pv = pt[:].rearrange("p (h d) -> p h d", d=128)
            A(lambda h, pv=pv, gidx=gidx: h.activation(out=k_bf[:, gidx * 4:(gidx + 1) * 4, 0:64], in_=pv[:, :, 0:64], func=AF.Identity),
              reads=(pbuf,), writes=(buf(f"k_bf_n{gidx}"),))
            V(lambda h, pv=pv, gidx=gidx: h.tensor_copy(out=Vaug[:, i, gidx * 4:(gidx + 1) * 4, 0:64], in_=pv[:, :, 64:128]),
              reads=(pbuf,), writes=(buf(f"Vm{i}"),))
        k_bufs = (buf("k_bf_n0"), buf("k_bf_n1"), buf("k_bf_r"))
        if TL < 7:
            return
        for (src, sbufs, dst, dname, ev) in ((q_bf, q_bufs, QT, "QT", "vector"), (k_bf, k_bufs, KT, "KT", "scalar")):
            pt_, pbuf_ = pb_ring.next()
            ptv = pt_[:].rearrange("p (c t) -> p c t", t=128)
            for hh in range(8):
                T(lambda h, hh=hh, ptv=ptv, src=src: h.transpose(out=ptv[0:96, hh, :], in_=src[:, hh, :], identity=ident[:]),
                  reads=sbufs + (buf("ident"),), writes=(pbuf_,))
            if ev == "vector":
                V(lambda h, ptv=ptv, dst=dst: h.tensor_copy(out=dst[0:96, :, tsl], in_=ptv[0:96, :, :]),
                  reads=(pbuf_,), writes=(buf(f"{dname}{i}"),))
            else:
                A(lambda h, ptv=ptv, dst=dst: h.activation(out=dst[0:96, :, tsl], in_=ptv[0:96, :, :], func=AF.Identity),
                  reads=(pbuf_,), writes=(buf(f"{dname}{i}"),))

    if STOP == "weights":
        dbg["w"] = dout("dbg_w", [128, IN_W], BF16)
        if LVL >= 2:
            tok = P.op("sync", lambda h: h.dma_start(out=dbg["w"][:, :], in_=w_in_sb[:, 0, :]),
                       reads=(buf("w_in0"),), writes=(), dma_buf=buf("dbgw"))
        else:
            tok = P.op("sync", lambda h: h.dma_start(out=dbg["w"][:, 0:128], in_=ident[:, :]),
                       reads=(buf("ident"),), writes=(), dma_buf=buf("dbgw"))
        P.final_tokens.append(tok)
    else:
        for i in range(NT if NT_RUN is None else NT_RUN):
            phase1_tile(i)

    if debug and phases == 1 and STOP is None:
        allb = [buf(f"{n}{i}") for n in ("QT", "KT", "QdT", "KdT", "Vm", "Vd") for i in range(NT if NT_RUN is None else NT_RUN)]
        for nm, ap, shp in (("QT", QT[0:96], [96, 8, S]), ("KT", KT[0:96], [96, 8, S]), ("QdT", QdT[:], [128, 4, S]),
                            ("KdT", KdT[:], [128, 4, S]), ("Vaug", Vaug[:], [128, NT, 8, 65]), ("Vdaug", Vdaug[:], [128, NT, 8, 65])):
            if DBG_ONLY and nm not in DBG_ONLY:
                continue
            dbg[nm] = dout("dbg_" + nm, shp, BF16)
            tok = P.op("sync", lambda h, nm=nm, ap=ap: h.dma_start(out=dbg[nm][:], in_=ap),
                       reads=tuple(allb) + (buf("Vaug_ones"), buf("Vdaug_ones")), writes=(), dma_buf=buf("dbg" + nm))
            P.final_tokens.append(tok)

    def alias(new_names, old_bufs):
        toks = []
        for ob in old_bufs:
            if ob.w is not None:
                toks.append(ob.w)
            toks.extend(ob.r)
        for n in new_names:
            nb = buf(n)
            nb.r = list(nb.r) + toks

    def mm(out, lhsT, rhs, start, stop, reads, writes, skip=False):
        if skip:
            T(lambda h: h.matmul(out, lhsT=lhsT, rhs=rhs, start=start, stop=stop, skip_group_check=True), reads, writes)
        else:
            T(lambda h: h.matmul(out, lhsT=lhsT, rhs=rhs, start=start, stop=stop), reads, writes)

    def act(out, in_, func, reads, writes, bias=None, scale=None, accum=None):
        kw = {}
        if bias is not None:
            kw["bias"] = bias
        if scale is not None:
            kw["scale"] = scale
        if accum is not None:
            kw["accum_out"] = accum
        A(lambda h: h.activation(out=out, in_=in_, func=func, **kw), reads, writes)

    def vtt(out, in0, in1, op, reads, writes):
        V(lambda h: h.tensor_tensor(out=out, in0=in0, in1=in1, op=op), reads, writes)

    def vts(out, in0, s1, op0, reads, writes, s2=None, op1=None):
        if op1 is None:
            V(lambda h: h.tensor_scalar(out=out, in0=in0, scalar1=s1, scalar2=None, op0=op0), reads, writes)
        else:
            V(lambda h: h.tensor_scalar(out=out, in0=in0, scalar1=s1, scalar2=s2, op0=op0, op1=op1), reads, writes)

    def vcopy(out, in_, reads, writes):
        V(lambda h: h.tensor_copy(out=out, in_=in_), reads, writes)

    def vrecip(out, in_, reads, writes):
        V(lambda h: h.reciprocal(out=out, in_=in_), reads, writes)

    if phases >= 2:
        p1_work = [buf(n) for n in ("xt0", "xt1", "junk", "hn_bf", "hnT")]
        w_bufs = [buf(f"w_in{c}") for c in range(8)] + [buf(f"w_uq{c}") for c in range(3)] + [buf(f"w_ukv{c}") for c in range(2)]
        pTs = [xt[0][:, k * 256:(k + 1) * 256].bitcast(BF16) for k in range(4)]
        alias([f"pT{k}" for k in range(4)] + ["rc"], p1_work)
        alias([f"o_mla{i}" for i in range(NT)] + [f"o_dil{i}" for i in range(NT)], w_bufs)
        pT_ring = Ring([(pTs[k], buf(f"pT{k}")) for k in range(4)])
        s_ring = Ring([(pf[k], buf(f"pf{k}")) for k in range(3)])
        acc_ring = Ring([(pf[3 + k], buf(f"pf{3 + k}")) for k in range(2)])
        rc = xt[1][:, 0:8]

        def attention(kind):
            for hd in range(8):
                if kind == "mla":
                    Qop = QT[0:96, hd, :]
                    Kop = KT[0:96, hd, :]
                    Vt = Vaug
                    qbufs = [buf(f"QT{i}") for i in range(NT)]
                    kbufs = [buf(f"KT{i}") for i in range(NT)]
                    vbufs_ = [buf(f"Vm{i}") for i in range(NT)]
                    vones = buf("Vaug_ones")
                    o_sb, oname = o_mla_sb, "o_mla"
                else:
                    r0 = (hd % 2) * 64
                    Qop = QdT[r0:r0 + 64, hd // 2, :]
                    Kop = KdT[r0:r0 + 64, hd // 2, :]
                    Vt = Vdaug
                    qbufs = [buf(f"QdT{i}") for i in range(NT)]
                    kbufs = [buf(f"KdT{i}") for i in range(NT)]
                    vbufs_ = [buf(f"Vd{i}") for i in range(NT)]
                    vones = buf("Vdaug_ones")
                    o_sb, oname = o_dil_sb, "o_dil"
                for qb in range(4):
                    acc, accb = acc_ring.next()
                    accv = acc[:, 0:264].rearrange("p (q d) -> p q d", d=66)
                    kts = []
                    for kt in range(NT):
                        d0 = qb * 512 - kt * 128
                        if kind == "dil" and (d0 < -1408 or d0 > 1024):
                            continue
                        kts.append(kt)
                    state = {"first": True}

                    def pv(kt, pT, pTb, last):
                        for qt in range(4):
                            st_flag = state["first"]
                            mm(accv[:, qt, 0:65], pT[:, qt * 128:(qt + 1) * 128], Vt[:, kt, hd, 0:65],
                               st_flag, last and qt == 3, (pTb, vbufs_[kt], vones), (accb,))
                            state["first"] = False

                    prev = None
                    for kt in kts:
                        sp, spb = s_ring.next()
                        mm(sp[:, 0:512], Kop[:, kt * 128:(kt + 1) * 128], Qop[:, qb * 512:(qb + 1) * 512], True, True,
                           (kbufs[kt],) + tuple(qbufs[qb * 4:(qb + 1) * 4]), (spb,))
                        pT, pTb = pT_ring.next()
                        act(pT, sp[:, 0:512], AF.Exp, (spb,), (pTb,))
                        if kind == "dil":
                            base = qb * 512 - kt * 128 + 1408
                            vtt(pT, pT, dmask[:, base:base + 512], ALU.mult, (pTb, buf("dmask")), (pTb,))
                        if prev is not None:
                            pv(*prev, False)
                        prev = (kt, pT, pTb)
                    pv(*prev, True)
                    vrecip(rc[:, 0:4], accv[:, :, 64], (accb,), (buf("rc"),))
                    obufs = tuple(buf(f"{oname}{qb * 4 + qt}") for qt in range(4))
                    vtt(o_sb[:, qb * 4:(qb + 1) * 4, hd * 64:(hd + 1) * 64], accv[:, :, 0:64],
                        rc[:, 0:4].unsqueeze(2).to_broadcast([128, 4, 64]), ALU.mult, (accb, buf("rc")), obufs)

        attention("mla")
        attention("dil")

    if debug and phases == 2:
        for nm, ap in (("o_mla", o_mla_sb), ("o_dil", o_dil_sb)):
            dbg[nm] = dout("dbg_" + nm, [128, NT, 512], BF16)
            tok = P.op("sync", lambda h, nm=nm, ap=ap: h.dma_start(out=dbg[nm][:], in_=ap[:]),
                       reads=tuple(buf(f"{nm}{i}") for i in range(NT)), writes=(), dma_buf=buf("dbg" + nm))
            P.final_tokens.append(tok)

    if phases >= 3:
        qkv_bufs = [buf(f"{n}{i}") for n in ("QT", "KT", "QdT", "KdT", "Vm", "Vd") for i in range(NT)] + \
                   [buf("Vaug_ones"), buf("Vdaug_ones")]
        NGB = 13
        uvbufs = [regB[:, 26624 + k * 2048: 26624 + (k + 1) * 2048] for k in range(8)] + [regA[:, 16384:18432]] + \
                 [p1w[:, k * 2048:(k + 1) * 2048] for k in range(4)]
        diagt = sb("diagt", [128, 4, 128], BF16)
        diag_ring = Ring([(diagt[:, k, :], buf(f"diag{k}")) for k in range(4)])
        gffn_bc3 = regB[:, 43008:45056].bitcast(F32)
        gfin_bc3 = regB[:, 45056:47104].bitcast(F32)
        hbuf = regB[:, 47104:49152].bitcast(F32)
        xn2a = regB[:, 49152:51200].bitcast(F32)
        xn2b = regA[:, 18432:20480].bitcast(F32)
        gates2 = regA[:, 20480:20736].bitcast(F32).rearrange("p (h k) -> p h k", h=8)
        gelu_t = sb("gelu_t", [128, 128], F32)
        ybuf = regB[:, 51200:53248].bitcast(F32)
        Ssc = regB[:, 53248:57344].bitcast(F32)
        cand = regB[:, 57344:59392].bitcast(F32)
        oh = regB[:, 59392:61440].bitcast(F32)
        gate_sb = regB[:, 61440:62464].bitcast(F32)
        qTp = regB[:, 62464:63488].rearrange("p (h t) -> p h t", t=128)
        v12 = regB[:, 63488:64000].bitcast(F32).rearrange("p (a h k) -> p a h k", a=2, h=8)
        i12 = regB[:, 64000:64512].bitcast(U32).rearrange("p (a h k) -> p a h k", a=2, h=8)
        i12f = regB[:, 64512:65024].bitcast(F32).rearrange("p (a h k) -> p a h k", a=2, h=8)
        wk = regB[:, 65024:65536].bitcast(F32)
        top = regB[:, 65536:65792].bitcast(F32).rearrange("p (h k) -> p h k", h=8)
        x0 = xt[0]
        pos = x0[:, 0:128].bitcast(U32).rearrange("p (h k) -> p h k", h=8)
        ai = x0[:, 128:256].bitcast(U32).rearrange("p (h k) -> p h k", h=8)
        bi = x0[:, 256:384].bitcast(U32).rearrange("p (h k) -> p h k", h=8)
        af = x0[:, 384:512].rearrange("p (h k) -> p h k", h=8)
        bf = x0[:, 512:640].rearrange("p (h k) -> p h k", h=8)
        i1sel = x0[:, 640:768].rearrange("p (h k) -> p h k", h=8)
        i2sel = x0[:, 768:896].rearrange("p (h k) -> p h k", h=8)
        eidxf = x0[:, 896:1024]
        x1_ = xt[1]
        eidxi = x1_[:, 0:128].bitcast(I32)
        gates = x1_[:, 128:256].rearrange("p (h k) -> p h k", h=8)
        avals = x1_[:, 256:384]
        coef = x1_[:, 384:512]
        ptile = x1_[:, 512:768]
        p_bf = x1_[:, 768:896].bitcast(BF16)
        pTt = x1_[:, 896:1024].bitcast(BF16).rearrange("p (c t) -> p c t", t=128)
        keys_bf = p1w[:, 8192:8320]

        p3_names = ["w_out%d" % c for c in range(8)] + ["w_pq%d" % c for c in range(8)] + ["w_pg%d" % c for c in range(8)] + \
                   ["w_pp0", "w_pp1", "gffn", "gfin", "hbuf0", "xn20", "hbuf1", "Ssc", "cand", "oh", "gate_sb", "qTp",
                    "v12", "i12", "i12f", "wk", "top"] + [f"ohs{k}" for k in range(8)] + [f"v12lo{j}" for j in range(16)] + \
                   [f"v12hi{j}" for j in range(16)] + [f"i12_{j}" for j in range(16)] + [f"toplo{h}" for h in range(8)] + [f"tophi{h}" for h in range(8)] + [f"uvb{k}" for k in range(8)]
        alias(p3_names, qkv_bufs)
        alias(["uvb8", "xn21", "gates1"], w_bufs)
        x0_names = [f"pos{h}" for h in range(8)] + ["ai", "bi", "af", "bf", "i1sel", "i2sel", "eidxf"]
        x1_names = ["eidxi0", "gates0", "ptile", "p_bf", "pTt"] + [f"coef{j}" for j in range(128)] + [f"avals{j}" for j in range(128)]
        alias(x0_names, [buf(f"pT{k}") for k in range(4)] + [buf("xt0")])
        alias(x1_names, [buf("rc"), buf("xt1")])
        all_prev = [b_ for n_, b_ in list(B.items()) if not n_.startswith("pf") and not n_.startswith("pb")]
        alias(["keys_bf"] + [f"uvb{k}" for k in range(9, 13)], all_prev)

        for c in range(8):
            dma("gpsimd", w_out_sb[:, c, :], w_out_d[c * 128:(c + 1) * 128, :], buf(f"w_out{c}"), True)
            vts(w_out_sb[:, c, :], w_out_sb[:, c, :], gcols[:, 13 + c:14 + c], ALU.mult,
                (buf(f"w_out{c}"), buf("gcols")), (buf(f"w_out{c}"),))
        for c in range(8):
            dma("gpsimd", w_pq_sb[:, c, :], w_pq_d[c * 128:(c + 1) * 128, :], buf(f"w_pq{c}"), True)
        for c in range(8):
            dma("gpsimd", w_pg_sb[:, c, :], w_pg_d[c * 128:(c + 1) * 128, :], buf(f"w_pg{c}"), True)
            vts(w_pg_sb[:, c, :], w_pg_sb[:, c, :], gcols[:, 21 + c:22 + c], ALU.mult,
                (buf(f"w_pg{c}"), buf("gcols")), (buf(f"w_pg{c}"),))
        for c in range(2):
            dma("gpsimd", w_pp_sb[:, c, :], w_pp_d[c * 128:(c + 1) * 128, :], buf(f"w_pp{c}"), True)
        dma("sync", gffn_bc3, g_ffn_d.partition_broadcast(128), buf("gffn"), True)
        dma("sync", gfin_bc3, g_fin_d.partition_broadcast(128), buf("gfin"), True)
        dma("gpsimd", keys_bf, keys_d[:, :], buf("keys_bf"), True)
        pt_, pbuf_ = pb_ring.next()
        T(lambda h: h.transpose(out=pt_[:, 0:128], in_=keys_bf, identity=ident[:]), reads=(buf("keys_bf"), buf("ident")), writes=(pbuf_,))
        vcopy(keysT[:], pt_[:, 0:128], (pbuf_,), (buf("keysT"),))
        w_out_bufs = tuple(buf(f"w_out{c}") for c in range(8))
        w_pq_bufs = tuple(buf(f"w_pq{c}") for c in range(8))
        w_pg_bufs = tuple(buf(f"w_pg{c}") for c in range(8))
        pf3_ring = Ring([(pf[k], buf(f"pf{k}")) for k in range(4)])
        uv_ring = Ring([(uvbufs[k], buf(f"uvb{k}")) for k in range(NGB)])
        xn2s = [(xn2a, buf("xn20")), (xn2b, buf("xn21"))]
        gatess = [(gates, buf("gates0")), (gates2, buf("gates1"))]

        def vmax(out, in_, reads, writes):
            V(lambda h: h.max(out=out, in_=in_), reads, writes)

        def vmatch(out, rep, vals, reads, writes):
            V(lambda h: h.match_replace(out=out, in_to_replace=rep, in_values=vals, imm_value=-1e30), reads, writes)

        def vmaxidx(out, mx, vals, reads, writes):
            V(lambda h: h.max_index(out=out, in_max=mx, in_values=vals), reads, writes)

        def vstt(out, in0, scalar, in1, op0, op1, reads, writes, accum=None):
            if accum is None:
                V(lambda h: h.scalar_tensor_tensor(out=out, in0=in0, scalar=scalar, in1=in1, op0=op0, op1=op1), reads, writes)
            else:
                V(lambda h: h.scalar_tensor_tensor(out=out, in0=in0, scalar=scalar, in1=in1, op0=op0, op1=op1,
                                                   accum_out=accum), reads, writes)

        def vreduce(out, in_, op, reads, writes):
            V(lambda h: h.tensor_reduce(out=out, in_=in_, axis=AX.X, op=op), reads, writes)

        def norm_rstd(src_ap, col, n, src_bufs, bname):
            act(junk[:, 0:n], src_ap, AF.Square, tuple(src_bufs), (buf("junk"), buf(bname)), accum=st[:, col:col + 1])
            return rstd_from_ss(st[:, col:col + 1], st[:, col:col + 1], n, bname)

        def gather(dst, dst_buf, table, col, tname, ei, eib):
            P.op("gpsimd", lambda h: h.indirect_dma_start(out=dst, out_offset=None, in_=table[:, :],
                                                          in_offset=bass.IndirectOffsetOnAxis(ap=ei[:, col:col + 1], axis=0)),
                 reads=(eib, buf(tname)), writes=(dst_buf,), dma_buf=dst_buf)

        hbufs = [(hbuf, buf("hbuf0")), (ybuf, buf("hbuf1"))]
        eidxi2 = sb("eidxi2", [128, 128], I32)
        eidxis = [(eidxi, buf("eidxi0")), (eidxi2[:], buf("eidxi1"))]

        def p3_pre(i):
            tsl = slice(i * 128, (i + 1) * 128)
            hb, hbb = hbufs[i % 2]
            ei, eib = eidxis[i % 2]
            xn2, xnb = xn2s[i % 2]
            gates, gtb = gatess[i % 2]
            rm = norm_rstd(o_mla_sb[:, i, :], 3, 512, (buf(f"o_mla{i}"),), "ssm")
            act(hn_bf[:, 0:512], o_mla_sb[:, i, :], AF.Identity, (buf(f"o_mla{i}"), rm), (buf("hn_bf"),), scale=st[:, 3:4])
            rd = norm_rstd(o_dil_sb[:, i, :], 4, 512, (buf(f"o_dil{i}"),), "ssd")
            act(hn_bf[:, 512:1024], o_dil_sb[:, i, :], AF.Identity, (buf(f"o_dil{i}"), rd), (buf("hn_bf"),), scale=st[:, 4:5])
            transposes(lambda c: hn_bf[:, c * 128:(c + 1) * 128], 8, 128, buf("hn_bf"), hnT[:], buf("hnT"), evac="scalar")
            dma("sync", hb, x_d[tsl, :], hbb, True)
            for cg in range(2):
                pt, pbuf = pf3_ring.next()
                for c in range(8):
                    mm(pt[:, :], hnT[:, c, :], w_out_sb[:, c, cg * 512:(cg + 1) * 512], c == 0, c == 7,
                       (buf("hnT"), w_out_bufs[c]), (pbuf,))
                P.lag(LAG)
                vtt(hb[:, cg * 512:(cg + 1) * 512], hb[:, cg * 512:(cg + 1) * 512], pt[:, :], ALU.add,
                    (hbb, pbuf), (hbb,))
            r2 = norm_rstd(hb, 5, D, (hbb,), "ss2")
            vstt(xn2, hb, st[:, 5:6], gffn_bc3, ALU.mult, ALU.mult, (hbb, r2, buf("gffn")), (xnb,))
            act(hn_bf[:], xn2, AF.Identity, (xnb,), (buf("hn_bf"),))
            transposes(lambda c: hn_bf[:, c * 128:(c + 1) * 128], 8, 128, buf("hn_bf"), hnT[:], buf("hnT"), evac="scalar")
            for g4 in range(2):
                pt, pbuf = pf3_ring.next()
                ptv = pt[:].rearrange("p (h t) -> p h t", t=128)
                for hl in range(4):
                    hd = g4 * 4 + hl
                    for c in range(8):
                        mm(ptv[:, hl, :], w_pq_sb[:, c, hd * 128:(hd + 1) * 128], hnT[:, c, :],
                           hl == 0 and c == 0, hl == 3 and c == 7, (buf("hnT"), w_pq_bufs[c]), (pbuf,))
                act(qTp[:, g4 * 4:(g4 + 1) * 4, :], ptv, AF.Identity, (pbuf,), (buf("qTp"),))
            for g4 in range(2):
                banks = [pf3_ring.next(), pf3_ring.next()]
                for hl in range(4):
                    hd = g4 * 4 + hl
                    for half in range(2):
                        pt, pbuf = banks[half]
                        r0 = half * 64
                        mm(pt[:, hl * 128:(hl + 1) * 128], qTp[r0:r0 + 64, hd, :], keysT[r0:r0 + 64, :], True, True,
                           (buf("qTp"), buf("keysT")), (pbuf,))
                for half in range(2):
                    pt, pbuf = banks[half]
                    j0 = half * 8 + g4 * 4
                    act(Ssc[:, j0 * 128:(j0 + 4) * 128], pt[:, :], AF.Identity, (pbuf,), (buf("Ssc"),))
            P.lag(2 * LAG)
            ohs = [buf(f"ohs{k}") for k in range(8)]
            v12lo = [buf(f"v12lo{j}") for j in range(16)]
            v12hi = [buf(f"v12hi{j}") for j in range(16)]
            i12b = [buf(f"i12_{j}") for j in range(16)]
            for grp in range(2):
                probs = [(grp * 8 + k, k) for k in range(8)]

                def views(j):
                    half, hd = j // 8, j % 8
                    return Ssc[:, j * 128:(j + 1) * 128], v12[:, half, hd, :], i12[:, half, hd, :]
                for (j, k) in probs:
                    Sv, vv, iv = views(j)
                    vmax(vv[:, 0:8], Sv, (buf("Ssc"),), (v12lo[j],))
                for (j, k) in probs:
                    Sv, vv, iv = views(j)
                    vmatch(oh[:, k * 128:(k + 1) * 128], vv[:, 0:8], Sv, (buf("Ssc"), v12lo[j]), (ohs[k],))
                for (j, k) in probs:
                    Sv, vv, iv = views(j)
                    vmax(vv[:, 8:16], oh[:, k * 128:(k + 1) * 128], (ohs[k],), (v12hi[j],))
                for (j, k) in probs:
                    Sv, vv, iv = views(j)
                    vmaxidx(iv[:, 0:8], vv[:, 0:8], Sv, (buf("Ssc"), v12lo[j]), (i12b[j],))
                for (j, k) in probs:
                    Sv, vv, iv = views(j)
                    vmaxidx(iv[:, 8:16], vv[:, 8:16], Sv, (buf("Ssc"), v12hi[j]), (i12b[j],))
            v12all = tuple(v12lo) + tuple(v12hi)
            vcopy(i12f[:], i12[:], tuple(i12b), (buf("i12f"),))
            vts(i12f[:, 0, :, :], i12f[:, 0, :, :], 128.0, ALU.mult, (buf("i12f"),), (buf("i12f"),))
            toplo = [buf(f"toplo{h}") for h in range(8)]
            tophi = [buf(f"tophi{h}") for h in range(8)]
            posb = [buf(f"pos{h}") for h in range(8)]
            for g4 in range(2):
                hs = slice(g4 * 4, (g4 + 1) * 4)
                candv = cand.rearrange("p (h a b) -> p h a b", h=4, a=16)
                vtt(candv, v12[:, 0, hs, :].unsqueeze(3).to_broadcast([128, 4, 16, 16]),
                    v12[:, 1, hs, :].unsqueeze(2).to_broadcast([128, 4, 16, 16]), ALU.add, v12all, (buf("cand"),))
                hds = [(g4 * 4 + hl, hl) for hl in range(4)]
                for (hd, hl) in hds:
                    vmax(top[:, hd, 0:8], cand[:, hl * 256:(hl + 1) * 256], (buf("cand"),), (toplo[hd],))
                for (hd, hl) in hds:
                    vmatch(oh[:, hl * 256:(hl + 1) * 256], top[:, hd, 0:8], cand[:, hl * 256:(hl + 1) * 256],
                           (buf("cand"), toplo[hd]), (ohs[2 * hl], ohs[2 * hl + 1]))
                for (hd, hl) in hds:
                    vmax(top[:, hd, 8:16], oh[:, hl * 256:(hl + 1) * 256], (ohs[2 * hl], ohs[2 * hl + 1]), (tophi[hd],))
                for (hd, hl) in hds:
                    vmaxidx(pos[:, hd, 0:8], top[:, hd, 0:8], cand[:, hl * 256:(hl + 1) * 256], (buf("cand"), toplo[hd]), (posb[hd],))
                for (hd, hl) in hds:
                    vmaxidx(pos[:, hd, 8:16], top[:, hd, 8:16], cand[:, hl * 256:(hl + 1) * 256], (buf("cand"), tophi[hd]), (posb[hd],))
            topall = tuple(toplo) + tuple(tophi)
            posall = tuple(posb)
            ohall = (buf("oh"),) + tuple(ohs)
            vts(ai[:], pos[:], 4, ALU.logical_shift_right, posall, (buf("ai"),))
            vts(bi[:], pos[:], 15, ALU.bitwise_and, posall, (buf("bi"),))
            vcopy(af[:], ai[:], (buf("ai"),), (buf("af"),))
            vcopy(bf[:], bi[:], (buf("bi"),), (buf("bf"),))
            iob = iota16[:].unsqueeze(1).unsqueeze(1).to_broadcast([128, 4, 16, 16])
            for (selv, srcv, half, sname, fname) in ((i1sel, af, 0, "i1sel", "af"), (i2sel, bf, 1, "i2sel", "bf")):
                for g4 in range(2):
                    hs = slice(g4 * 4, (g4 + 1) * 4)
                    ohv = oh.rearrange("p (h k a) -> p h k a", h=4, k=16)
                    vtt(ohv, iob, srcv[:, hs, :].unsqueeze(3).to_broadcast([128, 4, 16, 16]), ALU.is_equal,
                        (buf("iota16"), buf(fname)), ohall)
                    vtt(ohv, ohv, i12f[:, half, hs, :].unsqueeze(2).to_broadcast([128, 4, 16, 16]), ALU.mult,
                        ohall + (buf("i12f"),), ohall)
                    vreduce(selv[:, hs, :], ohv, ALU.add, ohall, (buf(sname),))
            vtt(eidxf, i1sel.rearrange("p h k -> p (h k)"), i2sel.rearrange("p h k -> p (h k)"), ALU.add,
                (buf("i1sel"), buf("i2sel")), (buf("eidxf"),))
            vcopy(ei, eidxf, (buf("eidxf"),), (eib,))
            vtt(gates[:], top[:], top[:, :, 0:1].to_broadcast([128, 8, 16]), ALU.subtract, topall, (gtb,))
            act(gates[:], gates[:], AF.Tanh, (gtb,), (gtb,), scale=0.5)
            P.lag(LAG)
            gden = cand[:, 0:128].rearrange("p (h k) -> p h k", h=8)
            vts(gden, gates[:], -1.0, ALU.mult, (gtb,), (buf("cand"),), s2=1.0, op1=ALU.add)
            vrecip(gden, gden, (buf("cand"),), (buf("cand"),))
            vstt(gates[:], gates[:], 1.0, gden, ALU.add, ALU.mult, (gtb, buf("cand")), (gtb,))
            P.lag(LAG)
            vreduce(st[:, 8:16], gates[:], ALU.add, (gtb,), (buf("gsum"),))
            vrecip(st[:, 8:16], st[:, 8:16], (buf("gsum"),), (buf("gsum"),))
            vtt(gates[:], gates[:], st[:, 8:16].unsqueeze(2).to_broadcast([128, 8, 16]), ALU.mult,
                (gtb, buf("gsum")), (gtb,))

        def p3_uv(i, stepper):
            ei, eib = eidxis[i % 2]
            xn2, xnb = xn2s[i % 2]
            gates, gtb = gatess[i % 2]
            gflat = gates.rearrange("p h k -> p (h k)")
            yps = [(pf[4], buf("pf4")), (pf[5], buf("pf5"))]
            G = 1
            held = []

            def finish_group(g0):
                act(gelu_t[:, g0:g0 + G], avals[:, g0:g0 + G], AF.Gelu, tuple(buf(f"avals{j}") for j in range(g0, g0 + G)),
                    (buf(f"gelu{g0}"),))
                for (j, ub, ubb) in held:
                    act(coef[:, j:j + 1], gelu_t[:, j:j + 1], AF.Identity, (buf(f"gelu{g0}"), gtb), (buf(f"coef{j}"),),
                        scale=gflat[:, j:j + 1])
                    dg, dgb = diag_ring.next()
                    act(dg, ident[:], AF.Identity, (buf("ident"), buf(f"coef{j}")), (dgb,), scale=coef[:, j:j + 1])
                    for cg in range(2):
                        mm(yps[cg][0][:, :], dg, ub[:, D + cg * 512:D + (cg + 1) * 512], j == 0, j == 127,
                           (dgb, ubb), (yps[cg][1],))
                del held[:]

            for j in range(128):
                ub, ubb = uv_ring.next()
                gather(ub, ubb, uv16_d, j, "uv16", ei, eib)
                vstt(ub[:, 0:D], ub[:, 0:D], 1.0, xn2, ALU.mult, ALU.mult, (ubb, xnb), (ubb, buf(f"avals{j}")),
                     accum=avals[:, j:j + 1])
                held.append((j, ub, ubb))
                if j % G == G - 1:
                    finish_group(j - G + 1)
                if stepper is not None:
                    stepper.advance(STEP_K)
            return yps

        def p3_post(i, yps):
            tsl = slice(i * 128, (i + 1) * 128)
            hb, hbb = hbufs[i % 2]
            for cg in range(2):
                cs = slice(cg * 512, (cg + 1) * 512)
                vtt(hb[:, cs], hb[:, cs], yps[cg][0][:, :], ALU.add, (hbb, yps[cg][1]), (hbb,))
            r3 = norm_rstd(hb, 6, D, (hbb,), "ss3")
            vts(hn_bf[:], hb, st[:, 6:7], ALU.mult, (hbb, r3), (buf("hn_bf"),))
            transposes(lambda c: hn_bf[:, c * 128:(c + 1) * 128], 8, 128, buf("hn_bf"), hnT[:], buf("hnT"))
            dma("sync", ptile, p_d[tsl, :], buf("ptile"), True)
            vcopy(p_bf, ptile, (buf("ptile"),), (buf("p_bf"),))
            transposes(lambda c: p_bf[:, c * 128:(c + 1) * 128], 2, 128, buf("p_bf"), pTt, buf("pTt"))
            for cg in range(2):
                cs = slice(cg * 512, (cg + 1) * 512)
                pg, pgb = pf3_ring.next()
                for c in range(8):
                    mm(pg[:, :], hnT[:, c, :], w_pg_sb[:, c, cs], c == 0, c == 7, (buf("hnT"), w_pg_bufs[c]), (pgb,))
                pp, ppb = pf3_ring.next()
                for c in range(2):
                    mm(pp[:, :], pTt[:, c, :], w_pp_sb[:, c, cs], c == 0, c == 1, (buf("pTt"), buf(f"w_pp{c}")), (ppb,))
                act(gate_sb, pg[:, :], AF.Tanh, (pgb,), (buf("gate_sb"),), scale=0.5)
                vstt(gate_sb, gate_sb, 1.0, pp[:, :], ALU.add, ALU.mult, (buf("gate_sb"), ppb), (buf("gate_sb"),))
                vstt(hb[:, cs], gate_sb, 0.5, hb[:, cs], ALU.mult, ALU.add, (hbb, buf("gate_sb")), (hbb,))
            r4 = norm_rstd(hb, 7, D, (hbb,), "ss4")
            vstt(hb, hb, st[:, 7:8], gfin_bc3, ALU.mult, ALU.mult, (hbb, r4, buf("gfin")), (hbb,))
            tok = dma("sync", out_d[tsl, :], hb, hbb, False)
            P.final_tokens.append(tok)


        ntr = NT if NT_RUN is None else NT_RUN
        p3_pre(0)
        for i in range(ntr):
            stepper = None
            if i + 1 < ntr:
                stepper = Stepper(P, lambda i=i: p3_pre(i + 1))
            yps = p3_uv(i, stepper)
            if stepper is not None:
                stepper.finish()
            p3_post(i, yps)

    P.op("sync", None, extra=tuple(P.final_tokens))

    P.finalize_marks()
    esems = {e: es.enter_context(nc.semaphore(f"sem_{e}")) for e in Prog.ENGS}
    for b in P.dma_bufs:
        b.dsem = es.enter_context(nc.semaphore(f"ds_{b.name}"))
    block = es.enter_context(nc.Block())

    @block.sync
    def _(h):
        P.emit("sync", h, esems)

    @block.scalar
    def _(h):
        P.emit("scalar", h, esems)

    @block.vector
    def _(h):
        P.emit("vector", h, esems)

    @block.gpsimd
    def _(h):
        P.emit("gpsimd", h, esems)

    @block.tensor
    def _(h):
        P.emit("tensor", h, esems)

    es.close()
    return nc, dbg, declared


def _constants():
    ident = np.eye(128, dtype=np.float32)
    t = np.arange(S, dtype=np.float32)

    def tables(rot):
        inv = (np.float32(500000.0) ** (-np.arange(0, rot, 2, dtype=np.float32) / np.float32(rot))).astype(np.float32)
        ang = (t[:, None] * inv[None, :]).astype(np.float32)
        return np.cos(ang).astype(np.float32), np.sin(ang).astype(np.float32)

    cm, sm = tables(32)
    cd, sd = tables(16)
    ropem = np.stack([cm, sm], axis=1).reshape(NT, 128, 2, 16).transpose(1, 0, 2, 3).copy()
    roped = np.stack([cd, sd], axis=1).reshape(NT, 128, 2, 8).transpose(1, 0, 2, 3).copy()
    u = np.arange(2944)[None, :]
    p = np.arange(128)[:, None]
    d = u - p - 1408
    ad = np.abs(d)
    c = (ad <= 64).astype(np.float32) + ((d % 4 == 0) & (ad <= 256)) + ((d % 16 == 0) & (ad <= 1024))
    iota16 = np.tile(np.arange(16, dtype=np.float32)[None, :], (128, 1))
    return ident, ropem, roped, c.astype(np.float32), iota16


_CACHE = {}


def kernel(**inputs):
    return _run(inputs)


def _prep_maps(inputs):
    ident, ropem, roped, mask, iota16 = _constants()
    f = lambda a: np.ascontiguousarray(np.asarray(a, dtype=np.float32))

    def col(g, n):
        return f(g).reshape(n, 128).T

    gcols = np.concatenate([
        col(inputs["g_mix"][0], 8), col(inputs["g_cq"][0], 3), col(inputs["g_ckv"][0], 2),
        col(inputs["g_out_mla"][0], 4), col(inputs["g_out_dil"][0], 4), col(inputs["g_ple"][0], 8),
        col(inputs["g_ffn"][0], 8)], axis=1)
    gcols = np.ascontiguousarray(gcols)
    keys12 = np.ascontiguousarray(np.concatenate([f(inputs["peer_keys1"][0]), f(inputs["peer_keys2"][0])], axis=1))
    shared = {
        "w_in": f(inputs["w_in"][0]), "w_uq": f(inputs["w_uq"][0]), "w_ukv": f(inputs["w_ukv"][0]),
        "w_out": f(inputs["w_out"][0]), "w_peer_q": f(inputs["w_peer_q"][0]), "keys12": keys12,
        "peer_u": f(inputs["peer_u"][0]), "peer_v": f(inputs["peer_v"][0]),
        "w_ple_gate": f(inputs["w_ple_gate"][0]), "w_ple_proj": f(inputs["w_ple_proj"][0]),
        "g_ffn": f(inputs["g_ffn"][0]), "g_final": f(inputs["g_final"]), "gcols": gcols,
        "ident": ident, "ropem": ropem, "roped": roped, "dilmask": mask, "iota16": iota16,
    }
    x = f(inputs["x"])
    p = f(inputs["p"][0])
    return [dict(shared, x=x[b], p=p[b]) for b in range(NCORES)]


def _run(inputs, debug=False, phases=3, cores=NCORES):
    nc, dbg, declared = build_program(debug=debug, phases=phases)
    maps = [{k: m[k] for k in declared} for m in _prep_maps(inputs)[:cores]]
    res = run_bass_kernel_spmd(nc, maps, core_ids=list(range(cores)))
    if debug:
        return res
    return np.stack([r["out"] for r in res.results], axis=0).astype(np.float32)
```
